# Optimizing a Trainium2 kernel written in Bass

```python
import math
import jax
import jax.numpy as jnp
from jax import lax
import numpy as np

D_MODEL = 1024
BATCH = 4
SEQ = 4096
DEPTH = 4

HEAD_DIM = 64
RMS_EPS = 1e-6
D_FF = 2816

A_HEADS = 8
IDX_HEADS = 4
IDX_DIM = 64
TOPK_MAX = 256
Q_BLOCK = 128
A_WIDTH = A_HEADS * HEAD_DIM

S5_WIDTH = 512
S5_GROUP = 16
S5_GROUPS = S5_WIDTH // S5_GROUP
S5_STATE = 64
S5_DT_MIN = 1e-3
S5_DT_MAX = 1e-1

DIL_PAIRS = ((128, 1), (512, 4), (2048, 16))
C_HEADS_PER_GROUP = 4
C_HEADS = C_HEADS_PER_GROUP * len(DIL_PAIRS)
C_WIDTH = C_HEADS * HEAD_DIM
C_OUT = C_HEADS_PER_GROUP * HEAD_DIM

N_BRANCHES = 3
IN_SIZES = (A_WIDTH, HEAD_DIM, HEAD_DIM, IDX_HEADS * IDX_DIM, IDX_DIM, IDX_HEADS,
            S5_WIDTH, C_WIDTH, C_WIDTH, C_WIDTH, N_BRANCHES * D_MODEL)
D_IN = sum(IN_SIZES)

kernel_name = "hybrid_dsa_s5_dilated_macaron"


def rmsnorm(x, g):
    xf = x.astype(jnp.float32)
    y = xf * lax.rsqrt(jnp.mean(xf * xf, axis=-1, keepdims=True) + RMS_EPS)
    return (y * g.astype(jnp.float32)).astype(x.dtype)


def swiglu(x, w_gate, w_up, w_down):
    return (jax.nn.silu(x @ w_gate) * (x @ w_up)) @ w_down


def split_columns(proj):
    parts, start = [], 0
    for size in IN_SIZES:
        parts.append(proj[..., start:start + size])
        start += size
    return parts


def dsa_attention(q, k, v, q_idx, k_idx, w_idx):
    bsz, seq = q.shape[0], q.shape[1]
    topk = min(TOPK_MAX, seq // 4)
    n_blk = seq // Q_BLOCK
    key_pos = jnp.arange(seq)
    k_idx32 = k_idx.astype(jnp.float32)

    def one_block(i):
        start = i * Q_BLOCK
        qb = lax.dynamic_slice_in_dim(q, start, Q_BLOCK, axis=1)
        qib = lax.dynamic_slice_in_dim(q_idx, start, Q_BLOCK, axis=1).astype(jnp.float32)
        wb = lax.dynamic_slice_in_dim(w_idx, start, Q_BLOCK, axis=1).astype(jnp.float32)
        q_pos = start + jnp.arange(Q_BLOCK)
        logits = jnp.einsum('bthd,bsd->bths', qib, k_idx32) * (IDX_DIM ** -0.5)
        score = jnp.einsum('bths,bth->bts', jax.nn.relu(logits), wb) * (IDX_HEADS ** -0.5)
        causal = key_pos[None, :] <= q_pos[:, None]
        score = jnp.where(causal[None], score, -jnp.inf)
        _, sel = lax.top_k(score, topk)
        k_sel = jax.vmap(lambda kk, ii: kk[ii])(k, sel)
        v_sel = jax.vmap(lambda vv, ii: vv[ii])(v, sel)
        att = jnp.einsum('bthd,btkd->bthk', qb, k_sel).astype(jnp.float32) * (HEAD_DIM ** -0.5)
        valid = sel <= q_pos[None, :, None]
        att = jnp.where(valid[:, :, None, :], att, -jnp.inf)
        p = jax.nn.softmax(att, axis=-1).astype(v.dtype)
        return jnp.einsum('bthk,btkd->bthd', p, v_sel)

    out = lax.map(one_block, jnp.arange(n_blk))
    return jnp.moveaxis(out, 0, 1).reshape(bsz, seq, -1)


def s5_layer(u, lam_re, lam_im, log_dt, b_re, b_im, c_re, c_im, d_skip):
    bsz, seq, _ = u.shape
    u32 = u.astype(jnp.float32).reshape(bsz, seq, S5_GROUPS, S5_GROUP)
    lam = lax.complex(jnp.minimum(lam_re.astype(jnp.float32), -1e-4), lam_im.astype(jnp.float32))
    dt = jnp.exp(log_dt.astype(jnp.float32))[:, None]
    lam_bar = jnp.exp(lam * dt)
    b = lax.complex(b_re.astype(jnp.float32), b_im.astype(jnp.float32))
    b_bar = ((lam_bar - 1.0) / lam)[:, :, None] * b
    bu = jnp.einsum('gpc,bsgc->bsgp', b_bar, u32.astype(jnp.complex64))
    a = jnp.broadcast_to(lam_bar, bu.shape)

    def combine(e_i, e_j):
        a_i, s_i = e_i
        a_j, s_j = e_j
        return a_j * a_i, a_j * s_i + s_j

    _, states = lax.associative_scan(combine, (a, bu), axis=1)
    c = lax.complex(c_re.astype(jnp.float32), c_im.astype(jnp.float32))
    y = jnp.einsum('gcp,bsgp->bsgc', c, states).real + d_skip.astype(jnp.float32) * u32
    return y.reshape(bsz, seq, S5_WIDTH).astype(u.dtype)


def dilated_group(q, k, v, window, dilation):
    bsz, seq, nh, hd = q.shape
    span = window // dilation
    sub = seq // dilation
    n_blk = -(-sub // span)
    pad = n_blk * span - sub

    def to_blocks(t):
        t = t.reshape(bsz, sub, dilation, nh, hd).transpose(0, 2, 1, 3, 4)
        t = jnp.pad(t, ((0, 0), (0, 0), (0, pad), (0, 0), (0, 0)))
        return t.reshape(bsz, dilation, n_blk, span, nh, hd)

    def with_prev(t):
        prev = jnp.pad(t, ((0, 0), (0, 0), (1, 0), (0, 0), (0, 0), (0, 0)))[:, :, :-1]
        return jnp.concatenate([prev, t], axis=3)

    qb = to_blocks(q)
    kw = with_prev(to_blocks(k))
    vw = with_prev(to_blocks(v))
    s = jnp.einsum('brnqhd,brnkhd->brnhqk', qb, kw).astype(jnp.float32) * (hd ** -0.5)
    qi = jnp.arange(span)[:, None]
    kj = jnp.arange(2 * span)[None, :]
    band = (kj >= qi) & (kj <= qi + span)
    has_prev = (jnp.arange(n_blk)[:, None, None] > 0) | (kj[None] >= span)
    mask = band[None] & has_prev
    s = jnp.where(mask[None, None, :, None], s, -jnp.inf)
    m = jnp.max(s, axis=-1, keepdims=True)
    p = jnp.exp(s - m)
    den = jnp.sum(p, axis=-1)
    o = jnp.einsum('brnhqk,brnkhd->brnqhd', p, vw.astype(jnp.float32))
    o = o / jnp.swapaxes(den, -1, -2)[..., None]
    lse = m[..., 0] + jnp.log(den)
    o = o.reshape(bsz, dilation, n_blk * span, nh, hd)[:, :, :sub]
    o = o.transpose(0, 2, 1, 3, 4).reshape(bsz, seq, nh, hd)
    lse = jnp.swapaxes(lse, -1, -2).reshape(bsz, dilation, n_blk * span, nh)[:, :, :sub]
    lse = lse.transpose(0, 2, 1, 3).reshape(bsz, seq, nh)
    return o, lse


def dilated_mixture(q, k, v):
    bsz, seq = q.shape[0], q.shape[1]
    outs, lses = [], []
    for g, (window, dilation) in enumerate(DIL_PAIRS):
        hs = slice(g * C_HEADS_PER_GROUP, (g + 1) * C_HEADS_PER_GROUP)
        o, l = dilated_group(q[:, :, hs], k[:, :, hs], v[:, :, hs], window, dilation)
        outs.append(o)
        lses.append(l)
    o = jnp.stack(outs, axis=2)
    wts = jax.nn.softmax(jnp.stack(lses, axis=2), axis=2)
    out = jnp.sum(wts[..., None] * o, axis=2)
    return out.reshape(bsz, seq, C_OUT).astype(q.dtype)


def setup_inputs(seed: int = 0) -> dict:
    key = jax.random.key(seed)
    ks = iter(jax.random.split(key, 32))
    f32 = jnp.float32
    L = DEPTH

    def nrm(shape, scale):
        return jax.random.normal(next(ks), shape, f32) * scale

    def gain(shape):
        return 1.0 + 0.02 * jax.random.normal(next(ks), shape, f32)

    n = jnp.arange(S5_STATE, dtype=f32)
    return {
        "x": jax.random.normal(next(ks), (BATCH, SEQ, D_MODEL), f32),
        "ffn1_norm": gain((L, D_MODEL)),
        "ffn1_gate": nrm((L, D_MODEL, D_FF), D_MODEL ** -0.5),
        "ffn1_up": nrm((L, D_MODEL, D_FF), D_MODEL ** -0.5),
        "ffn1_down": nrm((L, D_FF, D_MODEL), D_FF ** -0.5),
        "mix_norm": gain((L, D_MODEL)),
        "w_in": nrm((L, D_MODEL, D_IN), D_MODEL ** -0.5),
        "a_q_norm": gain((L, HEAD_DIM)),
        "a_k_norm": gain((L, HEAD_DIM)),
        "s5_lam_re": -0.5 + nrm((L, S5_GROUPS, S5_STATE), 0.01),
        "s5_lam_im": math.pi * n + nrm((L, S5_GROUPS, S5_STATE), 0.01),
        "s5_log_dt": jax.random.uniform(next(ks), (L, S5_GROUPS), f32,
                                        minval=math.log(S5_DT_MIN), maxval=math.log(S5_DT_MAX)),
        "s5_b_re": nrm((L, S5_GROUPS, S5_STATE, S5_GROUP), (2 * S5_GROUP) ** -0.5),
        "s5_b_im": nrm((L, S5_GROUPS, S5_STATE, S5_GROUP), (2 * S5_GROUP) ** -0.5),
        "s5_c_re": nrm((L, S5_GROUPS, S5_GROUP, S5_STATE), (2 * S5_STATE) ** -0.5),
        "s5_c_im": nrm((L, S5_GROUPS, S5_GROUP, S5_STATE), (2 * S5_STATE) ** -0.5),
        "s5_d": nrm((L, S5_GROUPS, S5_GROUP), 1.0),
        "s5_glu_a": nrm((L, S5_WIDTH, S5_WIDTH), S5_WIDTH ** -0.5),
        "s5_glu_b": nrm((L, S5_WIDTH, S5_WIDTH), S5_WIDTH ** -0.5),
        "c_q_norm": gain((L, HEAD_DIM)),
        "c_k_norm": gain((L, HEAD_DIM)),
        "w_branch_a": nrm((L, A_WIDTH, D_MODEL), A_WIDTH ** -0.5),
        "w_branch_b": nrm((L, S5_WIDTH, D_MODEL), S5_WIDTH ** -0.5),
        "w_branch_c": nrm((L, C_OUT, D_MODEL), C_OUT ** -0.5),
        "w_out": nrm((L, D_MODEL, D_MODEL), (N_BRANCHES * D_MODEL) ** -0.5),
        "ffn2_norm": gain((L, D_MODEL)),
        "ffn2_gate": nrm((L, D_MODEL, D_FF), D_MODEL ** -0.5),
        "ffn2_up": nrm((L, D_MODEL, D_FF), D_MODEL ** -0.5),
        "ffn2_down": nrm((L, D_FF, D_MODEL), D_FF ** -0.5),
    }


def reference(x, ffn1_norm, ffn1_gate, ffn1_up, ffn1_down, mix_norm, w_in, a_q_norm, a_k_norm,
              s5_lam_re, s5_lam_im, s5_log_dt, s5_b_re, s5_b_im, s5_c_re, s5_c_im, s5_d,
              s5_glu_a, s5_glu_b, c_q_norm, c_k_norm, w_branch_a, w_branch_b, w_branch_c, w_out,
              ffn2_norm, ffn2_gate, ffn2_up, ffn2_down):
    bsz, seq, _ = x.shape
    for l in range(DEPTH):
        x = x + 0.5 * swiglu(rmsnorm(x, ffn1_norm[l]), ffn1_gate[l], ffn1_up[l], ffn1_down[l])

        h = rmsnorm(x, mix_norm[l])
        aq, ak, av, iq, ik, iw, su, cq, ck, cv, gates = split_columns(h @ w_in[l])

        aq = rmsnorm(aq.reshape(bsz, seq, A_HEADS, HEAD_DIM), a_q_norm[l])
        ak = rmsnorm(ak, a_k_norm[l])
        y_a = dsa_attention(aq, ak, av, iq.reshape(bsz, seq, IDX_HEADS, IDX_DIM), ik, iw)

        y_b = jax.nn.gelu(s5_layer(su, s5_lam_re[l], s5_lam_im[l], s5_log_dt[l], s5_b_re[l], s5_b_im[l],
                                   s5_c_re[l], s5_c_im[l], s5_d[l]))
        y_b = (y_b @ s5_glu_a[l]) * jax.nn.sigmoid(y_b @ s5_glu_b[l])

        cq = rmsnorm(cq.reshape(bsz, seq, C_HEADS, HEAD_DIM), c_q_norm[l])
        ck = rmsnorm(ck.reshape(bsz, seq, C_HEADS, HEAD_DIM), c_k_norm[l])
        y_c = dilated_mixture(cq, ck, cv.reshape(bsz, seq, C_HEADS, HEAD_DIM))

        g = jax.nn.sigmoid(gates.astype(jnp.float32)).astype(x.dtype)
        g_a = g[..., :D_MODEL]
        g_b = g[..., D_MODEL:2 * D_MODEL]
        g_c = g[..., 2 * D_MODEL:]
        merged = g_a * (y_a @ w_branch_a[l]) + g_b * (y_b @ w_branch_b[l]) + g_c * (y_c @ w_branch_c[l])
        x = x + merged @ w_out[l]

        x = x + 0.5 * swiglu(rmsnorm(x, ffn2_norm[l]), ffn2_gate[l], ffn2_up[l], ffn2_down[l])
    return x
```

```python
import numpy as np
import concourse.bass as bass
import concourse.mybir as mybir
from concourse.bass_utils import run_bass_kernel_spmd

F32 = mybir.dt.float32
BF16 = mybir.dt.bfloat16
AF = mybir.ActivationFunctionType
ALU = mybir.AluOpType
AX = mybir.AxisListType

D = 1024
FF = 2816
DIN = 6852
EPS = 1e-6
MAGIC = 12582912.0
TWO_PI = 6.283185307179586


class Res:
    __slots__ = ("name", "writers", "readers", "sem", "cnt")

    _all = []

    def __init__(self, name):
        self.name = name
        self.writers = []
        self.readers = []
        self.sem = None
        self.cnt = 0
        Res._all.append(self)


class Tile:
    def __init__(self, t, res):
        self.t = t
        self.res = res

    def __getitem__(self, k):
        return self.t[k]


class Prog:
    ENGS = ("pe", "act", "dve", "pool", "sp")

    def __init__(self, nc):
        self.nc = nc
        self.ops = {e: [] for e in self.ENGS}
        self.cnt = {e: 0 for e in self.ENGS}
        self.esem = {e: nc.alloc_semaphore("s_" + e) for e in ("pe", "act", "dve", "pool")}
        self.waited = {e: {} for e in self.ENGS}
        self.free_sems = {True: [(nc.alloc_semaphore("dsw%d" % i), 0) for i in range(6)],
                          False: [(nc.alloc_semaphore("dhw%d" % i), 0) for i in range(80)]}
        self.live = []
        self.nsem = 4
        self.nops = 0

    def sb(self, name, shape, dtype):
        self.uid = getattr(self, "uid", 0) + 1
        name = "%s_u%d" % (name, self.uid)
        return Tile(self.nc.alloc_sbuf_tensor(name, list(shape), dtype), Res(name))

    def ps(self, name, shape, dtype=F32):
        return Tile(self.nc.alloc_psum_tensor(name, list(shape), dtype), Res(name))

    def op(self, eng, fn, reads=(), writes=(), dma=None, join=False):
        reads = [r.res if isinstance(r, Tile) else r for r in reads]
        writes = [w.res if isinstance(w, Tile) else w for w in writes]
        toks = []
        for r in reads:
            for t in r.writers:
                toks.append((t, "raw"))
        for w in writes:
            if not join:
                for t in w.writers:
                    toks.append((t, "waw"))
            for t in w.readers:
                toks.append((t, "war"))
        waits = {}
        for (sem, val, teng), kind in toks:
            if teng == eng and dma is None and eng == "pe":
                continue
            k = id(sem)
            if self.waited[eng].get(k, 0) >= val:
                continue
            if k not in waits or waits[k][1] < val:
                waits[k] = (sem, val)
        for k, (sem, val) in waits.items():
            self.waited[eng][k] = val
        if dma is None:
            self.cnt[eng] += 1
            tok = (self.esem[eng], self.cnt[eng], eng)
            inc = (self.esem[eng], 1)
        else:
            d = dma.res if isinstance(dma, Tile) else dma
            if d.sem is None:
                sw = (eng == "pool")
                d.sem, d.cnt = self.free_sems[sw].pop()
                self.nsem = max(self.nsem, 86 - len(self.free_sems[True]) - len(self.free_sems[False]))
                self.live.append((d, sw))
            d.cnt += 16
            tok = (d.sem, d.cnt, "dma")
            inc = (d.sem, 16)
        self.ops[eng].append((list(waits.values()), fn, inc))
        self.nops += 1
        for r in reads:
            r.readers.append(tok)
        for w in writes:
            if join:
                w.writers.append(tok)
            else:
                w.writers = [tok]
                w.readers = []
        return tok

    def flush(self):
        nc = self.nc
        finals = [(self.esem[e], self.cnt[e]) for e in ("pe", "act", "dve", "pool")]
        finals += [(d.sem, d.cnt) for d, _ in self.live]
        ops = self.ops

        def emit(eng, e):
            for waits, fn, inc in ops[eng]:
                for sem, val in waits:
                    e.wait_ge(sem, val)
                fn(e).then_inc(inc[0], inc[1])
            for sem, val in finals:
                if val > 0:
                    e.wait_ge(sem, val)

        with nc.Block() as block:
            @block.tensor
            def _(e):
                emit("pe", e)

            @block.scalar
            def _(e):
                emit("act", e)

            @block.vector
            def _(e):
                emit("dve", e)

            @block.gpsimd
            def _(e):
                emit("pool", e)

            @block.sync
            def _(e):
                emit("sp", e)

        self.ops = {e: [] for e in self.ENGS}
        for d, sw in self.live:
            self.free_sems[sw].append((d.sem, d.cnt))
            d.sem = None
        self.live = []
        for e in self.ENGS:
            for sem, val in finals:
                self.waited[e][id(sem)] = val


def _after_reset(p):
    for e in p.ENGS:
        p.cnt[e] = 0
        p.waited[e] = {}
    for sw in (True, False):
        p.free_sems[sw] = [(sem, 0) for sem, _ in p.free_sems[sw]]
    for r in Res._all:
        r.writers = []
        r.readers = []
        r.sem = None
        r.cnt = 0


class Cfg:
    def __init__(self, seq=4096, depth=4, ncores=4, stage=99, dbg=False, sub=9):
        self.sub = sub
        self.pset = set(range(1, 10))
        self.variant = ''
        self.SEQ = seq
        self.DEPTH = depth
        self.NCORES = ncores
        self.stage = stage
        self.dbg = dbg


WEIGHT_SHAPES = {
    "ffn1_norm": [D], "ffn1_gate": [D, FF], "ffn1_up": [D, FF], "ffn1_down": [FF, D],
    "mix_norm": [D], "w_in": [D, DIN], "a_q_norm": [64], "a_k_norm": [64],
    "s5_lam_re": [32, 64], "s5_lam_im": [32, 64], "s5_log_dt": [32],
    "s5_b_re": [32, 64, 16], "s5_b_im": [32, 64, 16], "s5_c_re": [32, 16, 64], "s5_c_im": [32, 16, 64],
    "s5_d": [32, 16], "s5_glu_a": [512, 512], "s5_glu_b": [512, 512],
    "c_q_norm": [64], "c_k_norm": [64],
    "w_branch_a": [512, D], "w_branch_b": [512, D], "w_branch_c": [256, D], "w_out": [D, D],
    "ffn2_norm": [D], "ffn2_gate": [D, FF], "ffn2_up": [D, FF], "ffn2_down": [FF, D],
}
BIG = ["ffn1_gate", "ffn1_up", "ffn1_down", "w_in", "s5_glu_a", "s5_glu_b",
       "w_branch_a", "w_branch_b", "w_branch_c", "w_out", "ffn2_gate", "ffn2_up", "ffn2_down"]
DIL = (1, 4, 16)


def build(cfg):
    Res._all = []
    nc = bass.Bass("TRN2", target_bir_lowering=False)
    NT = cfg.SEQ
    L = cfg.DEPTH
    T = 512
    NTT = NT // T
    NQB = NT // 128
    p = Prog(nc)

    X = nc.dram_tensor("x", [NT, D], F32, kind="ExternalInput").ap()
    W = {k: nc.dram_tensor(k, [L] + s, F32, kind="ExternalInput").ap() for k, s in WEIGHT_SHAPES.items()}
    OUT = nc.dram_tensor("out", [NT, D], F32, kind="ExternalOutput").ap()
    dbg_kind = "ExternalOutput" if cfg.dbg else "Internal"

    def scr(name, shape, dt, dbg=True):
        return nc.dram_tensor(name, list(shape), dt, kind=dbg_kind if dbg else "Internal").ap()

    WB = [{k: scr("wb%d_%s" % (l, k), WEIGHT_SHAPES[k], BF16, dbg=False) for k in BIG} for l in range(L)]
    WBres = [{k: Res("wb%d_%s" % (l, k)) for k in BIG} for l in range(L)]
    S_xT = scr("s_xT", [128, 8, NT], F32)
    S_aq = scr("s_aq", [128, 4, NT], BF16)
    S_kdup = scr("s_kdup", [128, NT], BF16)
    S_ik = scr("s_ik", [64, NT], BF16)
    S_su = scr("s_su", [128, 4, NT], BF16)
    S_tm1 = scr("s_tm1", [128, NT // 128, 512], F32)
    S_av = scr("s_av", [128, NT // 128, 64], BF16)
    S_cv = [scr("s_cv%d" % g, [128, 2, DIL[g], NT // DIL[g]], BF16) for g in range(3)]
    S_cq = [scr("s_cq%d" % g, [128, 2, DIL[g], NT // DIL[g]], BF16) for g in range(3)]
    S_ck = [scr("s_ck%d" % g, [128, 2, DIL[g], NT // DIL[g]], BF16) for g in range(3)]
    S_g = scr("s_g", [128, 24, NT], BF16)
    S_ya = scr("s_ya", [64, 8, NT], BF16)
    S_ys5 = scr("s_ys5", [128, 4, NT], F32)
    S_yc = scr("s_yc", [64, 4, NT], BF16)
    if cfg.dbg:
        D_vp = scr("dbg_vp", [128, NT // 128, 65], BF16)
        D_sc = scr("dbg_sc", [NT // 128, 128, NT], F32)
        D_lo = scr("dbg_lo", [NT // 128, 128, 2], F32)
    R = {n: Res(n) for n in ["xT", "aq", "kdup", "ik", "su", "tm1", "av", "cv", "cq", "ck", "g", "ya", "ys5", "yc", "out"]}

    ident32 = p.sb("ident32", [128, 128], F32)
    identb = p.sb("identb", [128, 128], BF16)
    ones32 = p.sb("ones32", [128, 128], F32)
    blk = p.sb("blk", [128, 128], BF16)
    epsc = p.sb("epsc", [128, 1], F32)
    gvec = p.sb("gvec", [128, 3, L, 8], F32)
    gqk = p.sb("gqk", [128, 4, L], F32)
    PS = [p.ps("ps%d" % i, [128, 512], F32) for i in range(8)]

    def pool(fn, reads=(), writes=()):
        return p.op("pool", fn, reads=reads, writes=writes)

    def act(fn, reads=(), writes=()):
        return p.op("act", fn, reads=reads, writes=writes)

    def dve(fn, reads=(), writes=()):
        return p.op("dve", fn, reads=reads, writes=writes)

    def pe(fn, reads=(), writes=()):
        return p.op("pe", fn, reads=reads, writes=writes)

    def mm(out_ap, lhsT, rhs, start, stop, reads, writes):
        return p.op("pe", lambda e: e.matmul(out_ap, lhsT=lhsT, rhs=rhs, start=start, stop=stop), reads=reads, writes=writes)

    def load(out_ap, in_ap, tile, reads=(), q="sp", slow=False, join=False):
        return p.op(q, lambda e: e.dma_start(out=out_ap, in_=in_ap, allow_slow_non_contiguous=slow),
                    reads=reads, writes=[tile], dma=tile, join=join)

    def store(out_ap, in_ap, tile, wres, q="act", join=True):
        return p.op(q, lambda e: e.dma_start(out=out_ap, in_=in_ap), reads=[tile], writes=[wres], dma=tile, join=join)

    pool(lambda e: e.memset(ident32[:], 1.0), writes=[ident32])
    pool(lambda e: e.affine_select(out=ident32[:], in_=ident32[:], pattern=[[-1, 128]], compare_op=ALU.is_equal,
                                   fill=0.0, base=0, channel_multiplier=1), reads=[ident32], writes=[ident32])
    pool(lambda e: e.tensor_copy(out=identb[:], in_=ident32[:]), reads=[ident32], writes=[identb])
    pool(lambda e: e.memset(ones32[:], 1.0), writes=[ones32])
    pool(lambda e: e.memset(blk[:], 0.0), writes=[blk])
    pool(lambda e: e.memset(blk[0:64, 0:64], 1.0), writes=[blk])
    pool(lambda e: e.memset(blk[64:128, 64:128], 1.0), writes=[blk])
    pool(lambda e: e.memset(epsc[:], EPS), writes=[epsc])
    for wi, nm in enumerate(["ffn1_norm", "mix_norm", "ffn2_norm"]):
        load(gvec[:, wi, :, :], W[nm].rearrange("l (c q) -> q l c", q=128), gvec, slow=True, join=True)
    for wi, nm in enumerate(["a_q_norm", "a_k_norm", "c_q_norm", "c_k_norm"]):
        for hf in range(2):
            load(gqk[hf * 64:(hf + 1) * 64, wi, :], W[nm].rearrange("l d -> d l"), gqk, slow=True, join=True)

    castchain = Res("castchain")
    for l in range(L):
        castres = Res("cast%d" % l)
        tok = None
        for k in BIG:
            src = W[k][l]
            K, N = WEIGHT_SHAPES[k]
            for n0 in range(0, N, 2048):
                n1 = min(N, n0 + 2048)
                tok = p.op("pool", lambda e, s=src[:, n0:n1], d=WB[l][k][:, n0:n1]: e.dma_start(out=d, in_=s),
                           reads=[castchain], writes=[WBres[l][k], castchain], dma=castres)
        for k in BIG:
            WBres[l][k].writers = [tok]

    xt = hb = ab = SQ = SQB = rs = RSD = SG = WS = STG = xin = xtr = hbr = abr = None
    NWS = 4
    NST = 6
    cnt = {"ws": 0, "st": 0, "pa": 0, "sq": 0, "sqb": 0, "rsd": 0, "sg": 0}

    def nxt(key, lst):
        t = lst[cnt[key] % len(lst)]
        cnt[key] += 1
        return t

    def psA():
        return nxt("pa", PS[0:4])

    def wload(src, wres, kc, n, kp=128):
        slot = nxt("ws", WS)
        view = slot.t[0:kp, 0:kc * n].rearrange("q (k n) -> q k n", k=kc)
        p.op("sp", lambda e: e.dma_start(out=view, in_=src.rearrange("(k q) n -> q k n", q=kp)),
             reads=[wres], writes=[slot], dma=slot)
        return slot, view

    def stg_bf(shape_free):
        st = nxt("st", STG)
        n = 1
        for s in shape_free:
            n *= s
        v = st.t[:].bitcast(BF16)[:, 0:n]
        return st, v

    def rmsnorm_fm(which, l):
        ss = PS[4]
        for c in range(8):
            sq = nxt("sq", SQ)
            act(lambda e, sq=sq, c=c: e.activation(out=sq[:], in_=xt[:, c, :], func=AF.Square), reads=[xtr[c]], writes=[sq])
            mm(ss[:], ones32[:], sq[:], c == 0, c == 7, [ones32, sq], [ss])
        act(lambda e: e.activation(out=rs[:], in_=ss[:], func=AF.Sqrt, bias=epsc[:, 0:1], scale=1.0 / D), reads=[ss, epsc], writes=[rs])
        dve(lambda e: e.reciprocal(out=rs[:], in_=rs[:]), reads=[rs], writes=[rs])
        for c in range(8):
            dve(lambda e, c=c: e.scalar_tensor_tensor(out=hb[:, c, :], in0=xt[:, c, :], scalar=gvec[:, which, l, c:c + 1],
                                                      in1=rs[:], op0=ALU.mult, op1=ALU.mult), reads=[xtr[c], gvec, rs], writes=[hbr[c]])

    def ffn(l, pre):
        Wg, Wu, Wd = WB[l][pre + "_gate"], WB[l][pre + "_up"], WB[l][pre + "_down"]
        rg, ru, rd = WBres[l][pre + "_gate"], WBres[l][pre + "_up"], WBres[l][pre + "_down"]
        for n0 in range(0, FF, 512):
            n1 = min(FF, n0 + 512)
            sg_, wg = wload(Wg[:, n0:n1], rg, 8, n1 - n0)
            su_, wu = wload(Wu[:, n0:n1], ru, 8, n1 - n0)
            for c0 in range(0, n1 - n0, 128):
                f = (n0 + c0) // 128
                pg = psA()
                pu = psA()
                for k in range(8):
                    mm(pg[:], wg[:, k, c0:c0 + 128], hb[:, k, :], k == 0, k == 7, [sg_, hbr[k]], [pg])
                for k in range(8):
                    mm(pu[:], wu[:, k, c0:c0 + 128], hb[:, k, :], k == 0, k == 7, [su_, hbr[k]], [pu])
                sg = nxt("sg", SG)
                act(lambda e, sg=sg, pg=pg: e.activation(out=sg[:], in_=pg[:], func=AF.Silu), reads=[pg], writes=[sg])
                dve(lambda e, sg=sg, pu=pu, f=f: e.tensor_tensor(out=ab[:, f, :], in0=pu[:], in1=sg[:], op=ALU.mult),
                    reads=[pu, sg], writes=[abr[f]])
        for half in range(2):
            accs = PS[4:8]
            for fg in range(0, 22, 4):
                nf = min(4, 22 - fg)
                sl, wv = wload(Wd[fg * 128:(fg + nf) * 128, half * 512:(half + 1) * 512], rd, nf, 512)
                for j in range(nf):
                    f = fg + j
                    for dc in range(4):
                        mm(accs[dc][:], wv[:, j, dc * 128:(dc + 1) * 128], ab[:, f, :], f == 0, f == 21, [sl, abr[f]], [accs[dc]])
            for dc in range(4):
                c = half * 4 + dc
                dve(lambda e, c=c, a=accs[dc]: e.scalar_tensor_tensor(out=xt[:, c, :], in0=a[:], scalar=0.5, in1=xt[:, c, :],
                                                                       op0=ALU.mult, op1=ALU.add), reads=[accs[dc], xtr[c]], writes=[xtr[c]])

    def qknorm_epi(ps, cw, gidx, l, out_ap, view=None):
        sqb = nxt("sqb", SQB)
        act(lambda e: e.activation(out=sqb[0:cw, :], in_=ps[0:cw, :], func=AF.Square), reads=[ps], writes=[sqb])
        ss = PS[5] if cnt["rsd"] % 2 == 0 else PS[6]
        mm(ss[0:cw, :], blk[0:cw, 0:cw], sqb[0:cw, :], True, True, [blk, sqb], [ss])
        rsd = nxt("rsd", RSD)
        act(lambda e: e.activation(out=rsd[0:cw, :], in_=ss[0:cw, :], func=AF.Sqrt, bias=epsc[0:cw, 0:1], scale=1.0 / 64),
            reads=[ss, epsc], writes=[rsd])
        dve(lambda e: e.reciprocal(out=rsd[0:cw, :], in_=rsd[0:cw, :]), reads=[rsd], writes=[rsd])
        in0 = ps[0:cw, :]
        in1 = rsd[0:cw, :]
        if view is not None:
            in0 = in0.rearrange(view[0], **view[1])
            in1 = in1.rearrange(view[0], **view[1])
        return (lambda st: dve(lambda e: e.scalar_tensor_tensor(out=out_ap, in0=in0, scalar=gqk[0:cw, gidx, l:l + 1], in1=in1,
                                                                op0=ALU.mult, op1=ALU.mult), reads=[ps, rsd, gqk], writes=[st]))

    def proj(l, t0):
        Win = WB[l]["w_in"]
        rw = WBres[l]["w_in"]

        def fm_group(n0, n1, epi):
            sl, wv = wload(Win[:, n0:n1], rw, 8, n1 - n0)
            for c0 in range(0, n1 - n0, 128):
                cw = min(128, n1 - n0 - c0)
                ps = psA()
                for k in range(8):
                    mm(ps[0:cw, :], wv[:, k, c0:c0 + cw], hb[:, k, :], k == 0, k == 7, [sl, hbr[k]], [ps])
                epi(n0 + c0, cw, ps)

        def epi_aq(col, cw, ps):
            hp = col // 128
            st, v = stg_bf([T])
            qknorm_epi(ps, 128, 0, l, v)(st)
            store(S_aq[:, hp, t0:t0 + T], v, st, R["aq"])
        if 1 in cfg.pset:
            fm_group(0, 512, epi_aq)

        def epi_ak(col, cw, ps):
            st, v = stg_bf([T])
            qknorm_epi(ps, 64, 1, l, v[0:64, :])(st)
            store(S_kdup[0:64, t0:t0 + T], v[0:64, :], st, R["kdup"])
            store(S_kdup[64:128, t0:t0 + T], v[0:64, :], st, R["kdup"])
        if 2 in cfg.pset:
            fm_group(512, 576, epi_ak)

        def epi_ik(col, cw, ps):
            st, v = stg_bf([T])
            act(lambda e: e.activation(out=v[0:64, :], in_=ps[0:64, :], func=AF.Copy), reads=[ps], writes=[st])
            store(S_ik[:, t0:t0 + T], v[0:64, :], st, R["ik"])
        if 3 in cfg.pset:
            fm_group(896, 960, epi_ik)

        def epi_su(col, cw, ps):
            j = (col - 964) // 128
            st, v = stg_bf([T])
            act(lambda e: e.activation(out=v, in_=ps[:], func=AF.Copy), reads=[ps], writes=[st])
            store(S_su[:, j, t0:t0 + T], v, st, R["su"])
        if 4 in cfg.pset:
            fm_group(964, 1476, epi_su)

        def mk_epi_c(base, gidx, S, rname):
            def epi(col, cw, ps):
                ch = (col - base) // 128
                g = ch // 2
                hp = ch % 2
                d = DIL[g]
                st, v = stg_bf([T])
                if d == 1:
                    qknorm_epi(ps, 128, gidx, l, v)(st)
                    store(S[g][:, hp, 0, t0:t0 + T], v, st, R[rname])
                else:
                    v3 = v.rearrange("q (r s) -> q r s", r=d)
                    qknorm_epi(ps, 128, gidx, l, v3, view=("q (s r) -> q r s", {"r": d}))(st)
                    store(S[g][:, hp, :, t0 // d:(t0 + T) // d], v3, st, R[rname])
            return epi
        if 5 in cfg.pset:
            fm_group(1476, 1988, mk_epi_c(1476, 2, S_cq, "cq"))
            fm_group(1988, 2244, mk_epi_c(1476, 2, S_cq, "cq"))
            fm_group(2244, 2756, mk_epi_c(2244, 3, S_ck, "ck"))
            fm_group(2756, 3012, mk_epi_c(2244, 3, S_ck, "ck"))

        def epi_g(col, cw, ps):
            j = (col - 3780) // 128
            st, v = stg_bf([T])
            act(lambda e: e.activation(out=v, in_=ps[:], func=AF.Sigmoid), reads=[ps], writes=[st])
            store(S_g[:, j, t0:t0 + T], v, st, R["g"])
        for n0 in range(3780, DIN, 512):
            if 6 in cfg.pset:
                fm_group(n0, n0 + 512, epi_g)

        def tm_group(n0, n1, epi):
            sl, wv = wload(Win[:, n0:n1], rw, 8, n1 - n0)
            for sub in range(4):
                ps = psA()
                for k in range(8):
                    mm(ps[:, 0:n1 - n0], hb[:, k, sub * 128:(sub + 1) * 128], wv[:, k, :], k == 0, k == 7, [sl, hbr[k]], [ps])
                epi(sub, ps)

        def epi_tm1(sub, ps):
            st = nxt("st", STG)
            act(lambda e: e.activation(out=st[:, 0:512], in_=ps[:, 0:512], func=AF.Copy), reads=[ps], writes=[st])
            store(S_tm1[:, t0 // 128 + sub, :], st[:, 0:512], st, R["tm1"])
        if 7 in cfg.pset:
            tm_group(576, 1088, epi_tm1)

        def epi_cv(col, cw, ps):
            ch = (col - 3012) // 128
            g = ch // 2
            hp = ch % 2
            d = DIL[g]
            st, v = stg_bf([T])
            if d == 1:
                act(lambda e: e.activation(out=v, in_=ps[:], func=AF.Copy), reads=[ps], writes=[st])
                store(S_cv[g][:, hp, 0, t0:t0 + T], v, st, R["cv"])
            else:
                v3 = v.rearrange("q (r s) -> q r s", r=d)
                act(lambda e: e.activation(out=v3, in_=ps[:].rearrange("q (s r) -> q r s", r=d), func=AF.Copy), reads=[ps], writes=[st])
                store(S_cv[g][:, hp, :, t0 // d:(t0 + T) // d], v3, st, R["cv"])
        if 8 in cfg.pset:
            fm_group(3012, 3524, epi_cv)
            fm_group(3524, 3780, epi_cv)

    def load_x_first(t0):
        load(xin[:], X[t0:t0 + T, :].rearrange("(s q) f -> q s f", q=128), xin)
        for c in range(8):
            ps = psA()
            for s in range(4):
                pe(lambda e, ps=ps, s=s, c=c: e.transpose(out=ps[:, s * 128:(s + 1) * 128], in_=xin[:, s, c * 128:(c + 1) * 128],
                                                          identity=ident32[:]), reads=[xin, ident32], writes=[ps])
            act(lambda e, ps=ps, c=c: e.activation(out=xt[:, c, :], in_=ps[:], func=AF.Copy), reads=[ps], writes=[xtr[c]])

    def store_x_last(t0):
        for s in range(4):
            for c2 in range(2):
                ps = psA()
                for c in range(4):
                    cc = c2 * 4 + c
                    pe(lambda e, ps=ps, s=s, c=c, cc=cc: e.transpose(out=ps[:, c * 128:(c + 1) * 128], in_=xt[:, cc, s * 128:(s + 1) * 128],
                                                                     identity=ident32[:]), reads=[xtr[cc], ident32], writes=[ps])
                act(lambda e, ps=ps, s=s, c2=c2: e.activation(out=xin[:, s, c2 * 512:(c2 + 1) * 512], in_=ps[:], func=AF.Copy),
                    reads=[ps], writes=[xin])
        return store(OUT[t0:t0 + T, :].rearrange("(s q) f -> q s f", q=128), xin[:], xin, R["out"])


    NIT = 22

    def phase_dsa(l):
        with nc.reset_on_exit():
            kidx = p.sb("kidx", [64, NT], BF16)
            kdup = p.sb("kdup", [128, NT], BF16)
            vp = p.sb("vp", [128, NQB, 65], BF16)
            sc = p.sb("sc", [128, NT], F32)
            scr_ = [Res("sc%d" % i) for i in range((NT + 511) // 512)]
            msk = p.sb("msk", [128, NT], BF16)
            junk = msk
            zt = p.sb("zt", [128, NT], BF16)
            cum = p.sb("cum", [128, NT], mybir.dt.float16)
            SM = {k: p.sb("sm_" + k, [128, 1], F32) for k in ["cpos", "t1", "c1", "c2", "tie", "nd0", "need"]}
            tri = p.sb("tri", [128, 128], F32)
            sel = p.sb("sel", [65, 64], F32)
            nb = p.sb("nb", [128, 1], F32)
            TMQ = [p.sb("tmq%d" % i, [128, 512], F32) for i in range(2)]
            QF = [p.sb("qf%d" % i, [128, 4, 128], BF16) for i in range(2)]
            WAB = [p.sb("wab%d" % i, [128, 4], F32) for i in range(2)]
            QLN = [p.sb("qln%d" % i, [128, 64], F32) for i in range(2)]
            QS = [p.sb("qs%d" % i, [128, 5, 64], BF16) for i in range(2)]
            QST = [p.sb("qst%d" % i, [64, 5, 128], BF16) for i in range(2)]
            LO = [p.sb("lo%d" % i, [128, 1], F32) for i in range(2)]
            HI = [p.sb("hi%d" % i, [128, 1], F32) for i in range(2)]
            mid = p.sb("mid", [128, 1], F32)
            cn = p.sb("cn", [128, 1], F32)
            pge = p.sb("pge", [128, 1], mybir.dt.int32)
            plt = p.sb("plt", [128, 1], mybir.dt.int32)
            EE = [p.sb("ee%d" % i, [128, 1024], BF16) for i in range(2)]
            EM = [p.sb("em%d" % i, [128, 1024], BF16) for i in range(2)]
            osb = p.sb("osb", [65, 1024], F32)
            rec = p.sb("rec", [64, 1024], F32)
            YA = [p.sb("yast%d" % i, [64, 1024], BF16) for i in range(2)]
            XH = [p.sb("xh%d" % i, [128, 512], F32) for i in range(3)]

            load(kidx[:], S_ik, kidx, reads=[R["ik"]])
            load(kdup[:], S_kdup, kdup, reads=[R["kdup"]])
            pool(lambda e: e.memset(vp[:, :, 64:65], 1.0), writes=[vp])
            vf = p.sb("vf", [128, NQB, 64], F32)
            load(vf[:], S_tm1[:, :, 0:64], vf, reads=[R["tm1"]])
            act(lambda e: e.activation(out=vp[:, :, 0:64], in_=vf[:], func=AF.Copy), reads=[vf], writes=[vp])
            pool(lambda e: e.memset(tri[:], 0.0), writes=[tri])
            pool(lambda e: e.affine_select(out=tri[:], in_=tri[:], pattern=[[-1, 128]], compare_op=ALU.is_ge,
                                           fill=-1e30, base=0, channel_multiplier=1), reads=[tri], writes=[tri])
            pool(lambda e: e.memset(sel[:], 0.0), writes=[sel])
            pool(lambda e: e.memset(sel[64:65, :], 1.0), writes=[sel])
            pool(lambda e: e.memset(nb[:], -8.0), writes=[nb])

            pl = PS[0]
            PH = [PS[1], PS[2]]
            ST = [PS[3], PS[4]]
            ptr = PS[5]
            ptr_b = ptr.t[:].bitcast(BF16)
            OO = [PS[6], PS[7]]
            hcnt = [0]

            for qb in range(NQB):
                t0 = qb * 128
                N = t0 + 128
                b = qb % 2
                tmq, qf, wab, qln, qs, qst, lo, hi = TMQ[b], QF[b], WAB[b], QLN[b], QS[b], QST[b], LO[b], HI[b]
                load(tmq[:], S_tm1[:, qb, :], tmq, reads=[R["tm1"]])
                load(qf[:], S_aq[:, :, t0:t0 + 128], qf, reads=[R["aq"]])
                dve(lambda e, qs=qs, tmq=tmq: e.tensor_copy(out=qs[:, 0:4, :].rearrange("q h d -> q (h d)"), in_=tmq[:, 64:320]),
                    reads=[tmq], writes=[qs])
                for v in range(4):
                    pe(lambda e, v=v, qs=qs: e.transpose(out=ptr_b[0:64, v * 128:(v + 1) * 128], in_=qs[:, v, :], identity=identb[:]),
                       reads=[qs, identb], writes=[ptr])
                act(lambda e, qst=qst: e.activation(out=qst[:, 0:4, :].rearrange("q v t -> q (v t)"), in_=ptr_b[0:64, 0:512], func=AF.Copy),
                    reads=[ptr], writes=[qst])
                nk = (N + 511) // 512
                for kc in range(nk):
                    w = min(512, N - kc * 512)
                    ksl = kidx[:, kc * 512:kc * 512 + w]
                    scs = sc[:, kc * 512:kc * 512 + w]
                    for h in range(4):
                        ph = PH[hcnt[0] % 2]
                        xh = XH[hcnt[0] % 3]
                        hcnt[0] += 1
                        mm(ph[:, 0:w], qst[:, h, :], ksl, True, True, [qst, kidx], [ph])
                        if h == 0:
                            dve(lambda e, ph=ph, scs=scs, w=w, tmq=tmq: e.tensor_scalar(out=scs, in0=ph[:, 0:w], scalar1=0.0, scalar2=tmq[:, 384:385],
                                                                                        op0=ALU.max, op1=ALU.mult),
                                reads=[ph, tmq], writes=[scr_[kc]])
                        else:
                            dve(lambda e, ph=ph, xh=xh, w=w, tmq=tmq, h=h: e.tensor_scalar(out=xh[:, 0:w], in0=ph[:, 0:w], scalar1=0.0,
                                                                                           scalar2=tmq[:, 384 + h:385 + h], op0=ALU.max, op1=ALU.mult),
                                reads=[ph, tmq], writes=[xh])
                            pool(lambda e, xh=xh, scs=scs, w=w: e.tensor_tensor(out=scs, in0=xh[:, 0:w], in1=scs, op=ALU.add),
                                 reads=[xh, scr_[kc]], writes=[scr_[kc]])
                allsc = scr_[0:nk]
                if qb >= 2:
                    dve(lambda e, N=N, hi=hi: e.tensor_reduce(out=hi[:], in_=sc[:, 0:N], axis=AX.X, op=ALU.max), reads=allsc, writes=[hi])
                    dve(lambda e, N=N, lo=lo: e.tensor_reduce(out=lo[:], in_=sc[:, 0:N], axis=AX.X, op=ALU.min), reads=allsc, writes=[lo])
                else:
                    pool(lambda e, lo=lo: e.memset(lo[:], -1e29), writes=[lo])
                dve(lambda e, t0=t0: e.tensor_tensor(out=sc[:, t0:t0 + 128], in0=sc[:, t0:t0 + 128], in1=tri[:], op=ALU.add),
                    reads=[scr_[nk - 1], tri], writes=[scr_[nk - 1]])
                if qb >= 2:
                    pool(lambda e, N=N: e.tensor_single_scalar(out=zt[:, 0:N], in_=sc[:, 0:N], scalar=0.0, op=ALU.is_equal),
                         reads=allsc, writes=[zt])
                    dve(lambda e, N=N: e.tensor_tensor_scan(out=cum[:, 0:N], data0=zt[:, 0:N], data1=zt[:, 0:N], initial=0.0,
                                                            op0=ALU.add, op1=ALU.max), reads=[zt], writes=[cum])
                    dve(lambda e, N=N: e.tensor_scalar(out=junk[:, 0:N], in0=sc[:, 0:N], scalar1=0.0, scalar2=None,
                                                       op0=ALU.is_gt, op1=ALU.add, accum_out=SM["cpos"][:]),
                        reads=allsc, writes=[junk, SM["cpos"]])
                    dve(lambda e, N=N: e.tensor_tensor(out=SM["t1"][:], in0=SM["cpos"][:], in1=cum[:, N - 1:N], op=ALU.add),
                        reads=[SM["cpos"], cum], writes=[SM["t1"]])
                    dve(lambda e: e.tensor_single_scalar(out=SM["c1"][:], in_=SM["cpos"][:], scalar=255.5, op=ALU.is_lt),
                        reads=[SM["cpos"]], writes=[SM["c1"]])
                    dve(lambda e: e.tensor_single_scalar(out=SM["c2"][:], in_=SM["t1"][:], scalar=255.5, op=ALU.is_ge),
                        reads=[SM["t1"]], writes=[SM["c2"]])
                    dve(lambda e: e.tensor_tensor(out=SM["tie"][:], in0=SM["c1"][:], in1=SM["c2"][:], op=ALU.mult),
                        reads=[SM["c1"], SM["c2"]], writes=[SM["tie"]])
                    dve(lambda e: e.tensor_scalar(out=SM["nd0"][:], in0=SM["cpos"][:], scalar1=-1.0, scalar2=256.0 - 16384.0,
                                                  op0=ALU.mult, op1=ALU.add), reads=[SM["cpos"]], writes=[SM["nd0"]])
                    dve(lambda e: e.tensor_scalar(out=SM["need"][:], in0=SM["nd0"][:], scalar1=SM["tie"][:, 0:1], scalar2=16384.0,
                                                  op0=ALU.mult, op1=ALU.add), reads=[SM["nd0"], SM["tie"]], writes=[SM["need"]])
                    pool(lambda e, N=N: e.tensor_tensor(out=cum[:, 0:N], in0=cum[:, 0:N], in1=zt[:, 0:N], op=ALU.mult),
                         reads=[cum, zt], writes=[cum])
                    pool(lambda e, N=N: e.tensor_scalar(out=zt[:, 0:N], in0=cum[:, 0:N], scalar1=SM["need"][:, 0:1], scalar2=None,
                                                        op0=ALU.is_gt), reads=[cum, SM["need"]], writes=[zt])
                    for it in range(NIT):
                        dve(lambda e, lo=lo, hi=hi: e.tensor_scalar(out=mid[:], in0=lo[:], scalar1=hi[:, 0:1], scalar2=0.5, op0=ALU.add, op1=ALU.mult),
                            reads=[lo, hi], writes=[mid])
                        dve(lambda e, N=N: e.tensor_scalar(out=junk[:, 0:N], in0=sc[:, 0:N], scalar1=mid[:, 0:1], scalar2=None,
                                                           op0=ALU.is_ge, op1=ALU.add, accum_out=cn[:]),
                            reads=allsc + [mid], writes=[junk, cn])
                        dve(lambda e: e.tensor_single_scalar(out=pge[:], in_=cn[:], scalar=255.5, op=ALU.is_ge), reads=[cn], writes=[pge])
                        dve(lambda e: e.tensor_single_scalar(out=plt[:], in_=cn[:], scalar=255.5, op=ALU.is_lt), reads=[cn], writes=[plt])
                        dve(lambda e, lo=lo: e.copy_predicated(out=lo[:], mask=pge[:], data=mid[:]), reads=[pge, mid], writes=[lo])
                        dve(lambda e, hi=hi: e.copy_predicated(out=hi[:], mask=plt[:], data=mid[:]), reads=[plt, mid], writes=[hi])
                if qb >= 2:
                    dve(lambda e, N=N, lo=lo: e.scalar_tensor_tensor(out=msk[:, 0:N], in0=sc[:, 0:N], scalar=lo[:, 0:1], in1=zt[:, 0:N],
                                                                     op0=ALU.is_ge, op1=ALU.subtract), reads=allsc + [lo, zt], writes=[msk])
                else:
                    dve(lambda e, N=N, lo=lo: e.tensor_scalar(out=msk[:, 0:N], in0=sc[:, 0:N], scalar1=lo[:, 0:1], scalar2=None, op0=ALU.is_ge),
                        reads=allsc + [lo], writes=[msk])
                if cfg.dbg:
                    p.op("sp", lambda e, qb=qb, N=N: e.dma_start(out=D_sc[qb, :, 0:N], in_=sc[:, 0:N]), reads=allsc, writes=[R["out"]], dma=sc, join=True)
                    p.op("sp", lambda e, qb=qb, lo=lo: e.dma_start(out=D_lo[qb, :, 0:1], in_=lo[:], allow_slow_non_contiguous=True), reads=[lo], writes=[R["out"]], dma=lo, join=True)
                    if qb >= 2:
                      p.op("sp", lambda e, qb=qb, hi=hi: e.dma_start(out=D_lo[qb, :, 1:2], in_=hi[:], allow_slow_non_contiguous=True), reads=[hi], writes=[R["out"]], dma=hi, join=True)
                for c in range(qb + 1):
                    ee = EE[c % 2]
                    em = EM[c % 2]
                    pe(lambda e, c=c: e.transpose(out=ptr_b[:, 0:128], in_=msk[:, c * 128:(c + 1) * 128], identity=identb[:]),
                       reads=[msk, identb], writes=[ptr])
                    for hl in range(2):
                        mm(ST[hl][:], kdup[hl * 64:(hl + 1) * 64, c * 128:(c + 1) * 128], qf[hl * 64:(hl + 1) * 64, :, :], True, True,
                           [kdup, qf], [ST[hl]])
                        act(lambda e, hl=hl, ee=ee: e.activation(out=ee[:, hl * 512:(hl + 1) * 512], in_=ST[hl][:], func=AF.Exp,
                                                                 bias=nb[:, 0:1], scale=0.125), reads=[ST[hl], nb], writes=[ee])
                    dve(lambda e, ee=ee, em=em: e.tensor_tensor(out=em[:].rearrange("q (h t) -> q h t", h=8),
                                                                in0=ee[:].rearrange("q (h t) -> q h t", h=8),
                                                                in1=ptr_b[:, 0:128].unsqueeze(1).broadcast_to([128, 8, 128]), op=ALU.mult),
                        reads=[ee, ptr], writes=[em])
                    for hl in range(2):
                        mm(OO[hl][0:65, :], vp[:, c, :], em[:, hl * 512:(hl + 1) * 512], c == 0, c == qb, [vp, em], [OO[hl]])
                for hl in range(2):
                    act(lambda e, hl=hl: e.activation(out=osb[:, hl * 512:(hl + 1) * 512], in_=OO[hl][0:65, :], func=AF.Copy),
                        reads=[OO[hl]], writes=[osb])
                for hl in range(2):
                    mm(PH[hl][0:64, :], sel[:, :], osb[:, hl * 512:(hl + 1) * 512], True, True, [sel, osb], [PH[hl]])
                    dve(lambda e, hl=hl: e.reciprocal(out=rec[:, hl * 512:(hl + 1) * 512], in_=PH[hl][0:64, :]), reads=[PH[hl]], writes=[rec])
                ya = YA[b]
                dve(lambda e, ya=ya: e.tensor_tensor(out=ya[:], in0=osb[0:64, :], in1=rec[:], op=ALU.mult), reads=[osb, rec], writes=[ya])
                for hl in range(2):
                    store(S_ya.rearrange("d (hp hl) t -> d hl hp t", hl=2)[:, hl, :, t0:t0 + 128],
                          ya[:, hl * 512:(hl + 1) * 512].rearrange("d (hp t) -> d hp t", hp=4), ya, R["ya"])
            p.flush()
        _after_reset(p)


    def phase_s5(l):
        TC = 512
        NTC = NT // TC
        with nc.reset_on_exit():
            def sm(name, shape=(128, 32), dt=F32):
                return p.sb("s5_" + name, list(shape), dt)
            lre, lim, dtt = sm("lre"), sm("lim"), sm("dtt")
            aa, th, rr = sm("aa"), sm("th"), sm("rr")
            ft, kk0, fr0, af0 = sm("ft"), sm("kk0"), sm("fr0"), sm("af0")
            sn, cs = sm("sn"), sm("cs")
            nre, nim, den = sm("nre"), sm("nim"), sm("den")
            cre, cim, ta, tb = sm("cre"), sm("cim"), sm("ta"), sm("tb")
            CB, CC = sm("CB"), sm("CC")
            hpi = sm("hpi", (128, 1))
            sgn1 = sm("sgn1", (128, 1))
            rowmask = sm("rowmask", (128, 8))
            colmask = sm("colmask", (128, 8, 128))
            dcol = sm("dcol", (128, 4))
            X1, X2 = sm("X1", (128, 32, 16)), sm("X2", (128, 32, 16))
            BB, BBs, tmpB = sm("BB", (128, 512)), sm("BBs", (128, 512)), sm("tmpB", (128, 512))
            Cin, Cin2 = sm("Cin", (128, 2, 64)), sm("Cin2", (128, 2, 64))
            LB = sm("LB", (128, 8, 2, 128), BF16)
            LC = sm("LC", (128, 8, 2, 128), BF16)
            uj = sm("uj", (128, NT), BF16)
            iota1, onesT = sm("iota1", (128, TC)), sm("onesT", (128, TC))
            carry = sm("carry", (128, 32))
            NB = 2
            TT = [sm("tt%d" % i, (128, TC)) for i in range(NB)]
            KK = [sm("kk%d" % i, (128, TC)) for i in range(NB)]
            FR = [sm("fr%d" % i, (128, TC)) for i in range(NB)]
            AFR = [sm("afr%d" % i, (128, TC)) for i in range(NB)]
            SN = [sm("sn%d" % i, (128, TC)) for i in range(NB)]
            CS = [sm("cs%d" % i, (128, TC)) for i in range(NB)]
            RT = [sm("rt%d" % i, (128, TC)) for i in range(NB)]
            T1 = [sm("t1%d" % i, (128, TC)) for i in range(NB)]
            T2 = [sm("t2%d" % i, (128, TC)) for i in range(NB)]
            STt = [sm("st%d" % i, (128, TC)) for i in range(NB)]
            Z1 = [sm("z1%d" % i, (128, TC), BF16) for i in range(NB)]
            Z2 = [sm("z2%d" % i, (128, TC), BF16) for i in range(NB)]
            YO = [sm("yo%d" % i, (128, TC)) for i in range(NB)]

            pool(lambda e: e.iota(iota1[:], pattern=[[1, TC]], base=1, channel_multiplier=0, allow_small_or_imprecise_dtypes=True), writes=[iota1])
            pool(lambda e: e.memset(onesT[:], 1.0), writes=[onesT])
            pool(lambda e: e.memset(hpi[:], TWO_PI / 4), writes=[hpi])
            pool(lambda e: e.memset(sgn1[:], 1.0), writes=[sgn1])
            pool(lambda e: e.memset(sgn1[64:128, :], -1.0), writes=[sgn1])
            pool(lambda e: e.memset(carry[:], 0.0), writes=[carry])
            pool(lambda e: e.memset(rowmask[:], 1.0), writes=[rowmask])
            pool(lambda e: e.affine_select(out=rowmask[:], in_=rowmask[:], pattern=[[-16, 8]], compare_op=ALU.is_ge, fill=0.0,
                                           base=0, channel_multiplier=1), reads=[rowmask], writes=[rowmask])
            pool(lambda e: e.affine_select(out=rowmask[:], in_=rowmask[:], pattern=[[16, 8]], compare_op=ALU.is_ge, fill=0.0,
                                           base=15, channel_multiplier=-1), reads=[rowmask], writes=[rowmask])
            pool(lambda e: e.memset(colmask[:], 0.0), writes=[colmask])
            for gl in range(8):
                pool(lambda e, gl=gl: e.memset(colmask[:, gl, 16 * gl:16 * gl + 16], 1.0), writes=[colmask])
            for hf in range(2):
                load(lre[hf * 64:(hf + 1) * 64, :], W["s5_lam_re"][l].rearrange("g q -> q g"), lre, slow=True, join=True)
                load(lim[hf * 64:(hf + 1) * 64, :], W["s5_lam_im"][l].rearrange("g q -> q g"), lim, slow=True, join=True)
            load(dtt[:], W["s5_log_dt"][l].partition_broadcast(128), dtt, slow=True)
            load(dcol[:], W["s5_d"][l].rearrange("g c -> (g c)").rearrange("(j q) -> q j", q=128), dcol, slow=True)
            bre = W["s5_b_re"][l].rearrange("g q c -> q g c")
            bim = W["s5_b_im"][l].rearrange("g q c -> q g c")
            load(X1[0:64], bre, X1, join=True)
            load(X1[64:128], bim, X1, join=True)
            load(X2[0:64], bim, X2, join=True)
            load(X2[64:128], bre, X2, join=True)

            def tt_(o, a, b, op):
                dve(lambda e: e.tensor_tensor(out=o[:], in0=a[:], in1=b[:], op=op), reads=[a, b], writes=[o])

            def ts_(o, a, s1, op0, s2=None, op1=None):
                if op1 is None:
                    dve(lambda e: e.tensor_scalar(out=o[:], in0=a[:], scalar1=s1, scalar2=None, op0=op0), reads=[a], writes=[o])
                else:
                    dve(lambda e: e.tensor_scalar(out=o[:], in0=a[:], scalar1=s1, scalar2=s2, op0=op0, op1=op1), reads=[a], writes=[o])

            act(lambda e: e.activation(out=dtt[:], in_=dtt[:], func=AF.Exp), reads=[dtt], writes=[dtt])
            ts_(lre, lre, -1e-4, ALU.min)
            tt_(aa, lre, dtt, ALU.mult)
            tt_(th, lim, dtt, ALU.mult)
            act(lambda e: e.activation(out=rr[:], in_=aa[:], func=AF.Exp), reads=[aa], writes=[rr])
            ts_(ft, th, 1.0 / TWO_PI, ALU.mult)
            ts_(kk0, ft, MAGIC, ALU.add)
            ts_(kk0, kk0, MAGIC, ALU.subtract)
            tt_(fr0, ft, kk0, ALU.subtract)
            act(lambda e: e.activation(out=sn[:], in_=fr0[:], func=AF.Sin, scale=TWO_PI), reads=[fr0], writes=[sn])
            act(lambda e: e.activation(out=af0[:], in_=fr0[:], func=AF.Abs), reads=[fr0], writes=[af0])
            act(lambda e: e.activation(out=cs[:], in_=af0[:], func=AF.Sin, scale=-TWO_PI, bias=hpi[:, 0:1]), reads=[af0, hpi], writes=[cs])
            tt_(nre, rr, cs, ALU.mult)
            ts_(nre, nre, -1.0, ALU.add)
            tt_(nim, rr, sn, ALU.mult)
            tt_(den, lre, lre, ALU.mult)
            tt_(ta, lim, lim, ALU.mult)
            tt_(den, den, ta, ALU.add)
            dve(lambda e: e.reciprocal(out=den[:], in_=den[:]), reads=[den], writes=[den])
            tt_(ta, nre, lre, ALU.mult)
            tt_(tb, nim, lim, ALU.mult)
            tt_(cre, ta, tb, ALU.add)
            tt_(cre, cre, den, ALU.mult)
            tt_(ta, nim, lre, ALU.mult)
            tt_(tb, nre, lim, ALU.mult)
            tt_(cim, ta, tb, ALU.subtract)
            tt_(cim, cim, den, ALU.mult)
            dve(lambda e: e.tensor_scalar(out=CB[:], in0=cim[:], scalar1=sgn1[:, 0:1], scalar2=-1.0, op0=ALU.mult, op1=ALU.mult),
                reads=[cim, sgn1], writes=[CB])
            dve(lambda e: e.tensor_scalar(out=CC[:], in0=cre[:], scalar1=sgn1[:, 0:1], scalar2=None, op0=ALU.mult),
                reads=[cre, sgn1], writes=[CC])

            def bc(t_):
                return t_[:].unsqueeze(2).broadcast_to([128, 32, 16])

            def v3(t_):
                return t_[:].rearrange("q (g c) -> q g c", c=16)
            dve(lambda e: e.tensor_tensor(out=v3(BB), in0=X1[:], in1=bc(cre), op=ALU.mult), reads=[X1, cre], writes=[BB])
            dve(lambda e: e.tensor_tensor(out=v3(tmpB), in0=X2[:], in1=bc(CB), op=ALU.mult), reads=[X2, CB], writes=[tmpB])
            tt_(BB, BB, tmpB, ALU.add)
            dve(lambda e: e.tensor_tensor(out=v3(BBs), in0=X2[:], in1=bc(CC), op=ALU.mult), reads=[X2, CC], writes=[BBs])
            dve(lambda e: e.tensor_tensor(out=v3(tmpB), in0=X1[:], in1=bc(cim), op=ALU.mult), reads=[X1, cim], writes=[tmpB])
            tt_(BBs, BBs, tmpB, ALU.add)

            cflat_re = W["s5_c_re"][l].rearrange("g c q -> (g c) q")
            cflat_im = W["s5_c_im"][l].rearrange("g c q -> (g c) q")
            bcnt = [0]
            ycnt = [0]
            for j in range(4):
                load(uj[:], S_su[:, j, :], uj, reads=[R["su"]])
                load(Cin[:, 0, :], cflat_re[j * 128:(j + 1) * 128, :], Cin, join=True)
                load(Cin[:, 1, :], cflat_im[j * 128:(j + 1) * 128, :], Cin, join=True)
                load(Cin2[:, 0, :], cflat_im[j * 128:(j + 1) * 128, :], Cin2, join=True)
                load(Cin2[:, 1, :], cflat_re[j * 128:(j + 1) * 128, :], Cin2, join=True)
                for var, src in ((0, BB), (1, BBs)):
                    ps = PS[var]
                    pe(lambda e, ps=ps, src=src, j=j: e.transpose(out=ps[:, 0:128], in_=src[:, j * 128:(j + 1) * 128], identity=ident32[:]),
                       reads=[src, ident32], writes=[ps])
                    for gl in range(8):
                        dve(lambda e, ps=ps, gl=gl, var=var: e.tensor_scalar(out=LB[:, gl, var, :], in0=ps[:, 0:128], scalar1=rowmask[:, gl:gl + 1],
                                                                             scalar2=None, op0=ALU.mult), reads=[ps, rowmask], writes=[LB])
                for var, src, sc_ in ((0, Cin, 1.0), (1, Cin2, -1.0)):
                    ps = PS[2 + var]
                    pe(lambda e, ps=ps, src=src: e.transpose(out=ps[:, 0:128], in_=src[:].rearrange("q a b -> q (a b)"), identity=ident32[:]),
                       reads=[src, ident32], writes=[ps])
                    for gl in range(8):
                        if var == 0:
                            dve(lambda e, ps=ps, gl=gl: e.scalar_tensor_tensor(out=LC[:, gl, 0, :], in0=ps[:, 0:128], scalar=sgn1[:, 0:1],
                                                                               in1=colmask[:, gl, :], op0=ALU.mult, op1=ALU.mult),
                                reads=[ps, sgn1, colmask], writes=[LC])
                        else:
                            dve(lambda e, ps=ps, gl=gl: e.scalar_tensor_tensor(out=LC[:, gl, 1, :], in0=ps[:, 0:128], scalar=-1.0,
                                                                               in1=colmask[:, gl, :], op0=ALU.mult, op1=ALU.mult),
                                reads=[ps, colmask], writes=[LC])
                for tc in range(NTC):
                    yps = PS[6 + ycnt[0] % 2]
                    ycnt[0] += 1
                    usl = uj[:, tc * TC:(tc + 1) * TC]
                    for gl in range(8):
                        g = 8 * j + gl
                        b = bcnt[0] % NB
                        bcnt[0] += 1
                        tt, kk, fr, afr, snT, csT, rt = TT[b], KK[b], FR[b], AFR[b], SN[b], CS[b], RT[b]
                        t1, t2, st, z1, z2 = T1[b], T2[b], STt[b], Z1[b], Z2[b]
                        pool(lambda e, tt=tt, g=g, tc=tc: e.tensor_scalar(out=tt[:], in0=iota1[:], scalar1=float(tc * TC), scalar2=ft[:, g:g + 1],
                                                                          op0=ALU.add, op1=ALU.mult), reads=[iota1, ft], writes=[tt])
                        pool(lambda e, tt=tt, kk=kk: e.tensor_scalar(out=kk[:], in0=tt[:], scalar1=MAGIC, scalar2=None, op0=ALU.add),
                             reads=[tt], writes=[kk])
                        pool(lambda e, kk=kk: e.tensor_scalar(out=kk[:], in0=kk[:], scalar1=MAGIC, scalar2=None, op0=ALU.subtract),
                             reads=[kk], writes=[kk])
                        pool(lambda e, tt=tt, kk=kk, fr=fr: e.tensor_tensor(out=fr[:], in0=tt[:], in1=kk[:], op=ALU.subtract),
                             reads=[tt, kk], writes=[fr])
                        act(lambda e, fr=fr, snT=snT: e.activation(out=snT[:], in_=fr[:], func=AF.Sin, scale=TWO_PI), reads=[fr], writes=[snT])
                        act(lambda e, fr=fr, afr=afr: e.activation(out=afr[:], in_=fr[:], func=AF.Abs), reads=[fr], writes=[afr])
                        act(lambda e, afr=afr, csT=csT: e.activation(out=csT[:], in_=afr[:], func=AF.Sin, scale=-TWO_PI, bias=hpi[:, 0:1]),
                            reads=[afr, hpi], writes=[csT])
                        act(lambda e, rt=rt, g=g: e.activation(out=rt[:], in_=onesT[:], func=AF.Copy, scale=rr[:, g:g + 1]),
                            reads=[onesT, rr], writes=[rt])
                        pa = PS[2 * (b % 2)]
                        pb = PS[2 * (b % 2) + 1]
                        mm(pa[:], LB[:, gl, 0, :], usl, True, True, [LB, uj], [pa])
                        mm(pb[:], LB[:, gl, 1, :], usl, True, True, [LB, uj], [pb])
                        dve(lambda e, t1=t1, pa=pa, csT=csT: e.tensor_tensor(out=t1[:], in0=pa[:], in1=csT[:], op=ALU.mult), reads=[pa, csT], writes=[t1])
                        dve(lambda e, t2=t2, pb=pb, snT=snT: e.tensor_tensor(out=t2[:], in0=pb[:], in1=snT[:], op=ALU.mult), reads=[pb, snT], writes=[t2])
                        dve(lambda e, t1=t1, t2=t2: e.tensor_tensor(out=t1[:], in0=t1[:], in1=t2[:], op=ALU.add), reads=[t1, t2], writes=[t1])
                        dve(lambda e, st=st, rt=rt, t1=t1, g=g: e.tensor_tensor_scan(out=st[:], data0=rt[:], data1=t1[:], initial=carry[:, g:g + 1],
                                                                                     op0=ALU.mult, op1=ALU.add), reads=[rt, t1, carry], writes=[st])
                        act(lambda e, st=st, g=g: e.activation(out=carry[:, g:g + 1], in_=st[:, TC - 1:TC], func=AF.Copy), reads=[st], writes=[carry])
                        pool(lambda e, z1=z1, st=st, csT=csT: e.tensor_tensor(out=z1[:], in0=st[:], in1=csT[:], op=ALU.mult), reads=[st, csT], writes=[z1])
                        pool(lambda e, z2=z2, st=st, snT=snT: e.tensor_tensor(out=z2[:], in0=st[:], in1=snT[:], op=ALU.mult), reads=[st, snT], writes=[z2])
                        mm(yps[:], LC[:, gl, 0, :], z1[:], gl == 0, False, [LC, z1], [yps])
                        mm(yps[:], LC[:, gl, 1, :], z2[:], False, gl == 7, [LC, z2], [yps])
                    yo = YO[ycnt[0] % 2]
                    dve(lambda e, yo=yo, usl=usl, yps=yps, j=j: e.scalar_tensor_tensor(out=yo[:], in0=usl, scalar=dcol[:, j:j + 1], in1=yps[:],
                                                                                      op0=ALU.mult, op1=ALU.add), reads=[uj, dcol, yps], writes=[yo])
                    store(S_ys5[:, j, tc * TC:(tc + 1) * TC], yo[:], yo, R["ys5"])
            p.flush()
        _after_reset(p)


    def phase_dil(l):
        NU = NT // 128
        with nc.reset_on_exit():
            QT = [p.sb("dq%d" % i, [128, NT], BF16) for i in range(2)]
            KT = [p.sb("dk%d" % i, [128, NT], BF16) for i in range(2)]
            VP = [p.sb("dv%d" % i, [128, NU, 65], BF16) for i in range(2)]
            VT = [p.sb("dvt%d" % i, [128, NT], BF16) for i in range(2)]
            ACC = p.sb("dacc", [65, NT], F32)
            band = p.sb("band", [128, 2, 128], BF16)
            bandf = p.sb("bandf", [128, 2, 128], F32)
            sel = p.sb("dsel", [65, 64], F32)
            nb = p.sb("dnb", [128, 1], F32)
            EE = [p.sb("dee%d" % i, [128, 256], BF16) for i in range(3)]
            EM = [p.sb("dem%d" % i, [128, 256], BF16) for i in range(3)]
            rec = p.sb("drec", [64, 512], F32)
            YC = [p.sb("dyc%d" % i, [64, 512], BF16) for i in range(2)]
            pool(lambda e: e.memset(bandf[:], 1.0), writes=[bandf])
            pool(lambda e: e.affine_select(out=bandf[:, 0, :], in_=bandf[:, 0, :], pattern=[[-1, 128]], compare_op=ALU.is_ge, fill=0.0,
                                           base=0, channel_multiplier=1), reads=[bandf], writes=[bandf])
            pool(lambda e: e.affine_select(out=bandf[:, 1, :], in_=bandf[:, 1, :], pattern=[[1, 128]], compare_op=ALU.is_ge, fill=0.0,
                                           base=0, channel_multiplier=-1), reads=[bandf], writes=[bandf])
            pool(lambda e: e.tensor_copy(out=band[:], in_=bandf[:]), reads=[bandf], writes=[band])
            pool(lambda e: e.memset(sel[:], 0.0), writes=[sel])
            pool(lambda e: e.memset(sel[64:65, :], 1.0), writes=[sel])
            pool(lambda e: e.memset(nb[:], -8.0), writes=[nb])
            for t_ in VP:
                pool(lambda e, t_=t_: e.memset(t_[:, :, 64:65], 1.0), writes=[t_])
            lc = [0]
            ec = [0]
            for hs in range(4):
                hp, hl = hs // 2, hs % 2
                psl = slice(hl * 64, (hl + 1) * 64)
                for g in range(3):
                    d = DIL[g]
                    nblk = NT // (d * 128)
                    qt, kt, vp = QT[lc[0] % 2], KT[lc[0] % 2], VP[lc[0] % 2]
                    lc[0] += 1
                    load(qt[psl, :], S_cq[g][psl, hp, :, :].rearrange("q r s -> q (r s)"), qt, reads=[R["cq"]])
                    load(kt[psl, :], S_ck[g][psl, hp, :, :].rearrange("q r s -> q (r s)"), kt, reads=[R["ck"]])
                    vt = VT[(lc[0] - 1) % 2]
                    load(vt[:, :], S_cv[g][:, hp, :, :].rearrange("q r s -> q (r s)"), vt, reads=[R["cv"]])
                    v3_ = vt[:].rearrange("q (r s) -> q r s", r=d)
                    for u in range(d * nblk):
                        r_, n_ = u // nblk, u % nblk
                        ptv = PS[6 + u % 2]
                        ptv_b = ptv.t[:].bitcast(BF16)
                        pe(lambda e, ptv_b=ptv_b, r_=r_, n_=n_, v3_=v3_: e.transpose(out=ptv_b[:, 0:128], in_=v3_[:, r_, n_ * 128:(n_ + 1) * 128],
                                                                                    identity=identb[:]), reads=[vt, identb], writes=[ptv])
                        act(lambda e, ptv_b=ptv_b, u=u, vp=vp, hl=hl: e.activation(out=vp[:, u, 0:64], in_=ptv_b[:, hl * 64:(hl + 1) * 64], func=AF.Copy),
                            reads=[ptv], writes=[vp])
                    if cfg.dbg and hs == 0 and g == 1:
                        p.op("sp", lambda e, vp=vp: e.dma_start(out=D_vp, in_=vp[:]), reads=[vp], writes=[R["out"]], dma=vp, join=True)
                    q3 = qt[:].rearrange("q (r s) -> q r s", r=d)
                    k3 = kt[:].rearrange("q (r s) -> q r s", r=d)
                    acc3 = ACC[:].rearrange("q (s r) -> q r s", r=d)
                    BU = min(4, nblk)
                    for r in range(d):
                        for n0 in range(0, nblk, BU):
                            ops_ = PS[4 + (ec[0] // BU) % 2]
                            for n in range(n0, n0 + BU):
                                u = r * nblk + n
                                stp = PS[ec[0] % 3]
                                ee = EE[ec[0] % 3]
                                em = EM[ec[0] % 3]
                                ec[0] += 1
                                qa = q3[psl, r, n * 128:(n + 1) * 128]
                                c0 = 0 if n > 0 else 128
                                if n > 0:
                                    mm(stp[:, 0:128], k3[psl, r, (n - 1) * 128:n * 128], qa, True, True, [kt, qt], [stp])
                                mm(stp[:, 128:256], k3[psl, r, n * 128:(n + 1) * 128], qa, True, True, [kt, qt], [stp])
                                act(lambda e, ee=ee, stp=stp, c0=c0: e.activation(out=ee[:, c0:256], in_=stp[:, c0:256], func=AF.Exp,
                                                                                 bias=nb[:, 0:1], scale=0.125), reads=[stp, nb], writes=[ee])
                                dve(lambda e, ee=ee, em=em, c0=c0: e.tensor_tensor(out=em[:, c0:256], in0=ee[:, c0:256],
                                                                                   in1=band[:].rearrange("q a b -> q (a b)")[:, c0:256], op=ALU.mult),
                                    reads=[ee, band], writes=[em])
                                oc = (n - n0) * 128
                                if n > 0:
                                    mm(ops_[0:65, oc:oc + 128], vp[:, u - 1, :], em[:, 0:128], True, False, [vp, em], [ops_])
                                    mm(ops_[0:65, oc:oc + 128], vp[:, u, :], em[:, 128:256], False, True, [vp, em], [ops_])
                                else:
                                    mm(ops_[0:65, oc:oc + 128], vp[:, u, :], em[:, 128:256], True, True, [vp, em], [ops_])
                            dst = acc3[:, r, n0 * 128:(n0 + BU) * 128]
                            if g == 0:
                                act(lambda e, dst=dst, ops_=ops_, BU=BU: e.activation(out=dst, in_=ops_[0:65, 0:BU * 128], func=AF.Copy),
                                    reads=[ops_], writes=[ACC])
                            else:
                                dve(lambda e, dst=dst, ops_=ops_, BU=BU: e.tensor_tensor(out=dst, in0=ops_[0:65, 0:BU * 128], in1=dst, op=ALU.add),
                                    reads=[ops_, ACC], writes=[ACC])
                for c in range(NT // 512):
                    bcp = PS[6 + c % 2]
                    mm(bcp[0:64, :], sel[:, :], ACC[:, c * 512:(c + 1) * 512], True, True, [sel, ACC], [bcp])
                    dve(lambda e, bcp=bcp: e.reciprocal(out=rec[:], in_=bcp[0:64, :]), reads=[bcp], writes=[rec])
                    yc = YC[c % 2]
                    dve(lambda e, yc=yc, c=c: e.tensor_tensor(out=yc[:], in0=ACC[0:64, c * 512:(c + 1) * 512], in1=rec[:], op=ALU.mult),
                        reads=[ACC, rec], writes=[yc])
                    store(S_yc[:, hs, c * 512:(c + 1) * 512], yc[:], yc, R["yc"])
            p.flush()
        _after_reset(p)

    def token_phase(lpost, lpre):
        nonlocal xt, hb, ab, SQ, SQB, rs, RSD, SG, WS, STG, xin, xtr, hbr, abr
        with nc.reset_on_exit():
            xt = p.sb("xt", [128, 8, T], F32)
            hb = p.sb("hb", [128, 8, T], BF16)
            ab = p.sb("ab", [128, 22, T], BF16)
            SQ = [p.sb("sq%d" % i, [128, T], F32) for i in range(2)]
            SQB = [p.sb("sqb%d" % i, [128, T], BF16) for i in range(2)]
            rs = p.sb("rs", [128, T], F32)
            RSD = [p.sb("rsd%d" % i, [128, T], F32) for i in range(2)]
            SG = [p.sb("sg%d" % i, [128, T], F32) for i in range(2)]
            WS = [p.sb("ws%d" % i, [128, 4096], BF16) for i in range(NWS)]
            STG = [p.sb("stg%d" % i, [128, 512], F32) for i in range(NST)]
            xin = p.sb("xin", [128, 4, D], F32)
            xtr = [Res("xt%d" % i) for i in range(8)]
            hbr = [Res("hb%d" % i) for i in range(8)]
            abr = [Res("ab%d" % i) for i in range(22)]
            if lpost is not None:
                ys5t = p.sb("ys5t", [128, 4, T], F32)
                tm_a = p.sb("tm_a", [128, 4, T], F32)
                tm_b = p.sb("tm_b", [128, 4, T], F32)
                yg = p.sb("yg", [128, 4, T], BF16)
                yb = p.sb("yb", [128, 4, T], BF16)
                yat = p.sb("yat", [64, 8, T], BF16)
                yct = p.sb("yct", [64, 4, T], BF16)
                gt = p.sb("gt", [128, 24, T], BF16)
                M1 = [p.sb("m1_%d" % i, [128, T], F32) for i in range(2)]
                M2 = [p.sb("m2_%d" % i, [128, T], F32) for i in range(2)]
                sgb = [p.sb("sgb%d" % i, [128, T], F32) for i in range(2)]
                ybr = [Res("yb%d" % i) for i in range(4)]

            def post_mixer(l, t0):
                tsl = slice(t0, t0 + T)
                p.op("sp", lambda e: e.dma_start(out=xt[:], in_=S_xT[:, :, tsl]), reads=[R["xT"]], writes=xtr, dma=xt)
                load(ys5t[:], S_ys5[:, :, tsl], ys5t, reads=[R["ys5"]])
                load(yat[:], S_ya[:, :, tsl], yat, reads=[R["ya"]])
                load(yct[:], S_yc[:, :, tsl], yct, reads=[R["yc"]])
                load(gt[:], S_g[:, :, tsl], gt, reads=[R["g"]])
                f2 = lambda t_: t_[:].rearrange("q a b -> q (a b)")
                act(lambda e: e.activation(out=f2(tm_a), in_=f2(ys5t), func=AF.Square), reads=[ys5t], writes=[tm_a])
                dve(lambda e: e.tensor_scalar(out=f2(tm_a), in0=f2(tm_a), scalar1=0.044715, scalar2=1.0, op0=ALU.mult, op1=ALU.add),
                    reads=[tm_a], writes=[tm_a])
                dve(lambda e: e.tensor_tensor(out=f2(tm_a), in0=f2(tm_a), in1=f2(ys5t), op=ALU.mult), reads=[tm_a, ys5t], writes=[tm_a])
                act(lambda e: e.activation(out=f2(tm_b), in_=f2(tm_a), func=AF.Sigmoid, scale=1.5957691216057308), reads=[tm_a], writes=[tm_b])
                dve(lambda e: e.tensor_tensor(out=f2(yg), in0=f2(tm_b), in1=f2(ys5t), op=ALU.mult), reads=[tm_b, ys5t], writes=[yg])
                sa, wa_ = wload(WB[l]["s5_glu_a"], WBres[l]["s5_glu_a"], 4, 512)
                sb_, wb_ = wload(WB[l]["s5_glu_b"], WBres[l]["s5_glu_b"], 4, 512)
                for oc in range(4):
                    pa, pb = psA(), psA()
                    for k in range(4):
                        mm(pa[:], wa_[:, k, oc * 128:(oc + 1) * 128], yg[:, k, :], k == 0, k == 3, [sa, yg], [pa])
                    for k in range(4):
                        mm(pb[:], wb_[:, k, oc * 128:(oc + 1) * 128], yg[:, k, :], k == 0, k == 3, [sb_, yg], [pb])
                    sg_ = sgb[oc % 2]
                    act(lambda e, sg_=sg_, pb=pb: e.activation(out=sg_[:], in_=pb[:], func=AF.Sigmoid), reads=[pb], writes=[sg_])
                    dve(lambda e, sg_=sg_, pa=pa, oc=oc: e.tensor_tensor(out=yb[:, oc, :], in0=pa[:], in1=sg_[:], op=ALU.mult),
                        reads=[pa, sg_], writes=[ybr[oc]])
                Wa, Wb, Wc = WB[l]["w_branch_a"], WB[l]["w_branch_b"], WB[l]["w_branch_c"]
                for half in range(2):
                    cs_ = slice(half * 512, (half + 1) * 512)
                    s1, wa = wload(Wa[:, cs_], WBres[l]["w_branch_a"], 8, 512, kp=64)
                    s2, wb = wload(Wb[:, cs_], WBres[l]["w_branch_b"], 4, 512)
                    s3, wc = wload(Wc[:, cs_], WBres[l]["w_branch_c"], 4, 512, kp=64)
                    for oc in range(4):
                        c = half * 4 + oc
                        osl = slice(oc * 128, (oc + 1) * 128)
                        pa, pb, pc = psA(), psA(), psA()
                        for h in range(8):
                            mm(pa[:], wa[:, h, osl], yat[:, h, :], h == 0, h == 7, [s1, yat], [pa])
                        for k in range(4):
                            mm(pb[:], wb[:, k, osl], yb[:, k, :], k == 0, k == 3, [s2, ybr[k]], [pb])
                        for k in range(4):
                            mm(pc[:], wc[:, k, osl], yct[:, k, :], k == 0, k == 3, [s3, yct], [pc])
                        m1, m2 = M1[c % 2], M2[c % 2]
                        dve(lambda e, m1=m1, pa=pa, c=c: e.tensor_tensor(out=m1[:], in0=pa[:], in1=gt[:, c, :], op=ALU.mult), reads=[pa, gt], writes=[m1])
                        dve(lambda e, m2=m2, pb=pb, c=c: e.tensor_tensor(out=m2[:], in0=pb[:], in1=gt[:, 8 + c, :], op=ALU.mult), reads=[pb, gt], writes=[m2])
                        pool(lambda e, m1=m1, m2=m2: e.tensor_tensor(out=m1[:], in0=m1[:], in1=m2[:], op=ALU.add), reads=[m1, m2], writes=[m1])
                        dve(lambda e, m2=m2, pc=pc, c=c: e.tensor_tensor(out=m2[:], in0=pc[:], in1=gt[:, 16 + c, :], op=ALU.mult), reads=[pc, gt], writes=[m2])
                        pool(lambda e, m1=m1, m2=m2, c=c: e.tensor_tensor(out=hb[:, c, :], in0=m1[:], in1=m2[:], op=ALU.add), reads=[m1, m2], writes=[hbr[c]])
                Wo = WB[l]["w_out"]
                for half in range(2):
                    so, wo = wload(Wo[:, half * 512:(half + 1) * 512], WBres[l]["w_out"], 8, 512)
                    for oc in range(4):
                        c = half * 4 + oc
                        ps = psA()
                        for k in range(8):
                            mm(ps[:], wo[:, k, oc * 128:(oc + 1) * 128], hb[:, k, :], k == 0, k == 7, [so, hbr[k]], [ps])
                        dve(lambda e, ps=ps, c=c: e.tensor_tensor(out=xt[:, c, :], in0=ps[:], in1=xt[:, c, :], op=ALU.add), reads=[ps, xtr[c]], writes=[xtr[c]])

            for tt in range(NTT):
                t0 = tt * T
                if lpost is None:
                    load_x_first(t0)
                else:
                    post_mixer(lpost, t0)
                    rmsnorm_fm(2, lpost)
                    ffn(lpost, "ffn2")
                if lpre is not None and cfg.sub < 9:
                    if cfg.sub >= 2:
                        rmsnorm_fm(0, lpre)
                    if cfg.sub >= 3:
                        ffn(lpre, "ffn1")
                    if cfg.sub >= 4:
                        rmsnorm_fm(1, lpre)
                        proj(lpre, t0)
                    store_x_last(t0)
                elif lpre is not None:
                    rmsnorm_fm(0, lpre)
                    ffn(lpre, "ffn1")
                    rmsnorm_fm(1, lpre)
                    proj(lpre, t0)
                    p.op("act", lambda e, t0=t0: e.dma_start(out=S_xT[:, :, t0:t0 + T], in_=xt[:]), reads=xtr, writes=[R["xT"]], dma=xt, join=True)
                else:
                    store_x_last(t0)
            p.flush()
        _after_reset(p)

    p.flush()
    if cfg.stage == 0:
        return nc
    token_phase(None, 0)
    for l in range(L):
        if cfg.stage in (2, 99):
            phase_dsa(l)
        if cfg.stage in (3, 99):
            phase_s5(l)
        if cfg.stage in (4, 99):
            phase_dil(l)
        if cfg.stage == 99:
            token_phase(l, l + 1 if l + 1 < L else None)
    print("ops", p.nops, "sems", p.nsem, "sbuf left", nc.sbuf_bytes_remaining)
    return nc


def _run(cfg, inputs):
    nc = build(cfg)
    x = np.ascontiguousarray(inputs["x"], dtype=np.float32)
    in_maps = []
    for c in range(cfg.NCORES):
        m = {"x": np.ascontiguousarray(x[c])}
        for k in WEIGHT_SHAPES:
            m[k] = np.ascontiguousarray(inputs[k], dtype=np.float32)
        in_maps.append(m)
    res = run_bass_kernel_spmd(nc, in_maps, core_ids=list(range(cfg.NCORES)))
    return res


def kernel(**inputs):
    cfg = Cfg()
    res = _run(cfg, inputs)
    return np.stack([r["out"] for r in res.results], axis=0).astype(np.float32)
```

```python
import numpy as np
import concourse.bass as bass
import concourse.mybir as mybir
from concourse.bass_utils import run_bass_kernel_spmd

F32 = mybir.dt.float32
BF16 = mybir.dt.bfloat16
AF = mybir.ActivationFunctionType
ALU = mybir.AluOpType
AX = mybir.AxisListType

D = 1024
FF = 2816
DIN = 6852
EPS = 1e-6
MAGIC = 12582912.0
TWO_PI = 6.283185307179586


class Res:
    __slots__ = ("name", "writers", "readers", "sem", "cnt")

    _all = []

    def __init__(self, name):
        self.name = name
        self.writers = []
        self.readers = []
        self.sem = None
        self.cnt = 0
        Res._all.append(self)


class Tile:
    def __init__(self, t, res):
        self.t = t
        self.res = res

    def __getitem__(self, k):
        return self.t[k]


class Prog:
    ENGS = ("pe", "act", "dve", "pool", "sp")

    def __init__(self, nc):
        self.nc = nc
        self.ops = {e: [] for e in self.ENGS}
        self.cnt = {e: 0 for e in self.ENGS}
        self.esem = {e: nc.alloc_semaphore("s_" + e) for e in ("pe", "act", "dve", "pool")}
        self.waited = {e: {} for e in self.ENGS}
        self.free_sems = {True: [(nc.alloc_semaphore("dsw%d" % i), 0) for i in range(6)],
                          False: [(nc.alloc_semaphore("dhw%d" % i), 0) for i in range(80)]}
        self.live = []
        self.nsem = 4
        self.nops = 0

    def sb(self, name, shape, dtype):
        self.uid = getattr(self, "uid", 0) + 1
        name = "%s_u%d" % (name, self.uid)
        return Tile(self.nc.alloc_sbuf_tensor(name, list(shape), dtype), Res(name))

    def ps(self, name, shape, dtype=F32):
        return Tile(self.nc.alloc_psum_tensor(name, list(shape), dtype), Res(name))

    def op(self, eng, fn, reads=(), writes=(), dma=None, join=False):
        reads = [r.res if isinstance(r, Tile) else r for r in reads]
        writes = [w.res if isinstance(w, Tile) else w for w in writes]
        toks = []
        for r in reads:
            for t in r.writers:
                toks.append((t, "raw"))
        for w in writes:
            if not join:
                for t in w.writers:
                    toks.append((t, "waw"))
            for t in w.readers:
                toks.append((t, "war"))
        waits = {}
        for (sem, val, teng), kind in toks:
            if teng == eng and dma is None and eng == "pe":
                continue
            k = id(sem)
            if self.waited[eng].get(k, 0) >= val:
                continue
            if k not in waits or waits[k][1] < val:
                waits[k] = (sem, val)
        for k, (sem, val) in waits.items():
            self.waited[eng][k] = val
        if dma is None:
            self.cnt[eng] += 1
            tok = (self.esem[eng], self.cnt[eng], eng)
            inc = (self.esem[eng], 1)
        else:
            d = dma.res if isinstance(dma, Tile) else dma
            if d.sem is None:
                sw = (eng == "pool")
                d.sem, d.cnt = self.free_sems[sw].pop()
                self.nsem = max(self.nsem, 86 - len(self.free_sems[True]) - len(self.free_sems[False]))
                self.live.append((d, sw))
            d.cnt += 16
            tok = (d.sem, d.cnt, "dma")
            inc = (d.sem, 16)
        self.ops[eng].append((list(waits.values()), fn, inc))
        self.nops += 1
        for r in reads:
            r.readers.append(tok)
        for w in writes:
            if join:
                w.writers.append(tok)
            else:
                w.writers = [tok]
                w.readers = []
        return tok

    def flush(self):
        nc = self.nc
        finals = [(self.esem[e], self.cnt[e]) for e in ("pe", "act", "dve", "pool")]
        finals += [(d.sem, d.cnt) for d, _ in self.live]
        ops = self.ops

        def emit(eng, e):
            for waits, fn, inc in ops[eng]:
                for sem, val in waits:
                    e.wait_ge(sem, val)
                fn(e).then_inc(inc[0], inc[1])
            for sem, val in finals:
                if val > 0:
                    e.wait_ge(sem, val)

        with nc.Block() as block:
            @block.tensor
            def _(e):
                emit("pe", e)

            @block.scalar
            def _(e):
                emit("act", e)

            @block.vector
            def _(e):
                emit("dve", e)

            @block.gpsimd
            def _(e):
                emit("pool", e)

            @block.sync
            def _(e):
                emit("sp", e)

        self.ops = {e: [] for e in self.ENGS}
        for d, sw in self.live:
            self.free_sems[sw].append((d.sem, d.cnt))
            d.sem = None
        self.live = []
        for e in self.ENGS:
            for sem, val in finals:
                self.waited[e][id(sem)] = val


def _after_reset(p):
    for e in p.ENGS:
        p.cnt[e] = 0
        p.waited[e] = {}
    for sw in (True, False):
        p.free_sems[sw] = [(sem, 0) for sem, _ in p.free_sems[sw]]
    for r in Res._all:
        r.writers = []
        r.readers = []
        r.sem = None
        r.cnt = 0


class Cfg:
    def __init__(self, seq=4096, depth=4, ncores=4, stage=99, dbg=False, sub=9):
        self.sub = sub
        self.pset = set(range(1, 10))
        self.variant = ''
        self.SEQ = seq
        self.DEPTH = depth
        self.NCORES = ncores
        self.stage = stage
        self.dbg = dbg


WEIGHT_SHAPES = {
    "ffn1_norm": [D], "ffn1_gate": [D, FF], "ffn1_up": [D, FF], "ffn1_down": [FF, D],
    "mix_norm": [D], "w_in": [D, DIN], "a_q_norm": [64], "a_k_norm": [64],
    "s5_lam_re": [32, 64], "s5_lam_im": [32, 64], "s5_log_dt": [32],
    "s5_b_re": [32, 64, 16], "s5_b_im": [32, 64, 16], "s5_c_re": [32, 16, 64], "s5_c_im": [32, 16, 64],
    "s5_d": [32, 16], "s5_glu_a": [512, 512], "s5_glu_b": [512, 512],
    "c_q_norm": [64], "c_k_norm": [64],
    "w_branch_a": [512, D], "w_branch_b": [512, D], "w_branch_c": [256, D], "w_out": [D, D],
    "ffn2_norm": [D], "ffn2_gate": [D, FF], "ffn2_up": [D, FF], "ffn2_down": [FF, D],
}
BIG = ["ffn1_gate", "ffn1_up", "ffn1_down", "w_in", "s5_glu_a", "s5_glu_b",
       "w_branch_a", "w_branch_b", "w_branch_c", "w_out", "ffn2_gate", "ffn2_up", "ffn2_down"]
DIL = (1, 4, 16)


def build(cfg):
    Res._all = []
    nc = bass.Bass("TRN2", target_bir_lowering=False)
    NT = cfg.SEQ
    L = cfg.DEPTH
    T = 512
    NTT = NT // T
    NQB = NT // 128
    p = Prog(nc)

    X = nc.dram_tensor("x", [NT, D], F32, kind="ExternalInput").ap()
    W = {k: nc.dram_tensor(k, [L] + s, F32, kind="ExternalInput").ap() for k, s in WEIGHT_SHAPES.items()}
    OUT = nc.dram_tensor("out", [NT, D], F32, kind="ExternalOutput").ap()
    dbg_kind = "ExternalOutput" if cfg.dbg else "Internal"

    def scr(name, shape, dt, dbg=True):
        return nc.dram_tensor(name, list(shape), dt, kind=dbg_kind if dbg else "Internal").ap()

    WB = [{k: scr("wb%d_%s" % (l, k), WEIGHT_SHAPES[k], BF16, dbg=False) for k in BIG} for l in range(L)]
    WBres = [{k: Res("wb%d_%s" % (l, k)) for k in BIG} for l in range(L)]
    S_xT = scr("s_xT", [128, 8, NT], F32)
    S_aq = scr("s_aq", [128, 4, NT], BF16)
    S_kdup = scr("s_kdup", [128, NT], BF16)
    S_ik = scr("s_ik", [64, NT], BF16)
    S_su = scr("s_su", [128, 4, NT], BF16)
    S_tm1 = scr("s_tm1", [128, NT // 128, 512], F32)
    S_av = scr("s_av", [128, NT // 128, 64], BF16)
    S_cv = [scr("s_cv%d" % g, [128, 2, DIL[g], NT // DIL[g]], BF16) for g in range(3)]
    S_cq = [scr("s_cq%d" % g, [128, 2, DIL[g], NT // DIL[g]], BF16) for g in range(3)]
    S_ck = [scr("s_ck%d" % g, [128, 2, DIL[g], NT // DIL[g]], BF16) for g in range(3)]
    S_g = scr("s_g", [128, 24, NT], BF16)
    S_ya = scr("s_ya", [64, 8, NT], BF16)
    S_ys5 = scr("s_ys5", [128, 4, NT], F32)
    S_yc = scr("s_yc", [64, 4, NT], BF16)
    if cfg.dbg:
        D_vp = scr("dbg_vp", [128, NT // 128, 65], BF16)
        D_sc = scr("dbg_sc", [NT // 128, 128, NT], F32)
        D_lo = scr("dbg_lo", [NT // 128, 128, 2], F32)
    R = {n: Res(n) for n in ["xT", "aq", "kdup", "ik", "su", "tm1", "av", "cv", "cq", "ck", "g", "ya", "ys5", "yc", "out"]}

    ident32 = p.sb("ident32", [128, 128], F32)
    identb = p.sb("identb", [128, 128], BF16)
    ones32 = p.sb("ones32", [128, 128], F32)
    blk = p.sb("blk", [128, 128], BF16)
    epsc = p.sb("epsc", [128, 1], F32)
    gvec = p.sb("gvec", [128, 3, L, 8], F32)
    gqk = p.sb("gqk", [128, 4, L], F32)
    PS = [p.ps("ps%d" % i, [128, 512], F32) for i in range(8)]

    def pool(fn, reads=(), writes=()):
        return p.op("pool", fn, reads=reads, writes=writes)

    def act(fn, reads=(), writes=()):
        return p.op("act", fn, reads=reads, writes=writes)

    def dve(fn, reads=(), writes=()):
        return p.op("dve", fn, reads=reads, writes=writes)

    def pe(fn, reads=(), writes=()):
        return p.op("pe", fn, reads=reads, writes=writes)

    def mm(out_ap, lhsT, rhs, start, stop, reads, writes):
        return p.op("pe", lambda e: e.matmul(out_ap, lhsT=lhsT, rhs=rhs, start=start, stop=stop), reads=reads, writes=writes)

    def load(out_ap, in_ap, tile, reads=(), q="sp", slow=False, join=False):
        return p.op(q, lambda e: e.dma_start(out=out_ap, in_=in_ap, allow_slow_non_contiguous=slow),
                    reads=reads, writes=[tile], dma=tile, join=join)

    def store(out_ap, in_ap, tile, wres, q="act", join=True):
        return p.op(q, lambda e: e.dma_start(out=out_ap, in_=in_ap), reads=[tile], writes=[wres], dma=tile, join=join)

    pool(lambda e: e.memset(ident32[:], 1.0), writes=[ident32])
    pool(lambda e: e.affine_select(out=ident32[:], in_=ident32[:], pattern=[[-1, 128]], compare_op=ALU.is_equal,
                                   fill=0.0, base=0, channel_multiplier=1), reads=[ident32], writes=[ident32])
    pool(lambda e: e.tensor_copy(out=identb[:], in_=ident32[:]), reads=[ident32], writes=[identb])
    pool(lambda e: e.memset(ones32[:], 1.0), writes=[ones32])
    pool(lambda e: e.memset(blk[:], 0.0), writes=[blk])
    pool(lambda e: e.memset(blk[0:64, 0:64], 1.0), writes=[blk])
    pool(lambda e: e.memset(blk[64:128, 64:128], 1.0), writes=[blk])
    pool(lambda e: e.memset(epsc[:], EPS), writes=[epsc])
    for wi, nm in enumerate(["ffn1_norm", "mix_norm", "ffn2_norm"]):
        load(gvec[:, wi, :, :], W[nm].rearrange("l (c q) -> q l c", q=128), gvec, slow=True, join=True)
    for wi, nm in enumerate(["a_q_norm", "a_k_norm", "c_q_norm", "c_k_norm"]):
        for hf in range(2):
            load(gqk[hf * 64:(hf + 1) * 64, wi, :], W[nm].rearrange("l d -> d l"), gqk, slow=True, join=True)

    castchain = Res("castchain")
    for l in range(L):
        castres = Res("cast%d" % l)
        tok = None
        for k in BIG:
            src = W[k][l]
            K, N = WEIGHT_SHAPES[k]
            for n0 in range(0, N, 2048):
                n1 = min(N, n0 + 2048)
                tok = p.op("pool", lambda e, s=src[:, n0:n1], d=WB[l][k][:, n0:n1]: e.dma_start(out=d, in_=s),
                           reads=[castchain], writes=[WBres[l][k], castchain], dma=castres)
        for k in BIG:
            WBres[l][k].writers = [tok]

    xt = hb = ab = SQ = SQB = rs = RSD = SG = WS = STG = xin = xtr = hbr = abr = None
    NWS = 4
    NST = 6
    cnt = {"ws": 0, "st": 0, "pa": 0, "sq": 0, "sqb": 0, "rsd": 0, "sg": 0}

    def nxt(key, lst):
        t = lst[cnt[key] % len(lst)]
        cnt[key] += 1
        return t

    def psA():
        return nxt("pa", PS[0:4])

    def wload(src, wres, kc, n, kp=128):
        slot = nxt("ws", WS)
        view = slot.t[0:kp, 0:kc * n].rearrange("q (k n) -> q k n", k=kc)
        p.op("sp", lambda e: e.dma_start(out=view, in_=src.rearrange("(k q) n -> q k n", q=kp)),
             reads=[wres], writes=[slot], dma=slot)
        return slot, view

    def stg_bf(shape_free):
        st = nxt("st", STG)
        n = 1
        for s in shape_free:
            n *= s
        v = st.t[:].bitcast(BF16)[:, 0:n]
        return st, v

    def rmsnorm_fm(which, l):
        ss = PS[4]
        for c in range(8):
            sq = nxt("sq", SQ)
            act(lambda e, sq=sq, c=c: e.activation(out=sq[:], in_=xt[:, c, :], func=AF.Square), reads=[xtr[c]], writes=[sq])
            mm(ss[:], ones32[:], sq[:], c == 0, c == 7, [ones32, sq], [ss])
        act(lambda e: e.activation(out=rs[:], in_=ss[:], func=AF.Sqrt, bias=epsc[:, 0:1], scale=1.0 / D), reads=[ss, epsc], writes=[rs])
        dve(lambda e: e.reciprocal(out=rs[:], in_=rs[:]), reads=[rs], writes=[rs])
        for c in range(8):
            dve(lambda e, c=c: e.scalar_tensor_tensor(out=hb[:, c, :], in0=xt[:, c, :], scalar=gvec[:, which, l, c:c + 1],
                                                      in1=rs[:], op0=ALU.mult, op1=ALU.mult), reads=[xtr[c], gvec, rs], writes=[hbr[c]])

    def ffn(l, pre):
        Wg, Wu, Wd = WB[l][pre + "_gate"], WB[l][pre + "_up"], WB[l][pre + "_down"]
        rg, ru, rd = WBres[l][pre + "_gate"], WBres[l][pre + "_up"], WBres[l][pre + "_down"]
        for n0 in range(0, FF, 512):
            n1 = min(FF, n0 + 512)
            sg_, wg = wload(Wg[:, n0:n1], rg, 8, n1 - n0)
            su_, wu = wload(Wu[:, n0:n1], ru, 8, n1 - n0)
            for c0 in range(0, n1 - n0, 128):
                f = (n0 + c0) // 128
                pg = psA()
                pu = psA()
                for k in range(8):
                    mm(pg[:], wg[:, k, c0:c0 + 128], hb[:, k, :], k == 0, k == 7, [sg_, hbr[k]], [pg])
                for k in range(8):
                    mm(pu[:], wu[:, k, c0:c0 + 128], hb[:, k, :], k == 0, k == 7, [su_, hbr[k]], [pu])
                sg = nxt("sg", SG)
                act(lambda e, sg=sg, pg=pg: e.activation(out=sg[:], in_=pg[:], func=AF.Silu), reads=[pg], writes=[sg])
                dve(lambda e, sg=sg, pu=pu, f=f: e.tensor_tensor(out=ab[:, f, :], in0=pu[:], in1=sg[:], op=ALU.mult),
                    reads=[pu, sg], writes=[abr[f]])
        for half in range(2):
            accs = PS[4:8]
            for fg in range(0, 22, 4):
                nf = min(4, 22 - fg)
                sl, wv = wload(Wd[fg * 128:(fg + nf) * 128, half * 512:(half + 1) * 512], rd, nf, 512)
                for j in range(nf):
                    f = fg + j
                    for dc in range(4):
                        mm(accs[dc][:], wv[:, j, dc * 128:(dc + 1) * 128], ab[:, f, :], f == 0, f == 21, [sl, abr[f]], [accs[dc]])
            for dc in range(4):
                c = half * 4 + dc
                dve(lambda e, c=c, a=accs[dc]: e.scalar_tensor_tensor(out=xt[:, c, :], in0=a[:], scalar=0.5, in1=xt[:, c, :],
                                                                       op0=ALU.mult, op1=ALU.add), reads=[accs[dc], xtr[c]], writes=[xtr[c]])

    def qknorm_epi(ps, cw, gidx, l, out_ap, view=None):
        sqb = nxt("sqb", SQB)
        act(lambda e: e.activation(out=sqb[0:cw, :], in_=ps[0:cw, :], func=AF.Square), reads=[ps], writes=[sqb])
        ss = PS[5] if cnt["rsd"] % 2 == 0 else PS[6]
        mm(ss[0:cw, :], blk[0:cw, 0:cw], sqb[0:cw, :], True, True, [blk, sqb], [ss])
        rsd = nxt("rsd", RSD)
        act(lambda e: e.activation(out=rsd[0:cw, :], in_=ss[0:cw, :], func=AF.Sqrt, bias=epsc[0:cw, 0:1], scale=1.0 / 64),
            reads=[ss, epsc], writes=[rsd])
        dve(lambda e: e.reciprocal(out=rsd[0:cw, :], in_=rsd[0:cw, :]), reads=[rsd], writes=[rsd])
        in0 = ps[0:cw, :]
        in1 = rsd[0:cw, :]
        if view is not None:
            in0 = in0.rearrange(view[0], **view[1])
            in1 = in1.rearrange(view[0], **view[1])
        return (lambda st: dve(lambda e: e.scalar_tensor_tensor(out=out_ap, in0=in0, scalar=gqk[0:cw, gidx, l:l + 1], in1=in1,
                                                                op0=ALU.mult, op1=ALU.mult), reads=[ps, rsd, gqk], writes=[st]))

    def proj(l, t0):
        Win = WB[l]["w_in"]
        rw = WBres[l]["w_in"]

        def fm_group(n0, n1, epi):
            sl, wv = wload(Win[:, n0:n1], rw, 8, n1 - n0)
            for c0 in range(0, n1 - n0, 128):
                cw = min(128, n1 - n0 - c0)
                ps = psA()
                for k in range(8):
                    mm(ps[0:cw, :], wv[:, k, c0:c0 + cw], hb[:, k, :], k == 0, k == 7, [sl, hbr[k]], [ps])
                epi(n0 + c0, cw, ps)

        def epi_aq(col, cw, ps):
            hp = col // 128
            st, v = stg_bf([T])
            qknorm_epi(ps, 128, 0, l, v)(st)
            store(S_aq[:, hp, t0:t0 + T], v, st, R["aq"])
        if 1 in cfg.pset:
            fm_group(0, 512, epi_aq)

        def epi_ak(col, cw, ps):
            st, v = stg_bf([T])
            qknorm_epi(ps, 64, 1, l, v[0:64, :])(st)
            store(S_kdup[0:64, t0:t0 + T], v[0:64, :], st, R["kdup"])
            store(S_kdup[64:128, t0:t0 + T], v[0:64, :], st, R["kdup"])
        if 2 in cfg.pset:
            fm_group(512, 576, epi_ak)

        def epi_ik(col, cw, ps):
            st, v = stg_bf([T])
            act(lambda e: e.activation(out=v[0:64, :], in_=ps[0:64, :], func=AF.Copy), reads=[ps], writes=[st])
            store(S_ik[:, t0:t0 + T], v[0:64, :], st, R["ik"])
        if 3 in cfg.pset:
            fm_group(896, 960, epi_ik)

        def epi_su(col, cw, ps):
            j = (col - 964) // 128
            st, v = stg_bf([T])
            act(lambda e: e.activation(out=v, in_=ps[:], func=AF.Copy), reads=[ps], writes=[st])
            store(S_su[:, j, t0:t0 + T], v, st, R["su"])
        if 4 in cfg.pset:
            fm_group(964, 1476, epi_su)

        def mk_epi_c(base, gidx, S, rname):
            def epi(col, cw, ps):
                ch = (col - base) // 128
                g = ch // 2
                hp = ch % 2
                d = DIL[g]
                st, v = stg_bf([T])
                if d == 1:
                    qknorm_epi(ps, 128, gidx, l, v)(st)
                    store(S[g][:, hp, 0, t0:t0 + T], v, st, R[rname])
                else:
                    v3 = v.rearrange("q (r s) -> q r s", r=d)
                    qknorm_epi(ps, 128, gidx, l, v3, view=("q (s r) -> q r s", {"r": d}))(st)
                    store(S[g][:, hp, :, t0 // d:(t0 + T) // d], v3, st, R[rname])
            return epi
        if 5 in cfg.pset:
            fm_group(1476, 1988, mk_epi_c(1476, 2, S_cq, "cq"))
            fm_group(1988, 2244, mk_epi_c(1476, 2, S_cq, "cq"))
            fm_group(2244, 2756, mk_epi_c(2244, 3, S_ck, "ck"))
            fm_group(2756, 3012, mk_epi_c(2244, 3, S_ck, "ck"))

        def epi_g(col, cw, ps):
            j = (col - 3780) // 128
            st, v = stg_bf([T])
            act(lambda e: e.activation(out=v, in_=ps[:], func=AF.Sigmoid), reads=[ps], writes=[st])
            store(S_g[:, j, t0:t0 + T], v, st, R["g"])
        for n0 in range(3780, DIN, 512):
            if 6 in cfg.pset:
                fm_group(n0, n0 + 512, epi_g)

        def tm_group(n0, n1, epi):
            sl, wv = wload(Win[:, n0:n1], rw, 8, n1 - n0)
            for sub in range(4):
                ps = psA()
                for k in range(8):
                    mm(ps[:, 0:n1 - n0], hb[:, k, sub * 128:(sub + 1) * 128], wv[:, k, :], k == 0, k == 7, [sl, hbr[k]], [ps])
                epi(sub, ps)

        def epi_tm1(sub, ps):
            st = nxt("st", STG)
            act(lambda e: e.activation(out=st[:, 0:512], in_=ps[:, 0:512], func=AF.Copy), reads=[ps], writes=[st])
            store(S_tm1[:, t0 // 128 + sub, :], st[:, 0:512], st, R["tm1"])
        if 7 in cfg.pset:
            tm_group(576, 1088, epi_tm1)

        def epi_cv(col, cw, ps):
            ch = (col - 3012) // 128
            g = ch // 2
            hp = ch % 2
            d = DIL[g]
            st, v = stg_bf([T])
            if d == 1:
                act(lambda e: e.activation(out=v, in_=ps[:], func=AF.Copy), reads=[ps], writes=[st])
                store(S_cv[g][:, hp, 0, t0:t0 + T], v, st, R["cv"])
            else:
                v3 = v.rearrange("q (r s) -> q r s", r=d)
                act(lambda e: e.activation(out=v3, in_=ps[:].rearrange("q (s r) -> q r s", r=d), func=AF.Copy), reads=[ps], writes=[st])
                store(S_cv[g][:, hp, :, t0 // d:(t0 + T) // d], v3, st, R["cv"])
        if 8 in cfg.pset:
            fm_group(3012, 3524, epi_cv)
            fm_group(3524, 3780, epi_cv)

    def load_x_first(t0):
        load(xin[:], X[t0:t0 + T, :].rearrange("(s q) f -> q s f", q=128), xin)
        for c in range(8):
            ps = psA()
            for s in range(4):
                pe(lambda e, ps=ps, s=s, c=c: e.transpose(out=ps[:, s * 128:(s + 1) * 128], in_=xin[:, s, c * 128:(c + 1) * 128],
                                                          identity=ident32[:]), reads=[xin, ident32], writes=[ps])
            act(lambda e, ps=ps, c=c: e.activation(out=xt[:, c, :], in_=ps[:], func=AF.Copy), reads=[ps], writes=[xtr[c]])

    def store_x_last(t0):
        for s in range(4):
            for c2 in range(2):
                ps = psA()
                for c in range(4):
                    cc = c2 * 4 + c
                    pe(lambda e, ps=ps, s=s, c=c, cc=cc: e.transpose(out=ps[:, c * 128:(c + 1) * 128], in_=xt[:, cc, s * 128:(s + 1) * 128],
                                                                     identity=ident32[:]), reads=[xtr[cc], ident32], writes=[ps])
                act(lambda e, ps=ps, s=s, c2=c2: e.activation(out=xin[:, s, c2 * 512:(c2 + 1) * 512], in_=ps[:], func=AF.Copy),
                    reads=[ps], writes=[xin])
        return store(OUT[t0:t0 + T, :].rearrange("(s q) f -> q s f", q=128), xin[:], xin, R["out"])


    NIT = 16

    def phase_dsa(l):
        with nc.reset_on_exit():
            kidx = p.sb("kidx", [64, NT], BF16)
            kdup = p.sb("kdup", [128, NT], BF16)
            vp = p.sb("vp", [128, NQB, 65], BF16)
            sc = p.sb("sc", [128, NT], F32)
            scr_ = [Res("sc%d" % i) for i in range((NT + 511) // 512)]
            msk = p.sb("msk", [128, NT], BF16)
            junk = msk
            zt = p.sb("zt", [128, NT], BF16)
            cum = p.sb("cum", [128, NT], mybir.dt.float16)
            SM = {k: p.sb("sm_" + k, [128, 1], F32) for k in ["cpos", "t1", "c1", "c2", "tie", "nd0", "need"]}
            tri = p.sb("tri", [128, 128], F32)
            sel = p.sb("sel", [65, 64], F32)
            nb = p.sb("nb", [128, 1], F32)
            TMQ = [p.sb("tmq%d" % i, [128, 512], F32) for i in range(2)]
            QF = [p.sb("qf%d" % i, [128, 4, 128], BF16) for i in range(2)]
            WAB = [p.sb("wab%d" % i, [128, 4], F32) for i in range(2)]
            QLN = [p.sb("qln%d" % i, [128, 64], F32) for i in range(2)]
            QS = [p.sb("qs%d" % i, [128, 5, 64], BF16) for i in range(2)]
            QST = [p.sb("qst%d" % i, [64, 5, 128], BF16) for i in range(2)]
            LO = [p.sb("lo%d" % i, [128, 1], F32) for i in range(2)]
            HI = [p.sb("hi%d" % i, [128, 1], F32) for i in range(2)]
            mid = p.sb("mid", [128, 1], F32)
            tq = p.sb("tq", [128, 1], F32)
            HH = p.sb("HH", [128, NIT], F32)
            pw2 = p.sb("pw2", [128, NIT], F32)
            for i_ in range(NIT):
                pool(lambda e, i_=i_: e.memset(pw2[:, i_:i_ + 1], 2.0 ** (-(i_ + 1))), writes=[pw2])
            cn = p.sb("cn", [128, 1], F32)
            pge = p.sb("pge", [128, 1], mybir.dt.int32)
            plt = p.sb("plt", [128, 1], mybir.dt.int32)
            EE = [p.sb("ee%d" % i, [128, 1024], BF16) for i in range(3)]
            EM = [p.sb("em%d" % i, [128, 1024], BF16) for i in range(3)]
            osb = p.sb("osb", [65, 1024], F32)
            rec = p.sb("rec", [64, 1024], F32)
            YA = [p.sb("yast%d" % i, [64, 1024], BF16) for i in range(2)]
            XH = [p.sb("xh%d" % i, [128, 512], F32) for i in range(3)]

            load(kidx[:], S_ik, kidx, reads=[R["ik"]])
            load(kdup[:], S_kdup, kdup, reads=[R["kdup"]])
            pool(lambda e: e.memset(vp[:, :, 64:65], 1.0), writes=[vp])
            vf = p.sb("vf", [128, NQB, 64], F32)
            load(vf[:], S_tm1[:, :, 0:64], vf, reads=[R["tm1"]])
            act(lambda e: e.activation(out=vp[:, :, 0:64], in_=vf[:], func=AF.Copy), reads=[vf], writes=[vp])
            pool(lambda e: e.memset(tri[:], 0.0), writes=[tri])
            pool(lambda e: e.affine_select(out=tri[:], in_=tri[:], pattern=[[-1, 128]], compare_op=ALU.is_ge,
                                           fill=-1e30, base=0, channel_multiplier=1), reads=[tri], writes=[tri])
            pool(lambda e: e.memset(sel[:], 0.0), writes=[sel])
            pool(lambda e: e.memset(sel[64:65, :], 1.0), writes=[sel])
            pool(lambda e: e.memset(nb[:], -8.0), writes=[nb])

            pl = PS[0]
            PH = [PS[1], PS[2]]
            ST = [PS[3], PS[4]]
            STB = [[PS[3], PS[4]], [PS[1], PS[2]]]
            PTR = [PS[5], PS[0]]
            ptr = PS[5]
            ptr_b = ptr.t[:].bitcast(BF16)
            OO = [PS[6], PS[7]]
            hcnt = [0]

            for qb in range(NQB):
                t0 = qb * 128
                N = t0 + 128
                b = qb % 2
                tmq, qf, wab, qln, qs, qst, lo, hi = TMQ[b], QF[b], WAB[b], QLN[b], QS[b], QST[b], LO[b], HI[b]
                load(tmq[:], S_tm1[:, qb, :], tmq, reads=[R["tm1"]])
                load(qf[:], S_aq[:, :, t0:t0 + 128], qf, reads=[R["aq"]])
                dve(lambda e, qs=qs, tmq=tmq: e.tensor_copy(out=qs[:, 0:4, :].rearrange("q h d -> q (h d)"), in_=tmq[:, 64:320]),
                    reads=[tmq], writes=[qs])
                for v in range(4):
                    pe(lambda e, v=v, qs=qs: e.transpose(out=ptr_b[0:64, v * 128:(v + 1) * 128], in_=qs[:, v, :], identity=identb[:]),
                       reads=[qs, identb], writes=[ptr])
                act(lambda e, qst=qst: e.activation(out=qst[:, 0:4, :].rearrange("q v t -> q (v t)"), in_=ptr_b[0:64, 0:512], func=AF.Copy),
                    reads=[ptr], writes=[qst])
                nk = (N + 511) // 512
                for kc in range(nk):
                    w = min(512, N - kc * 512)
                    ksl = kidx[:, kc * 512:kc * 512 + w]
                    scs = sc[:, kc * 512:kc * 512 + w]
                    for h in range(4):
                        ph = PH[hcnt[0] % 2]
                        xh = XH[hcnt[0] % 3]
                        hcnt[0] += 1
                        mm(ph[:, 0:w], qst[:, h, :], ksl, True, True, [qst, kidx], [ph])
                        if h == 0:
                            dve(lambda e, ph=ph, scs=scs, w=w, tmq=tmq: e.tensor_scalar(out=scs, in0=ph[:, 0:w], scalar1=0.0, scalar2=tmq[:, 384:385],
                                                                                        op0=ALU.max, op1=ALU.mult),
                                reads=[ph, tmq], writes=[scr_[kc]])
                        else:
                            dve(lambda e, ph=ph, xh=xh, w=w, tmq=tmq, h=h: e.tensor_scalar(out=xh[:, 0:w], in0=ph[:, 0:w], scalar1=0.0,
                                                                                           scalar2=tmq[:, 384 + h:385 + h], op0=ALU.max, op1=ALU.mult),
                                reads=[ph, tmq], writes=[xh])
                            pool(lambda e, xh=xh, scs=scs, w=w: e.tensor_tensor(out=scs, in0=xh[:, 0:w], in1=scs, op=ALU.add),
                                 reads=[xh, scr_[kc]], writes=[scr_[kc]])
                allsc = scr_[0:nk]
                if qb >= 2:
                    dve(lambda e, N=N, hi=hi: e.tensor_reduce(out=hi[:], in_=sc[:, 0:N], axis=AX.X, op=ALU.max), reads=allsc, writes=[hi])
                    dve(lambda e, N=N, lo=lo: e.tensor_reduce(out=lo[:], in_=sc[:, 0:N], axis=AX.X, op=ALU.min), reads=allsc, writes=[lo])
                else:
                    pool(lambda e, lo=lo: e.memset(lo[:], -1e29), writes=[lo])
                dve(lambda e, t0=t0: e.tensor_tensor(out=sc[:, t0:t0 + 128], in0=sc[:, t0:t0 + 128], in1=tri[:], op=ALU.add),
                    reads=[scr_[nk - 1], tri], writes=[scr_[nk - 1]])
                if qb >= 2:
                    pool(lambda e, N=N: e.tensor_single_scalar(out=zt[:, 0:N], in_=sc[:, 0:N], scalar=0.0, op=ALU.is_equal),
                         reads=allsc, writes=[zt])
                    dve(lambda e, N=N: e.tensor_tensor_scan(out=cum[:, 0:N], data0=zt[:, 0:N], data1=zt[:, 0:N], initial=0.0,
                                                            op0=ALU.add, op1=ALU.max), reads=[zt], writes=[cum])
                    dve(lambda e, N=N: e.tensor_scalar(out=junk[:, 0:N], in0=sc[:, 0:N], scalar1=0.0, scalar2=None,
                                                       op0=ALU.is_gt, op1=ALU.add, accum_out=SM["cpos"][:]),
                        reads=allsc, writes=[junk, SM["cpos"]])
                    dve(lambda e, N=N: e.tensor_tensor(out=SM["t1"][:], in0=SM["cpos"][:], in1=cum[:, N - 1:N], op=ALU.add),
                        reads=[SM["cpos"], cum], writes=[SM["t1"]])
                    dve(lambda e: e.tensor_single_scalar(out=SM["c1"][:], in_=SM["cpos"][:], scalar=255.5, op=ALU.is_lt),
                        reads=[SM["cpos"]], writes=[SM["c1"]])
                    dve(lambda e: e.tensor_single_scalar(out=SM["c2"][:], in_=SM["t1"][:], scalar=255.5, op=ALU.is_ge),
                        reads=[SM["t1"]], writes=[SM["c2"]])
                    dve(lambda e: e.tensor_tensor(out=SM["tie"][:], in0=SM["c1"][:], in1=SM["c2"][:], op=ALU.mult),
                        reads=[SM["c1"], SM["c2"]], writes=[SM["tie"]])
                    dve(lambda e: e.tensor_scalar(out=SM["nd0"][:], in0=SM["cpos"][:], scalar1=-1.0, scalar2=256.0 - 16384.0,
                                                  op0=ALU.mult, op1=ALU.add), reads=[SM["cpos"]], writes=[SM["nd0"]])
                    dve(lambda e: e.tensor_scalar(out=SM["need"][:], in0=SM["nd0"][:], scalar1=SM["tie"][:, 0:1], scalar2=16384.0,
                                                  op0=ALU.mult, op1=ALU.add), reads=[SM["nd0"], SM["tie"]], writes=[SM["need"]])
                    pool(lambda e, N=N: e.tensor_tensor(out=cum[:, 0:N], in0=cum[:, 0:N], in1=zt[:, 0:N], op=ALU.mult),
                         reads=[cum, zt], writes=[cum])
                    pool(lambda e, N=N: e.tensor_scalar(out=zt[:, 0:N], in0=cum[:, 0:N], scalar1=SM["need"][:, 0:1], scalar2=None,
                                                        op0=ALU.is_gt), reads=[cum, SM["need"]], writes=[zt])
                    dve(lambda e, lo=lo, hi=hi: e.tensor_tensor(out=hi[:], in0=hi[:], in1=lo[:], op=ALU.subtract), reads=[hi, lo], writes=[hi])
                    dve(lambda e, hi=hi: e.tensor_scalar(out=HH[:], in0=pw2[:], scalar1=hi[:, 0:1], scalar2=None, op0=ALU.mult),
                        reads=[pw2, hi], writes=[HH])
                    dve(lambda e, lo=lo: e.tensor_tensor(out=mid[:], in0=lo[:], in1=HH[:, 0:1], op=ALU.add), reads=[lo, HH], writes=[mid])
                    for it in range(NIT):
                        dve(lambda e, N=N: e.tensor_scalar(out=junk[:, 0:N], in0=sc[:, 0:N], scalar1=mid[:, 0:1], scalar2=None,
                                                           op0=ALU.is_ge, op1=ALU.add, accum_out=cn[:]),
                            reads=allsc + [mid], writes=[junk, cn])
                        dve(lambda e, it=it: e.tensor_scalar(out=tq[:], in0=cn[:], scalar1=255.5, scalar2=HH[:, it:it + 1],
                                                             op0=ALU.is_ge, op1=ALU.mult), reads=[cn, HH], writes=[tq])
                        if it < NIT - 1:
                            dve(lambda e, it=it: e.scalar_tensor_tensor(out=mid[:], in0=tq[:], scalar=HH[:, it + 1:it + 2], in1=mid[:],
                                                                        op0=ALU.subtract, op1=ALU.add), reads=[tq, HH, mid], writes=[mid])
                        else:
                            dve(lambda e, it=it, lo=lo: e.scalar_tensor_tensor(out=lo[:], in0=tq[:], scalar=HH[:, it:it + 1], in1=mid[:],
                                                                               op0=ALU.subtract, op1=ALU.add), reads=[tq, HH, mid], writes=[lo])
                if qb >= 2:
                    dve(lambda e, N=N, lo=lo: e.scalar_tensor_tensor(out=msk[:, 0:N], in0=sc[:, 0:N], scalar=lo[:, 0:1], in1=zt[:, 0:N],
                                                                     op0=ALU.is_ge, op1=ALU.subtract), reads=allsc + [lo, zt], writes=[msk])
                else:
                    dve(lambda e, N=N, lo=lo: e.tensor_scalar(out=msk[:, 0:N], in0=sc[:, 0:N], scalar1=lo[:, 0:1], scalar2=None, op0=ALU.is_ge),
                        reads=allsc + [lo], writes=[msk])
                if cfg.dbg:
                    p.op("sp", lambda e, qb=qb, N=N: e.dma_start(out=D_sc[qb, :, 0:N], in_=sc[:, 0:N]), reads=allsc, writes=[R["out"]], dma=sc, join=True)
                    p.op("sp", lambda e, qb=qb, lo=lo: e.dma_start(out=D_lo[qb, :, 0:1], in_=lo[:], allow_slow_non_contiguous=True), reads=[lo], writes=[R["out"]], dma=lo, join=True)
                    if qb >= 2:
                      p.op("sp", lambda e, qb=qb, hi=hi: e.dma_start(out=D_lo[qb, :, 1:2], in_=hi[:], allow_slow_non_contiguous=True), reads=[hi], writes=[R["out"]], dma=hi, join=True)
                pendA = []
                for c in range(qb + 1):
                    ee = EE[c % 3]
                    em = EM[c % 3]
                    STc = STB[c % 2]
                    pt = PTR[c % 2]
                    pt_b = pt.t[:].bitcast(BF16)
                    pe(lambda e, c=c, pt_b=pt_b: e.transpose(out=pt_b[:, 0:128], in_=msk[:, c * 128:(c + 1) * 128], identity=identb[:]),
                       reads=[msk, identb], writes=[pt])
                    for hl in range(2):
                        mm(STc[hl][:], kdup[hl * 64:(hl + 1) * 64, c * 128:(c + 1) * 128], qf[hl * 64:(hl + 1) * 64, :, :], True, True,
                           [kdup, qf], [STc[hl]])
                        act(lambda e, hl=hl, ee=ee, STc=STc: e.activation(out=ee[:, hl * 512:(hl + 1) * 512], in_=STc[hl][:], func=AF.Exp,
                                                                          bias=nb[:, 0:1], scale=0.125), reads=[STc[hl], nb], writes=[ee])
                    dve(lambda e, ee=ee, em=em, pt_b=pt_b: e.tensor_tensor(out=em[:].rearrange("q (h t) -> q h t", h=8),
                                                                           in0=ee[:].rearrange("q (h t) -> q h t", h=8),
                                                                           in1=pt_b[:, 0:128].unsqueeze(1).broadcast_to([128, 8, 128]), op=ALU.mult),
                        reads=[ee, pt], writes=[em])

                    def tailA(c=c, em=em, qb=qb):
                        for hl in range(2):
                            mm(OO[hl][0:65, :], vp[:, c, :], em[:, hl * 512:(hl + 1) * 512], c == 0, c == qb, [vp, em], [OO[hl]])
                    pendA.append(tailA)
                    if len(pendA) > 1:
                        pendA.pop(0)()
                while pendA:
                    pendA.pop(0)()
                for hl in range(2):
                    act(lambda e, hl=hl: e.activation(out=osb[:, hl * 512:(hl + 1) * 512], in_=OO[hl][0:65, :], func=AF.Copy),
                        reads=[OO[hl]], writes=[osb])
                for hl in range(2):
                    mm(PH[hl][0:64, :], sel[:, :], osb[:, hl * 512:(hl + 1) * 512], True, True, [sel, osb], [PH[hl]])
                    dve(lambda e, hl=hl: e.reciprocal(out=rec[:, hl * 512:(hl + 1) * 512], in_=PH[hl][0:64, :]), reads=[PH[hl]], writes=[rec])
                ya = YA[b]
                dve(lambda e, ya=ya: e.tensor_tensor(out=ya[:], in0=osb[0:64, :], in1=rec[:], op=ALU.mult), reads=[osb, rec], writes=[ya])
                for hl in range(2):
                    store(S_ya.rearrange("d (hp hl) t -> d hl hp t", hl=2)[:, hl, :, t0:t0 + 128],
                          ya[:, hl * 512:(hl + 1) * 512].rearrange("d (hp t) -> d hp t", hp=4), ya, R["ya"])
            p.flush()
        _after_reset(p)


    def phase_s5(l):
        TC = 512
        NTC = NT // TC
        with nc.reset_on_exit():
            def sm(name, shape=(128, 32), dt=F32):
                return p.sb("s5_" + name, list(shape), dt)
            lre, lim, dtt = sm("lre"), sm("lim"), sm("dtt")
            aa, th, rr = sm("aa"), sm("th"), sm("rr")
            ft, kk0, fr0, af0 = sm("ft"), sm("kk0"), sm("fr0"), sm("af0")
            sn, cs = sm("sn"), sm("cs")
            nre, nim, den = sm("nre"), sm("nim"), sm("den")
            cre, cim, ta, tb = sm("cre"), sm("cim"), sm("ta"), sm("tb")
            CB, CC = sm("CB"), sm("CC")
            hpi = sm("hpi", (128, 1))
            sgn1 = sm("sgn1", (128, 1))
            rowmask = sm("rowmask", (128, 8))
            colmask = sm("colmask", (128, 8, 128))
            dcol = sm("dcol", (128, 4))
            X1, X2 = sm("X1", (128, 32, 16)), sm("X2", (128, 32, 16))
            BB, BBs, tmpB = sm("BB", (128, 512)), sm("BBs", (128, 512)), sm("tmpB", (128, 512))
            Cin, Cin2 = sm("Cin", (128, 2, 64)), sm("Cin2", (128, 2, 64))
            LB = sm("LB", (128, 8, 2, 128), BF16)
            LC = sm("LC", (128, 8, 2, 128), BF16)
            uj = sm("uj", (128, NT), BF16)
            iota1, onesT = sm("iota1", (128, TC)), sm("onesT", (128, TC))
            carry = sm("carry", (128, 32))
            NB = 4
            TT = [sm("tt%d" % i, (128, TC)) for i in range(NB)]
            KK = [sm("kk%d" % i, (128, TC)) for i in range(NB)]
            FR = [sm("fr%d" % i, (128, TC)) for i in range(NB)]
            AFR = [sm("afr%d" % i, (128, TC)) for i in range(NB)]
            SN = [sm("sn%d" % i, (128, TC)) for i in range(NB)]
            CS = [sm("cs%d" % i, (128, TC)) for i in range(NB)]
            RT = [sm("rt%d" % i, (128, TC)) for i in range(NB)]
            T1 = [sm("t1%d" % i, (128, TC)) for i in range(NB)]
            T2 = [sm("t2%d" % i, (128, TC)) for i in range(NB)]
            STt = [sm("st%d" % i, (128, TC)) for i in range(NB)]
            Z1 = [sm("z1%d" % i, (128, TC), BF16) for i in range(NB)]
            Z2 = [sm("z2%d" % i, (128, TC), BF16) for i in range(NB)]
            YO = [sm("yo%d" % i, (128, TC)) for i in range(NB)]

            pool(lambda e: e.iota(iota1[:], pattern=[[1, TC]], base=1, channel_multiplier=0, allow_small_or_imprecise_dtypes=True), writes=[iota1])
            pool(lambda e: e.memset(onesT[:], 1.0), writes=[onesT])
            pool(lambda e: e.memset(hpi[:], TWO_PI / 4), writes=[hpi])
            pool(lambda e: e.memset(sgn1[:], 1.0), writes=[sgn1])
            pool(lambda e: e.memset(sgn1[64:128, :], -1.0), writes=[sgn1])
            pool(lambda e: e.memset(carry[:], 0.0), writes=[carry])
            pool(lambda e: e.memset(rowmask[:], 1.0), writes=[rowmask])
            pool(lambda e: e.affine_select(out=rowmask[:], in_=rowmask[:], pattern=[[-16, 8]], compare_op=ALU.is_ge, fill=0.0,
                                           base=0, channel_multiplier=1), reads=[rowmask], writes=[rowmask])
            pool(lambda e: e.affine_select(out=rowmask[:], in_=rowmask[:], pattern=[[16, 8]], compare_op=ALU.is_ge, fill=0.0,
                                           base=15, channel_multiplier=-1), reads=[rowmask], writes=[rowmask])
            pool(lambda e: e.memset(colmask[:], 0.0), writes=[colmask])
            for gl in range(8):
                pool(lambda e, gl=gl: e.memset(colmask[:, gl, 16 * gl:16 * gl + 16], 1.0), writes=[colmask])
            for hf in range(2):
                load(lre[hf * 64:(hf + 1) * 64, :], W["s5_lam_re"][l].rearrange("g q -> q g"), lre, slow=True, join=True)
                load(lim[hf * 64:(hf + 1) * 64, :], W["s5_lam_im"][l].rearrange("g q -> q g"), lim, slow=True, join=True)
            load(dtt[:], W["s5_log_dt"][l].partition_broadcast(128), dtt, slow=True)
            load(dcol[:], W["s5_d"][l].rearrange("g c -> (g c)").rearrange("(j q) -> q j", q=128), dcol, slow=True)
            bre = W["s5_b_re"][l].rearrange("g q c -> q g c")
            bim = W["s5_b_im"][l].rearrange("g q c -> q g c")
            load(X1[0:64], bre, X1, join=True)
            load(X1[64:128], bim, X1, join=True)
            load(X2[0:64], bim, X2, join=True)
            load(X2[64:128], bre, X2, join=True)

            def tt_(o, a, b, op):
                dve(lambda e: e.tensor_tensor(out=o[:], in0=a[:], in1=b[:], op=op), reads=[a, b], writes=[o])

            def ts_(o, a, s1, op0, s2=None, op1=None):
                if op1 is None:
                    dve(lambda e: e.tensor_scalar(out=o[:], in0=a[:], scalar1=s1, scalar2=None, op0=op0), reads=[a], writes=[o])
                else:
                    dve(lambda e: e.tensor_scalar(out=o[:], in0=a[:], scalar1=s1, scalar2=s2, op0=op0, op1=op1), reads=[a], writes=[o])

            act(lambda e: e.activation(out=dtt[:], in_=dtt[:], func=AF.Exp), reads=[dtt], writes=[dtt])
            ts_(lre, lre, -1e-4, ALU.min)
            tt_(aa, lre, dtt, ALU.mult)
            tt_(th, lim, dtt, ALU.mult)
            act(lambda e: e.activation(out=rr[:], in_=aa[:], func=AF.Exp), reads=[aa], writes=[rr])
            ts_(ft, th, 1.0 / TWO_PI, ALU.mult)
            ts_(kk0, ft, MAGIC, ALU.add)
            ts_(kk0, kk0, MAGIC, ALU.subtract)
            tt_(fr0, ft, kk0, ALU.subtract)
            act(lambda e: e.activation(out=sn[:], in_=fr0[:], func=AF.Sin, scale=TWO_PI), reads=[fr0], writes=[sn])
            act(lambda e: e.activation(out=af0[:], in_=fr0[:], func=AF.Abs), reads=[fr0], writes=[af0])
            act(lambda e: e.activation(out=cs[:], in_=af0[:], func=AF.Sin, scale=-TWO_PI, bias=hpi[:, 0:1]), reads=[af0, hpi], writes=[cs])
            tt_(nre, rr, cs, ALU.mult)
            ts_(nre, nre, -1.0, ALU.add)
            tt_(nim, rr, sn, ALU.mult)
            tt_(den, lre, lre, ALU.mult)
            tt_(ta, lim, lim, ALU.mult)
            tt_(den, den, ta, ALU.add)
            dve(lambda e: e.reciprocal(out=den[:], in_=den[:]), reads=[den], writes=[den])
            tt_(ta, nre, lre, ALU.mult)
            tt_(tb, nim, lim, ALU.mult)
            tt_(cre, ta, tb, ALU.add)
            tt_(cre, cre, den, ALU.mult)
            tt_(ta, nim, lre, ALU.mult)
            tt_(tb, nre, lim, ALU.mult)
            tt_(cim, ta, tb, ALU.subtract)
            tt_(cim, cim, den, ALU.mult)
            dve(lambda e: e.tensor_scalar(out=CB[:], in0=cim[:], scalar1=sgn1[:, 0:1], scalar2=-1.0, op0=ALU.mult, op1=ALU.mult),
                reads=[cim, sgn1], writes=[CB])
            dve(lambda e: e.tensor_scalar(out=CC[:], in0=cre[:], scalar1=sgn1[:, 0:1], scalar2=None, op0=ALU.mult),
                reads=[cre, sgn1], writes=[CC])

            def bc(t_):
                return t_[:].unsqueeze(2).broadcast_to([128, 32, 16])

            def v3(t_):
                return t_[:].rearrange("q (g c) -> q g c", c=16)
            dve(lambda e: e.tensor_tensor(out=v3(BB), in0=X1[:], in1=bc(cre), op=ALU.mult), reads=[X1, cre], writes=[BB])
            dve(lambda e: e.tensor_tensor(out=v3(tmpB), in0=X2[:], in1=bc(CB), op=ALU.mult), reads=[X2, CB], writes=[tmpB])
            tt_(BB, BB, tmpB, ALU.add)
            dve(lambda e: e.tensor_tensor(out=v3(BBs), in0=X2[:], in1=bc(CC), op=ALU.mult), reads=[X2, CC], writes=[BBs])
            dve(lambda e: e.tensor_tensor(out=v3(tmpB), in0=X1[:], in1=bc(cim), op=ALU.mult), reads=[X1, cim], writes=[tmpB])
            tt_(BBs, BBs, tmpB, ALU.add)

            cflat_re = W["s5_c_re"][l].rearrange("g c q -> (g c) q")
            cflat_im = W["s5_c_im"][l].rearrange("g c q -> (g c) q")
            bcnt = [0]
            ycnt = [0]
            pend = []
            for j in range(4):
                load(uj[:], S_su[:, j, :], uj, reads=[R["su"]])
                load(Cin[:, 0, :], cflat_re[j * 128:(j + 1) * 128, :], Cin, join=True)
                load(Cin[:, 1, :], cflat_im[j * 128:(j + 1) * 128, :], Cin, join=True)
                load(Cin2[:, 0, :], cflat_im[j * 128:(j + 1) * 128, :], Cin2, join=True)
                load(Cin2[:, 1, :], cflat_re[j * 128:(j + 1) * 128, :], Cin2, join=True)
                for var, src in ((0, BB), (1, BBs)):
                    ps = PS[var]
                    pe(lambda e, ps=ps, src=src, j=j: e.transpose(out=ps[:, 0:128], in_=src[:, j * 128:(j + 1) * 128], identity=ident32[:]),
                       reads=[src, ident32], writes=[ps])
                    for gl in range(8):
                        dve(lambda e, ps=ps, gl=gl, var=var: e.tensor_scalar(out=LB[:, gl, var, :], in0=ps[:, 0:128], scalar1=rowmask[:, gl:gl + 1],
                                                                             scalar2=None, op0=ALU.mult), reads=[ps, rowmask], writes=[LB])
                for var, src, sc_ in ((0, Cin, 1.0), (1, Cin2, -1.0)):
                    ps = PS[2 + var]
                    pe(lambda e, ps=ps, src=src: e.transpose(out=ps[:, 0:128], in_=src[:].rearrange("q a b -> q (a b)"), identity=ident32[:]),
                       reads=[src, ident32], writes=[ps])
                    for gl in range(8):
                        if var == 0:
                            dve(lambda e, ps=ps, gl=gl: e.scalar_tensor_tensor(out=LC[:, gl, 0, :], in0=ps[:, 0:128], scalar=sgn1[:, 0:1],
                                                                               in1=colmask[:, gl, :], op0=ALU.mult, op1=ALU.mult),
                                reads=[ps, sgn1, colmask], writes=[LC])
                        else:
                            dve(lambda e, ps=ps, gl=gl: e.scalar_tensor_tensor(out=LC[:, gl, 1, :], in0=ps[:, 0:128], scalar=-1.0,
                                                                               in1=colmask[:, gl, :], op0=ALU.mult, op1=ALU.mult),
                                reads=[ps, colmask], writes=[LC])
                for tc in range(NTC):
                    yps = PS[6 + ycnt[0] % 2]
                    ycnt[0] += 1
                    usl = uj[:, tc * TC:(tc + 1) * TC]
                    for gl in range(8):
                        g = 8 * j + gl
                        b = bcnt[0] % NB
                        bcnt[0] += 1
                        tt, kk, fr, afr, snT, csT, rt = TT[b], KK[b], FR[b], AFR[b], SN[b], CS[b], RT[b]
                        t1, t2, st, z1, z2 = T1[b], T2[b], STt[b], Z1[b], Z2[b]
                        pool(lambda e, tt=tt, g=g, tc=tc: e.tensor_scalar(out=tt[:], in0=iota1[:], scalar1=float(tc * TC), scalar2=ft[:, g:g + 1],
                                                                          op0=ALU.add, op1=ALU.mult), reads=[iota1, ft], writes=[tt])
                        pool(lambda e, tt=tt, kk=kk: e.tensor_scalar(out=kk[:], in0=tt[:], scalar1=MAGIC, scalar2=None, op0=ALU.add),
                             reads=[tt], writes=[kk])
                        pool(lambda e, kk=kk: e.tensor_scalar(out=kk[:], in0=kk[:], scalar1=MAGIC, scalar2=None, op0=ALU.subtract),
                             reads=[kk], writes=[kk])
                        pool(lambda e, tt=tt, kk=kk, fr=fr: e.tensor_tensor(out=fr[:], in0=tt[:], in1=kk[:], op=ALU.subtract),
                             reads=[tt, kk], writes=[fr])
                        act(lambda e, fr=fr, snT=snT: e.activation(out=snT[:], in_=fr[:], func=AF.Sin, scale=TWO_PI), reads=[fr], writes=[snT])
                        act(lambda e, fr=fr, afr=afr: e.activation(out=afr[:], in_=fr[:], func=AF.Abs), reads=[fr], writes=[afr])
                        act(lambda e, afr=afr, csT=csT: e.activation(out=csT[:], in_=afr[:], func=AF.Sin, scale=-TWO_PI, bias=hpi[:, 0:1]),
                            reads=[afr, hpi], writes=[csT])
                        act(lambda e, rt=rt, g=g: e.activation(out=rt[:], in_=onesT[:], func=AF.Copy, scale=rr[:, g:g + 1]),
                            reads=[onesT, rr], writes=[rt])
                        pa = PS[2 * (b % 2)]
                        pb = PS[2 * (b % 2) + 1]
                        mm(pa[:], LB[:, gl, 0, :], usl, True, True, [LB, uj], [pa])
                        mm(pb[:], LB[:, gl, 1, :], usl, True, True, [LB, uj], [pb])
                        dve(lambda e, t1=t1, pa=pa, csT=csT: e.tensor_tensor(out=t1[:], in0=pa[:], in1=csT[:], op=ALU.mult), reads=[pa, csT], writes=[t1])
                        dve(lambda e, t2=t2, pb=pb, snT=snT: e.tensor_tensor(out=t2[:], in0=pb[:], in1=snT[:], op=ALU.mult), reads=[pb, snT], writes=[t2])
                        dve(lambda e, t1=t1, t2=t2: e.tensor_tensor(out=t1[:], in0=t1[:], in1=t2[:], op=ALU.add), reads=[t1, t2], writes=[t1])
                        dve(lambda e, st=st, rt=rt, t1=t1, g=g: e.tensor_tensor_scan(out=st[:], data0=rt[:], data1=t1[:], initial=carry[:, g:g + 1],
                                                                                     op0=ALU.mult, op1=ALU.add), reads=[rt, t1, carry], writes=[st])
                        act(lambda e, st=st, g=g: e.activation(out=carry[:, g:g + 1], in_=st[:, TC - 1:TC], func=AF.Copy), reads=[st], writes=[carry])
                        pool(lambda e, z1=z1, st=st, csT=csT: e.tensor_tensor(out=z1[:], in0=st[:], in1=csT[:], op=ALU.mult), reads=[st, csT], writes=[z1])
                        pool(lambda e, z2=z2, st=st, snT=snT: e.tensor_tensor(out=z2[:], in0=st[:], in1=snT[:], op=ALU.mult), reads=[st, snT], writes=[z2])
                        def tail(yps=yps, gl=gl, z1=z1, z2=z2, usl=usl, j=j, tc=tc, yo=YO[ycnt[0] % 2]):
                            mm(yps[:], LC[:, gl, 0, :], z1[:], gl == 0, False, [LC, z1], [yps])
                            mm(yps[:], LC[:, gl, 1, :], z2[:], False, gl == 7, [LC, z2], [yps])
                            if gl == 7:
                                dve(lambda e: e.scalar_tensor_tensor(out=yo[:], in0=usl, scalar=dcol[:, j:j + 1], in1=yps[:],
                                                                     op0=ALU.mult, op1=ALU.add), reads=[uj, dcol, yps], writes=[yo])
                                store(S_ys5[:, j, tc * TC:(tc + 1) * TC], yo[:], yo, R["ys5"])
                        pend.append(tail)
                        if len(pend) > 2:
                            pend.pop(0)()
                while pend:
                    pend.pop(0)()
            p.flush()
        _after_reset(p)


    def phase_dil(l):
        NU = NT // 128
        with nc.reset_on_exit():
            QT = [p.sb("dq%d" % i, [128, NT], BF16) for i in range(2)]
            KT = [p.sb("dk%d" % i, [128, NT], BF16) for i in range(2)]
            VP = [p.sb("dv%d" % i, [128, NU, 65], BF16) for i in range(2)]
            VT = [p.sb("dvt%d" % i, [128, NT], BF16) for i in range(2)]
            ACC = p.sb("dacc", [65, NT], F32)
            band = p.sb("band", [128, 2, 128], BF16)
            bandf = p.sb("bandf", [128, 2, 128], F32)
            sel = p.sb("dsel", [65, 64], F32)
            nb = p.sb("dnb", [128, 1], F32)
            EE = [p.sb("dee%d" % i, [128, 256], BF16) for i in range(4)]
            EM = [p.sb("dem%d" % i, [128, 256], BF16) for i in range(4)]
            rec = p.sb("drec", [64, 512], F32)
            YC = [p.sb("dyc%d" % i, [64, 512], BF16) for i in range(2)]
            pool(lambda e: e.memset(bandf[:], 1.0), writes=[bandf])
            pool(lambda e: e.affine_select(out=bandf[:, 0, :], in_=bandf[:, 0, :], pattern=[[-1, 128]], compare_op=ALU.is_ge, fill=0.0,
                                           base=0, channel_multiplier=1), reads=[bandf], writes=[bandf])
            pool(lambda e: e.affine_select(out=bandf[:, 1, :], in_=bandf[:, 1, :], pattern=[[1, 128]], compare_op=ALU.is_ge, fill=0.0,
                                           base=0, channel_multiplier=-1), reads=[bandf], writes=[bandf])
            pool(lambda e: e.tensor_copy(out=band[:], in_=bandf[:]), reads=[bandf], writes=[band])
            pool(lambda e: e.memset(sel[:], 0.0), writes=[sel])
            pool(lambda e: e.memset(sel[64:65, :], 1.0), writes=[sel])
            pool(lambda e: e.memset(nb[:], -8.0), writes=[nb])
            for t_ in VP:
                pool(lambda e, t_=t_: e.memset(t_[:, :, 64:65], 1.0), writes=[t_])
            lc = [0]
            ec = [0]
            pend = []
            for hs in range(4):
                hp, hl = hs // 2, hs % 2
                psl = slice(hl * 64, (hl + 1) * 64)
                for g in range(3):
                    d = DIL[g]
                    nblk = NT // (d * 128)
                    qt, kt, vp = QT[lc[0] % 2], KT[lc[0] % 2], VP[lc[0] % 2]
                    lc[0] += 1
                    load(qt[psl, :], S_cq[g][psl, hp, :, :].rearrange("q r s -> q (r s)"), qt, reads=[R["cq"]])
                    load(kt[psl, :], S_ck[g][psl, hp, :, :].rearrange("q r s -> q (r s)"), kt, reads=[R["ck"]])
                    vt = VT[(lc[0] - 1) % 2]
                    load(vt[:, :], S_cv[g][:, hp, :, :].rearrange("q r s -> q (r s)"), vt, reads=[R["cv"]])
                    v3_ = vt[:].rearrange("q (r s) -> q r s", r=d)
                    for u in range(d * nblk):
                        r_, n_ = u // nblk, u % nblk
                        ptv = PS[6 + u % 2]
                        ptv_b = ptv.t[:].bitcast(BF16)
                        pe(lambda e, ptv_b=ptv_b, r_=r_, n_=n_, v3_=v3_: e.transpose(out=ptv_b[:, 0:128], in_=v3_[:, r_, n_ * 128:(n_ + 1) * 128],
                                                                                    identity=identb[:]), reads=[vt, identb], writes=[ptv])
                        act(lambda e, ptv_b=ptv_b, u=u, vp=vp, hl=hl: e.activation(out=vp[:, u, 0:64], in_=ptv_b[:, hl * 64:(hl + 1) * 64], func=AF.Copy),
                            reads=[ptv], writes=[vp])
                    if cfg.dbg and hs == 0 and g == 1:
                        p.op("sp", lambda e, vp=vp: e.dma_start(out=D_vp, in_=vp[:]), reads=[vp], writes=[R["out"]], dma=vp, join=True)
                    q3 = qt[:].rearrange("q (r s) -> q r s", r=d)
                    k3 = kt[:].rearrange("q (r s) -> q r s", r=d)
                    acc3 = ACC[:].rearrange("q (s r) -> q r s", r=d)
                    BU = min(4, nblk)
                    for r in range(d):
                        for n0 in range(0, nblk, BU):
                            ops_ = PS[4 + (ec[0] // BU) % 2]
                            for n in range(n0, n0 + BU):
                                u = r * nblk + n
                                stp = PS[ec[0] % 4]
                                ee = EE[ec[0] % 4]
                                em = EM[ec[0] % 4]
                                ec[0] += 1
                                qa = q3[psl, r, n * 128:(n + 1) * 128]
                                c0 = 0 if n > 0 else 128
                                if n > 0:
                                    mm(stp[:, 0:128], k3[psl, r, (n - 1) * 128:n * 128], qa, True, True, [kt, qt], [stp])
                                mm(stp[:, 128:256], k3[psl, r, n * 128:(n + 1) * 128], qa, True, True, [kt, qt], [stp])
                                act(lambda e, ee=ee, stp=stp, c0=c0: e.activation(out=ee[:, c0:256], in_=stp[:, c0:256], func=AF.Exp,
                                                                                 bias=nb[:, 0:1], scale=0.125), reads=[stp, nb], writes=[ee])
                                dve(lambda e, ee=ee, em=em, c0=c0: e.tensor_tensor(out=em[:, c0:256], in0=ee[:, c0:256],
                                                                                   in1=band[:].rearrange("q a b -> q (a b)")[:, c0:256], op=ALU.mult),
                                    reads=[ee, band], writes=[em])
                                oc = (n - n0) * 128
                                last = (n == n0 + BU - 1)
                                dst = acc3[:, r, n0 * 128:(n0 + BU) * 128]

                                def tail(n=n, u=u, oc=oc, ops_=ops_, em=em, vp=vp, last=last, dst=dst, g=g, BU=BU):
                                    if n > 0:
                                        mm(ops_[0:65, oc:oc + 128], vp[:, u - 1, :], em[:, 0:128], True, False, [vp, em], [ops_])
                                        mm(ops_[0:65, oc:oc + 128], vp[:, u, :], em[:, 128:256], False, True, [vp, em], [ops_])
                                    else:
                                        mm(ops_[0:65, oc:oc + 128], vp[:, u, :], em[:, 128:256], True, True, [vp, em], [ops_])
                                    if last:
                                        if g == 0:
                                            act(lambda e: e.activation(out=dst, in_=ops_[0:65, 0:BU * 128], func=AF.Copy), reads=[ops_], writes=[ACC])
                                        else:
                                            dve(lambda e: e.tensor_tensor(out=dst, in0=ops_[0:65, 0:BU * 128], in1=dst, op=ALU.add),
                                                reads=[ops_, ACC], writes=[ACC])
                                pend.append(tail)
                                if len(pend) > 2:
                                    pend.pop(0)()
                    while pend:
                        pend.pop(0)()
                for c in range(NT // 512):
                    bcp = PS[6 + c % 2]
                    mm(bcp[0:64, :], sel[:, :], ACC[:, c * 512:(c + 1) * 512], True, True, [sel, ACC], [bcp])
                    dve(lambda e, bcp=bcp: e.reciprocal(out=rec[:], in_=bcp[0:64, :]), reads=[bcp], writes=[rec])
                    yc = YC[c % 2]
                    dve(lambda e, yc=yc, c=c: e.tensor_tensor(out=yc[:], in0=ACC[0:64, c * 512:(c + 1) * 512], in1=rec[:], op=ALU.mult),
                        reads=[ACC, rec], writes=[yc])
                    store(S_yc[:, hs, c * 512:(c + 1) * 512], yc[:], yc, R["yc"])
            p.flush()
        _after_reset(p)

    def token_phase(lpost, lpre):
        nonlocal xt, hb, ab, SQ, SQB, rs, RSD, SG, WS, STG, xin, xtr, hbr, abr
        with nc.reset_on_exit():
            xt = p.sb("xt", [128, 8, T], F32)
            hb = p.sb("hb", [128, 8, T], BF16)
            ab = p.sb("ab", [128, 22, T], BF16)
            SQ = [p.sb("sq%d" % i, [128, T], F32) for i in range(2)]
            SQB = [p.sb("sqb%d" % i, [128, T], BF16) for i in range(2)]
            rs = p.sb("rs", [128, T], F32)
            RSD = [p.sb("rsd%d" % i, [128, T], F32) for i in range(2)]
            SG = [p.sb("sg%d" % i, [128, T], F32) for i in range(2)]
            WS = [p.sb("ws%d" % i, [128, 4096], BF16) for i in range(NWS)]
            STG = [p.sb("stg%d" % i, [128, 512], F32) for i in range(NST)]
            xin = p.sb("xin", [128, 4, D], F32)
            xtr = [Res("xt%d" % i) for i in range(8)]
            hbr = [Res("hb%d" % i) for i in range(8)]
            abr = [Res("ab%d" % i) for i in range(22)]
            if lpost is not None:
                ys5t = p.sb("ys5t", [128, 4, T], F32)
                tm_a = p.sb("tm_a", [128, 4, T], F32)
                tm_b = p.sb("tm_b", [128, 4, T], F32)
                yg = p.sb("yg", [128, 4, T], BF16)
                yb = p.sb("yb", [128, 4, T], BF16)
                yat = p.sb("yat", [64, 8, T], BF16)
                yct = p.sb("yct", [64, 4, T], BF16)
                gt = p.sb("gt", [128, 24, T], BF16)
                M1 = [p.sb("m1_%d" % i, [128, T], F32) for i in range(2)]
                M2 = [p.sb("m2_%d" % i, [128, T], F32) for i in range(2)]
                sgb = [p.sb("sgb%d" % i, [128, T], F32) for i in range(2)]
                ybr = [Res("yb%d" % i) for i in range(4)]

            def post_mixer(l, t0):
                tsl = slice(t0, t0 + T)
                p.op("sp", lambda e: e.dma_start(out=xt[:], in_=S_xT[:, :, tsl]), reads=[R["xT"]], writes=xtr, dma=xt)
                load(ys5t[:], S_ys5[:, :, tsl], ys5t, reads=[R["ys5"]])
                load(yat[:], S_ya[:, :, tsl], yat, reads=[R["ya"]])
                load(yct[:], S_yc[:, :, tsl], yct, reads=[R["yc"]])
                load(gt[:], S_g[:, :, tsl], gt, reads=[R["g"]])
                f2 = lambda t_: t_[:].rearrange("q a b -> q (a b)")
                act(lambda e: e.activation(out=f2(tm_a), in_=f2(ys5t), func=AF.Square), reads=[ys5t], writes=[tm_a])
                dve(lambda e: e.tensor_scalar(out=f2(tm_a), in0=f2(tm_a), scalar1=0.044715, scalar2=1.0, op0=ALU.mult, op1=ALU.add),
                    reads=[tm_a], writes=[tm_a])
                dve(lambda e: e.tensor_tensor(out=f2(tm_a), in0=f2(tm_a), in1=f2(ys5t), op=ALU.mult), reads=[tm_a, ys5t], writes=[tm_a])
                act(lambda e: e.activation(out=f2(tm_b), in_=f2(tm_a), func=AF.Sigmoid, scale=1.5957691216057308), reads=[tm_a], writes=[tm_b])
                dve(lambda e: e.tensor_tensor(out=f2(yg), in0=f2(tm_b), in1=f2(ys5t), op=ALU.mult), reads=[tm_b, ys5t], writes=[yg])
                sa, wa_ = wload(WB[l]["s5_glu_a"], WBres[l]["s5_glu_a"], 4, 512)
                sb_, wb_ = wload(WB[l]["s5_glu_b"], WBres[l]["s5_glu_b"], 4, 512)
                for oc in range(4):
                    pa, pb = psA(), psA()
                    for k in range(4):
                        mm(pa[:], wa_[:, k, oc * 128:(oc + 1) * 128], yg[:, k, :], k == 0, k == 3, [sa, yg], [pa])
                    for k in range(4):
                        mm(pb[:], wb_[:, k, oc * 128:(oc + 1) * 128], yg[:, k, :], k == 0, k == 3, [sb_, yg], [pb])
                    sg_ = sgb[oc % 2]
                    act(lambda e, sg_=sg_, pb=pb: e.activation(out=sg_[:], in_=pb[:], func=AF.Sigmoid), reads=[pb], writes=[sg_])
                    dve(lambda e, sg_=sg_, pa=pa, oc=oc: e.tensor_tensor(out=yb[:, oc, :], in0=pa[:], in1=sg_[:], op=ALU.mult),
                        reads=[pa, sg_], writes=[ybr[oc]])
                Wa, Wb, Wc = WB[l]["w_branch_a"], WB[l]["w_branch_b"], WB[l]["w_branch_c"]
                for half in range(2):
                    cs_ = slice(half * 512, (half + 1) * 512)
                    s1, wa = wload(Wa[:, cs_], WBres[l]["w_branch_a"], 8, 512, kp=64)
                    s2, wb = wload(Wb[:, cs_], WBres[l]["w_branch_b"], 4, 512)
                    s3, wc = wload(Wc[:, cs_], WBres[l]["w_branch_c"], 4, 512, kp=64)
                    for oc in range(4):
                        c = half * 4 + oc
                        osl = slice(oc * 128, (oc + 1) * 128)
                        pa, pb, pc = psA(), psA(), psA()
                        for h in range(8):
                            mm(pa[:], wa[:, h, osl], yat[:, h, :], h == 0, h == 7, [s1, yat], [pa])
                        for k in range(4):
                            mm(pb[:], wb[:, k, osl], yb[:, k, :], k == 0, k == 3, [s2, ybr[k]], [pb])
                        for k in range(4):
                            mm(pc[:], wc[:, k, osl], yct[:, k, :], k == 0, k == 3, [s3, yct], [pc])
                        m1, m2 = M1[c % 2], M2[c % 2]
                        dve(lambda e, m1=m1, pa=pa, c=c: e.tensor_tensor(out=m1[:], in0=pa[:], in1=gt[:, c, :], op=ALU.mult), reads=[pa, gt], writes=[m1])
                        dve(lambda e, m2=m2, pb=pb, c=c: e.tensor_tensor(out=m2[:], in0=pb[:], in1=gt[:, 8 + c, :], op=ALU.mult), reads=[pb, gt], writes=[m2])
                        pool(lambda e, m1=m1, m2=m2: e.tensor_tensor(out=m1[:], in0=m1[:], in1=m2[:], op=ALU.add), reads=[m1, m2], writes=[m1])
                        dve(lambda e, m2=m2, pc=pc, c=c: e.tensor_tensor(out=m2[:], in0=pc[:], in1=gt[:, 16 + c, :], op=ALU.mult), reads=[pc, gt], writes=[m2])
                        pool(lambda e, m1=m1, m2=m2, c=c: e.tensor_tensor(out=hb[:, c, :], in0=m1[:], in1=m2[:], op=ALU.add), reads=[m1, m2], writes=[hbr[c]])
                Wo = WB[l]["w_out"]
                for half in range(2):
                    so, wo = wload(Wo[:, half * 512:(half + 1) * 512], WBres[l]["w_out"], 8, 512)
                    for oc in range(4):
                        c = half * 4 + oc
                        ps = psA()
                        for k in range(8):
                            mm(ps[:], wo[:, k, oc * 128:(oc + 1) * 128], hb[:, k, :], k == 0, k == 7, [so, hbr[k]], [ps])
                        dve(lambda e, ps=ps, c=c: e.tensor_tensor(out=xt[:, c, :], in0=ps[:], in1=xt[:, c, :], op=ALU.add), reads=[ps, xtr[c]], writes=[xtr[c]])

            for tt in range(NTT):
                t0 = tt * T
                if lpost is None:
                    load_x_first(t0)
                else:
                    post_mixer(lpost, t0)
                    rmsnorm_fm(2, lpost)
                    ffn(lpost, "ffn2")
                if lpre is not None and cfg.sub < 9:
                    if cfg.sub >= 2:
                        rmsnorm_fm(0, lpre)
                    if cfg.sub >= 3:
                        ffn(lpre, "ffn1")
                    if cfg.sub >= 4:
                        rmsnorm_fm(1, lpre)
                        proj(lpre, t0)
                    store_x_last(t0)
                elif lpre is not None:
                    rmsnorm_fm(0, lpre)
                    ffn(lpre, "ffn1")
                    rmsnorm_fm(1, lpre)
                    proj(lpre, t0)
                    p.op("act", lambda e, t0=t0: e.dma_start(out=S_xT[:, :, t0:t0 + T], in_=xt[:]), reads=xtr, writes=[R["xT"]], dma=xt, join=True)
                else:
                    store_x_last(t0)
            p.flush()
        _after_reset(p)

    p.flush()
    if cfg.stage == 0:
        return nc
    token_phase(None, 0)
    for l in range(L):
        if cfg.stage in (2, 99):
            phase_dsa(l)
        if cfg.stage in (3, 99):
            phase_s5(l)
        if cfg.stage in (4, 99):
            phase_dil(l)
        if cfg.stage == 99:
            token_phase(l, l + 1 if l + 1 < L else None)
    print("ops", p.nops, "sems", p.nsem, "sbuf left", nc.sbuf_bytes_remaining)
    return nc


def _run(cfg, inputs):
    nc = build(cfg)
    x = np.ascontiguousarray(inputs["x"], dtype=np.float32)
    in_maps = []
    for c in range(cfg.NCORES):
        m = {"x": np.ascontiguousarray(x[c])}
        for k in WEIGHT_SHAPES:
            m[k] = np.ascontiguousarray(inputs[k], dtype=np.float32)
        in_maps.append(m)
    res = run_bass_kernel_spmd(nc, in_maps, core_ids=list(range(cfg.NCORES)))
    return res


def kernel(**inputs):
    cfg = Cfg()
    res = _run(cfg, inputs)
    return np.stack([r["out"] for r in res.results], axis=0).astype(np.float32)
```

```python
import numpy as np
import concourse.bass as bass
import concourse.mybir as mybir
from concourse.bass_utils import run_bass_kernel_spmd

F32 = mybir.dt.float32
BF16 = mybir.dt.bfloat16
AF = mybir.ActivationFunctionType
ALU = mybir.AluOpType
AX = mybir.AxisListType

D = 1024
FF = 2816
DIN = 6852
EPS = 1e-6
MAGIC = 12582912.0
TWO_PI = 6.283185307179586


class Res:
    __slots__ = ("name", "writers", "readers", "sem", "cnt")

    _all = []

    def __init__(self, name):
        self.name = name
        self.writers = []
        self.readers = []
        self.sem = None
        self.cnt = 0
        Res._all.append(self)


class Tile:
    def __init__(self, t, res):
        self.t = t
        self.res = res

    def __getitem__(self, k):
        return self.t[k]


class Prog:
    ENGS = ("pe", "act", "dve", "pool", "sp")

    def __init__(self, nc):
        self.nc = nc
        self.ops = {e: [] for e in self.ENGS}
        self.cnt = {e: 0 for e in self.ENGS}
        self.esem = {e: nc.alloc_semaphore("s_" + e) for e in ("pe", "act", "dve", "pool")}
        self.waited = {e: {} for e in self.ENGS}
        self.free_sems = {True: [(nc.alloc_semaphore("dsw%d" % i), 0) for i in range(6)],
                          False: [(nc.alloc_semaphore("dhw%d" % i), 0) for i in range(80)]}
        self.live = []
        self.nsem = 4
        self.nops = 0

    def sb(self, name, shape, dtype):
        self.uid = getattr(self, "uid", 0) + 1
        name = "%s_u%d" % (name, self.uid)
        return Tile(self.nc.alloc_sbuf_tensor(name, list(shape), dtype), Res(name))

    def ps(self, name, shape, dtype=F32):
        return Tile(self.nc.alloc_psum_tensor(name, list(shape), dtype), Res(name))

    def op(self, eng, fn, reads=(), writes=(), dma=None, join=False):
        reads = [r.res if isinstance(r, Tile) else r for r in reads]
        writes = [w.res if isinstance(w, Tile) else w for w in writes]
        toks = []
        for r in reads:
            for t in r.writers:
                toks.append((t, "raw"))
        for w in writes:
            if not join:
                for t in w.writers:
                    toks.append((t, "waw"))
            for t in w.readers:
                toks.append((t, "war"))
        waits = {}
        for (sem, val, teng), kind in toks:
            if teng == eng and dma is None and eng == "pe":
                continue
            k = id(sem)
            if self.waited[eng].get(k, 0) >= val:
                continue
            if k not in waits or waits[k][1] < val:
                waits[k] = (sem, val)
        for k, (sem, val) in waits.items():
            self.waited[eng][k] = val
        if dma is None:
            self.cnt[eng] += 1
            tok = (self.esem[eng], self.cnt[eng], eng)
            inc = (self.esem[eng], 1)
        else:
            d = dma.res if isinstance(dma, Tile) else dma
            if d.sem is None:
                sw = (eng == "pool")
                d.sem, d.cnt = self.free_sems[sw].pop()
                self.nsem = max(self.nsem, 86 - len(self.free_sems[True]) - len(self.free_sems[False]))
                self.live.append((d, sw))
            d.cnt += 16
            tok = (d.sem, d.cnt, "dma")
            inc = (d.sem, 16)
        self.ops[eng].append((list(waits.values()), fn, inc))
        self.nops += 1
        for r in reads:
            r.readers.append(tok)
        for w in writes:
            if join:
                w.writers.append(tok)
            else:
                w.writers = [tok]
                w.readers = []
        return tok

    def flush(self):
        nc = self.nc
        finals = [(self.esem[e], self.cnt[e]) for e in ("pe", "act", "dve", "pool")]
        finals += [(d.sem, d.cnt) for d, _ in self.live]
        ops = self.ops

        def emit(eng, e):
            for waits, fn, inc in ops[eng]:
                for sem, val in waits:
                    e.wait_ge(sem, val)
                fn(e).then_inc(inc[0], inc[1])
            for sem, val in finals:
                if val > 0:
                    e.wait_ge(sem, val)

        with nc.Block() as block:
            @block.tensor
            def _(e):
                emit("pe", e)

            @block.scalar
            def _(e):
                emit("act", e)

            @block.vector
            def _(e):
                emit("dve", e)

            @block.gpsimd
            def _(e):
                emit("pool", e)

            @block.sync
            def _(e):
                emit("sp", e)

        self.ops = {e: [] for e in self.ENGS}
        for d, sw in self.live:
            self.free_sems[sw].append((d.sem, d.cnt))
            d.sem = None
        self.live = []
        for e in self.ENGS:
            for sem, val in finals:
                self.waited[e][id(sem)] = val


def _after_reset(p):
    for e in p.ENGS:
        p.cnt[e] = 0
        p.waited[e] = {}
    for sw in (True, False):
        p.free_sems[sw] = [(sem, 0) for sem, _ in p.free_sems[sw]]
    for r in Res._all:
        r.writers = []
        r.readers = []
        r.sem = None
        r.cnt = 0


class Cfg:
    def __init__(self, seq=4096, depth=4, ncores=4, stage=99, dbg=False, sub=9):
        self.sub = sub
        self.pset = set(range(1, 10))
        self.variant = ''
        self.SEQ = seq
        self.DEPTH = depth
        self.NCORES = ncores
        self.stage = stage
        self.dbg = dbg


WEIGHT_SHAPES = {
    "ffn1_norm": [D], "ffn1_gate": [D, FF], "ffn1_up": [D, FF], "ffn1_down": [FF, D],
    "mix_norm": [D], "w_in": [D, DIN], "a_q_norm": [64], "a_k_norm": [64],
    "s5_lam_re": [32, 64], "s5_lam_im": [32, 64], "s5_log_dt": [32],
    "s5_b_re": [32, 64, 16], "s5_b_im": [32, 64, 16], "s5_c_re": [32, 16, 64], "s5_c_im": [32, 16, 64],
    "s5_d": [32, 16], "s5_glu_a": [512, 512], "s5_glu_b": [512, 512],
    "c_q_norm": [64], "c_k_norm": [64],
    "w_branch_a": [512, D], "w_branch_b": [512, D], "w_branch_c": [256, D], "w_out": [D, D],
    "ffn2_norm": [D], "ffn2_gate": [D, FF], "ffn2_up": [D, FF], "ffn2_down": [FF, D],
}
BIG = ["ffn1_gate", "ffn1_up", "ffn1_down", "w_in", "s5_glu_a", "s5_glu_b",
       "w_branch_a", "w_branch_b", "w_branch_c", "w_out", "ffn2_gate", "ffn2_up", "ffn2_down"]
DIL = (1, 4, 16)


def build(cfg):
    Res._all = []
    nc = bass.Bass("TRN2", target_bir_lowering=False)
    NT = cfg.SEQ
    L = cfg.DEPTH
    T = 512
    NTT = NT // T
    NQB = NT // 128
    p = Prog(nc)

    X = nc.dram_tensor("x", [NT, D], F32, kind="ExternalInput").ap()
    W = {k: nc.dram_tensor(k, [L] + s, F32, kind="ExternalInput").ap() for k, s in WEIGHT_SHAPES.items()}
    OUT = nc.dram_tensor("out", [NT, D], F32, kind="ExternalOutput").ap()
    dbg_kind = "ExternalOutput" if cfg.dbg else "Internal"

    def scr(name, shape, dt, dbg=True):
        return nc.dram_tensor(name, list(shape), dt, kind=dbg_kind if dbg else "Internal").ap()

    WB = [{k: scr("wb%d_%s" % (l, k), WEIGHT_SHAPES[k], BF16, dbg=False) for k in BIG} for l in range(L)]
    WBres = [{k: Res("wb%d_%s" % (l, k)) for k in BIG} for l in range(L)]
    S_xT = scr("s_xT", [128, 8, NT], F32)
    S_aq = scr("s_aq", [128, 4, NT], BF16)
    S_kdup = scr("s_kdup", [128, NT], BF16)
    S_ik = scr("s_ik", [64, NT], BF16)
    S_su = scr("s_su", [128, 4, NT], BF16)
    S_tm1 = scr("s_tm1", [128, NT // 128, 512], F32)
    S_av = scr("s_av", [128, NT // 128, 64], BF16)
    S_cv = [scr("s_cv%d" % g, [128, 2, DIL[g], NT // DIL[g]], BF16) for g in range(3)]
    S_cq = [scr("s_cq%d" % g, [128, 2, DIL[g], NT // DIL[g]], BF16) for g in range(3)]
    S_ck = [scr("s_ck%d" % g, [128, 2, DIL[g], NT // DIL[g]], BF16) for g in range(3)]
    S_g = scr("s_g", [128, 24, NT], BF16)
    S_ya = scr("s_ya", [64, 8, NT], BF16)
    S_ys5 = scr("s_ys5", [128, 4, NT], F32)
    S_yc = scr("s_yc", [64, 4, NT], BF16)
    if cfg.dbg:
        D_vp = scr("dbg_vp", [128, NT // 128, 65], BF16)
        D_sc = scr("dbg_sc", [NT // 128, 128, NT], F32)
        D_lo = scr("dbg_lo", [NT // 128, 128, 2], F32)
    R = {n: Res(n) for n in ["xT", "aq", "kdup", "ik", "su", "tm1", "av", "cv", "cq", "ck", "g", "ya", "ys5", "yc", "out"]}

    ident32 = p.sb("ident32", [128, 128], F32)
    identb = p.sb("identb", [128, 128], BF16)
    ones32 = p.sb("ones32", [128, 128], F32)
    blk = p.sb("blk", [128, 128], BF16)
    epsc = p.sb("epsc", [128, 1], F32)
    gvec = p.sb("gvec", [128, 3, L, 8], F32)
    gqk = p.sb("gqk", [128, 4, L], F32)
    PS = [p.ps("ps%d" % i, [128, 512], F32) for i in range(8)]

    def pool(fn, reads=(), writes=()):
        return p.op("pool", fn, reads=reads, writes=writes)

    def act(fn, reads=(), writes=()):
        return p.op("act", fn, reads=reads, writes=writes)

    def dve(fn, reads=(), writes=()):
        return p.op("dve", fn, reads=reads, writes=writes)

    def pe(fn, reads=(), writes=()):
        return p.op("pe", fn, reads=reads, writes=writes)

    def mm(out_ap, lhsT, rhs, start, stop, reads, writes):
        return p.op("pe", lambda e: e.matmul(out_ap, lhsT=lhsT, rhs=rhs, start=start, stop=stop), reads=reads, writes=writes)

    def load(out_ap, in_ap, tile, reads=(), q="sp", slow=False, join=False):
        return p.op(q, lambda e: e.dma_start(out=out_ap, in_=in_ap, allow_slow_non_contiguous=slow),
                    reads=reads, writes=[tile], dma=tile, join=join)

    def store(out_ap, in_ap, tile, wres, q="act", join=True):
        return p.op(q, lambda e: e.dma_start(out=out_ap, in_=in_ap), reads=[tile], writes=[wres], dma=tile, join=join)

    pool(lambda e: e.memset(ident32[:], 1.0), writes=[ident32])
    pool(lambda e: e.affine_select(out=ident32[:], in_=ident32[:], pattern=[[-1, 128]], compare_op=ALU.is_equal,
                                   fill=0.0, base=0, channel_multiplier=1), reads=[ident32], writes=[ident32])
    pool(lambda e: e.tensor_copy(out=identb[:], in_=ident32[:]), reads=[ident32], writes=[identb])
    pool(lambda e: e.memset(ones32[:], 1.0), writes=[ones32])
    pool(lambda e: e.memset(blk[:], 0.0), writes=[blk])
    pool(lambda e: e.memset(blk[0:64, 0:64], 1.0), writes=[blk])
    pool(lambda e: e.memset(blk[64:128, 64:128], 1.0), writes=[blk])
    pool(lambda e: e.memset(epsc[:], EPS), writes=[epsc])
    for wi, nm in enumerate(["ffn1_norm", "mix_norm", "ffn2_norm"]):
        load(gvec[:, wi, :, :], W[nm].rearrange("l (c q) -> q l c", q=128), gvec, slow=True, join=True)
    for wi, nm in enumerate(["a_q_norm", "a_k_norm", "c_q_norm", "c_k_norm"]):
        for hf in range(2):
            load(gqk[hf * 64:(hf + 1) * 64, wi, :], W[nm].rearrange("l d -> d l"), gqk, slow=True, join=True)

    castchain = Res("castchain")
    for l in range(L):
        castres = Res("cast%d" % l)
        tok = None
        for k in BIG:
            src = W[k][l]
            K, N = WEIGHT_SHAPES[k]
            for n0 in range(0, N, 2048):
                n1 = min(N, n0 + 2048)
                tok = p.op("pool", lambda e, s=src[:, n0:n1], d=WB[l][k][:, n0:n1]: e.dma_start(out=d, in_=s),
                           reads=[castchain], writes=[WBres[l][k], castchain], dma=castres)
        for k in BIG:
            WBres[l][k].writers = [tok]

    xt = hb = ab = SQ = SQB = rs = RSD = SG = WS = STG = xin = xtr = hbr = abr = None
    NWS = 4
    NST = 6
    cnt = {"ws": 0, "st": 0, "pa": 0, "sq": 0, "sqb": 0, "rsd": 0, "sg": 0}

    def nxt(key, lst):
        t = lst[cnt[key] % len(lst)]
        cnt[key] += 1
        return t

    def psA():
        return nxt("pa", PS[0:4])

    def wload(src, wres, kc, n, kp=128):
        slot = nxt("ws", WS)
        view = slot.t[0:kp, 0:kc * n].rearrange("q (k n) -> q k n", k=kc)
        p.op("sp", lambda e: e.dma_start(out=view, in_=src.rearrange("(k q) n -> q k n", q=kp)),
             reads=[wres], writes=[slot], dma=slot)
        return slot, view

    def stg_bf(shape_free):
        st = nxt("st", STG)
        n = 1
        for s in shape_free:
            n *= s
        v = st.t[:].bitcast(BF16)[:, 0:n]
        return st, v

    def rmsnorm_fm(which, l):
        ss = PS[4]
        for c in range(8):
            sq = nxt("sq", SQ)
            act(lambda e, sq=sq, c=c: e.activation(out=sq[:], in_=xt[:, c, :], func=AF.Square), reads=[xtr[c]], writes=[sq])
            mm(ss[:], ones32[:], sq[:], c == 0, c == 7, [ones32, sq], [ss])
        act(lambda e: e.activation(out=rs[:], in_=ss[:], func=AF.Sqrt, bias=epsc[:, 0:1], scale=1.0 / D), reads=[ss, epsc], writes=[rs])
        dve(lambda e: e.reciprocal(out=rs[:], in_=rs[:]), reads=[rs], writes=[rs])
        for c in range(8):
            dve(lambda e, c=c: e.scalar_tensor_tensor(out=hb[:, c, :], in0=xt[:, c, :], scalar=gvec[:, which, l, c:c + 1],
                                                      in1=rs[:], op0=ALU.mult, op1=ALU.mult), reads=[xtr[c], gvec, rs], writes=[hbr[c]])

    def ffn(l, pre):
        Wg, Wu, Wd = WB[l][pre + "_gate"], WB[l][pre + "_up"], WB[l][pre + "_down"]
        rg, ru, rd = WBres[l][pre + "_gate"], WBres[l][pre + "_up"], WBres[l][pre + "_down"]
        for n0 in range(0, FF, 512):
            n1 = min(FF, n0 + 512)
            sg_, wg = wload(Wg[:, n0:n1], rg, 8, n1 - n0)
            su_, wu = wload(Wu[:, n0:n1], ru, 8, n1 - n0)
            for c0 in range(0, n1 - n0, 128):
                f = (n0 + c0) // 128
                pg = psA()
                pu = psA()
                for k in range(8):
                    mm(pg[:], wg[:, k, c0:c0 + 128], hb[:, k, :], k == 0, k == 7, [sg_, hbr[k]], [pg])
                for k in range(8):
                    mm(pu[:], wu[:, k, c0:c0 + 128], hb[:, k, :], k == 0, k == 7, [su_, hbr[k]], [pu])
                sg = nxt("sg", SG)
                act(lambda e, sg=sg, pg=pg: e.activation(out=sg[:], in_=pg[:], func=AF.Silu), reads=[pg], writes=[sg])
                dve(lambda e, sg=sg, pu=pu, f=f: e.tensor_tensor(out=ab[:, f, :], in0=pu[:], in1=sg[:], op=ALU.mult),
                    reads=[pu, sg], writes=[abr[f]])
        for half in range(2):
            accs = PS[4:8]
            for fg in range(0, 22, 4):
                nf = min(4, 22 - fg)
                sl, wv = wload(Wd[fg * 128:(fg + nf) * 128, half * 512:(half + 1) * 512], rd, nf, 512)
                for j in range(nf):
                    f = fg + j
                    for dc in range(4):
                        mm(accs[dc][:], wv[:, j, dc * 128:(dc + 1) * 128], ab[:, f, :], f == 0, f == 21, [sl, abr[f]], [accs[dc]])
            for dc in range(4):
                c = half * 4 + dc
                dve(lambda e, c=c, a=accs[dc]: e.scalar_tensor_tensor(out=xt[:, c, :], in0=a[:], scalar=0.5, in1=xt[:, c, :],
                                                                       op0=ALU.mult, op1=ALU.add), reads=[accs[dc], xtr[c]], writes=[xtr[c]])

    def qknorm_epi(ps, cw, gidx, l, out_ap, view=None):
        sqb = nxt("sqb", SQB)
        act(lambda e: e.activation(out=sqb[0:cw, :], in_=ps[0:cw, :], func=AF.Square), reads=[ps], writes=[sqb])
        ss = PS[5] if cnt["rsd"] % 2 == 0 else PS[6]
        mm(ss[0:cw, :], blk[0:cw, 0:cw], sqb[0:cw, :], True, True, [blk, sqb], [ss])
        rsd = nxt("rsd", RSD)
        act(lambda e: e.activation(out=rsd[0:cw, :], in_=ss[0:cw, :], func=AF.Sqrt, bias=epsc[0:cw, 0:1], scale=1.0 / 64),
            reads=[ss, epsc], writes=[rsd])
        dve(lambda e: e.reciprocal(out=rsd[0:cw, :], in_=rsd[0:cw, :]), reads=[rsd], writes=[rsd])
        in0 = ps[0:cw, :]
        in1 = rsd[0:cw, :]
        if view is not None:
            in0 = in0.rearrange(view[0], **view[1])
            in1 = in1.rearrange(view[0], **view[1])
        return (lambda st: dve(lambda e: e.scalar_tensor_tensor(out=out_ap, in0=in0, scalar=gqk[0:cw, gidx, l:l + 1], in1=in1,
                                                                op0=ALU.mult, op1=ALU.mult), reads=[ps, rsd, gqk], writes=[st]))

    def proj(l, t0):
        Win = WB[l]["w_in"]
        rw = WBres[l]["w_in"]

        def fm_group(n0, n1, epi):
            sl, wv = wload(Win[:, n0:n1], rw, 8, n1 - n0)
            for c0 in range(0, n1 - n0, 128):
                cw = min(128, n1 - n0 - c0)
                ps = psA()
                for k in range(8):
                    mm(ps[0:cw, :], wv[:, k, c0:c0 + cw], hb[:, k, :], k == 0, k == 7, [sl, hbr[k]], [ps])
                epi(n0 + c0, cw, ps)

        def epi_aq(col, cw, ps):
            hp = col // 128
            st, v = stg_bf([T])
            qknorm_epi(ps, 128, 0, l, v)(st)
            store(S_aq[:, hp, t0:t0 + T], v, st, R["aq"])
        if 1 in cfg.pset:
            fm_group(0, 512, epi_aq)

        def epi_ak(col, cw, ps):
            st, v = stg_bf([T])
            qknorm_epi(ps, 64, 1, l, v[0:64, :])(st)
            store(S_kdup[0:64, t0:t0 + T], v[0:64, :], st, R["kdup"])
            store(S_kdup[64:128, t0:t0 + T], v[0:64, :], st, R["kdup"])
        if 2 in cfg.pset:
            fm_group(512, 576, epi_ak)

        def epi_ik(col, cw, ps):
            st, v = stg_bf([T])
            act(lambda e: e.activation(out=v[0:64, :], in_=ps[0:64, :], func=AF.Copy), reads=[ps], writes=[st])
            store(S_ik[:, t0:t0 + T], v[0:64, :], st, R["ik"])
        if 3 in cfg.pset:
            fm_group(896, 960, epi_ik)

        def epi_su(col, cw, ps):
            j = (col - 964) // 128
            st, v = stg_bf([T])
            act(lambda e: e.activation(out=v, in_=ps[:], func=AF.Copy), reads=[ps], writes=[st])
            store(S_su[:, j, t0:t0 + T], v, st, R["su"])
        if 4 in cfg.pset:
            fm_group(964, 1476, epi_su)

        def mk_epi_c(base, gidx, S, rname):
            def epi(col, cw, ps):
                ch = (col - base) // 128
                g = ch // 2
                hp = ch % 2
                d = DIL[g]
                st, v = stg_bf([T])
                if d == 1:
                    qknorm_epi(ps, 128, gidx, l, v)(st)
                    store(S[g][:, hp, 0, t0:t0 + T], v, st, R[rname])
                else:
                    v3 = v.rearrange("q (r s) -> q r s", r=d)
                    qknorm_epi(ps, 128, gidx, l, v3, view=("q (s r) -> q r s", {"r": d}))(st)
                    store(S[g][:, hp, :, t0 // d:(t0 + T) // d], v3, st, R[rname])
            return epi
        if 5 in cfg.pset:
            fm_group(1476, 1988, mk_epi_c(1476, 2, S_cq, "cq"))
            fm_group(1988, 2244, mk_epi_c(1476, 2, S_cq, "cq"))
            fm_group(2244, 2756, mk_epi_c(2244, 3, S_ck, "ck"))
            fm_group(2756, 3012, mk_epi_c(2244, 3, S_ck, "ck"))

        def epi_g(col, cw, ps):
            j = (col - 3780) // 128
            st, v = stg_bf([T])
            act(lambda e: e.activation(out=v, in_=ps[:], func=AF.Sigmoid), reads=[ps], writes=[st])
            store(S_g[:, j, t0:t0 + T], v, st, R["g"])
        for n0 in range(3780, DIN, 512):
            if 6 in cfg.pset:
                fm_group(n0, n0 + 512, epi_g)

        def tm_group(n0, n1, epi):
            sl, wv = wload(Win[:, n0:n1], rw, 8, n1 - n0)
            for sub in range(4):
                ps = psA()
                for k in range(8):
                    mm(ps[:, 0:n1 - n0], hb[:, k, sub * 128:(sub + 1) * 128], wv[:, k, :], k == 0, k == 7, [sl, hbr[k]], [ps])
                epi(sub, ps)

        def epi_tm1(sub, ps):
            st = nxt("st", STG)
            act(lambda e: e.activation(out=st[:, 0:512], in_=ps[:, 0:512], func=AF.Copy), reads=[ps], writes=[st])
            store(S_tm1[:, t0 // 128 + sub, :], st[:, 0:512], st, R["tm1"])
        if 7 in cfg.pset:
            tm_group(576, 1088, epi_tm1)

        def epi_cv(col, cw, ps):
            ch = (col - 3012) // 128
            g = ch // 2
            hp = ch % 2
            d = DIL[g]
            st, v = stg_bf([T])
            if d == 1:
                act(lambda e: e.activation(out=v, in_=ps[:], func=AF.Copy), reads=[ps], writes=[st])
                store(S_cv[g][:, hp, 0, t0:t0 + T], v, st, R["cv"])
            else:
                v3 = v.rearrange("q (r s) -> q r s", r=d)
                act(lambda e: e.activation(out=v3, in_=ps[:].rearrange("q (s r) -> q r s", r=d), func=AF.Copy), reads=[ps], writes=[st])
                store(S_cv[g][:, hp, :, t0 // d:(t0 + T) // d], v3, st, R["cv"])
        if 8 in cfg.pset:
            fm_group(3012, 3524, epi_cv)
            fm_group(3524, 3780, epi_cv)

    def load_x_first(t0):
        load(xin[:], X[t0:t0 + T, :].rearrange("(s q) f -> q s f", q=128), xin)
        for c in range(8):
            ps = psA()
            for s in range(4):
                pe(lambda e, ps=ps, s=s, c=c: e.transpose(out=ps[:, s * 128:(s + 1) * 128], in_=xin[:, s, c * 128:(c + 1) * 128],
                                                          identity=ident32[:]), reads=[xin, ident32], writes=[ps])
            act(lambda e, ps=ps, c=c: e.activation(out=xt[:, c, :], in_=ps[:], func=AF.Copy), reads=[ps], writes=[xtr[c]])

    def store_x_last(t0):
        for s in range(4):
            for c2 in range(2):
                ps = psA()
                for c in range(4):
                    cc = c2 * 4 + c
                    pe(lambda e, ps=ps, s=s, c=c, cc=cc: e.transpose(out=ps[:, c * 128:(c + 1) * 128], in_=xt[:, cc, s * 128:(s + 1) * 128],
                                                                     identity=ident32[:]), reads=[xtr[cc], ident32], writes=[ps])
                act(lambda e, ps=ps, s=s, c2=c2: e.activation(out=xin[:, s, c2 * 512:(c2 + 1) * 512], in_=ps[:], func=AF.Copy),
                    reads=[ps], writes=[xin])
        return store(OUT[t0:t0 + T, :].rearrange("(s q) f -> q s f", q=128), xin[:], xin, R["out"])


    NIT = 16

    def phase_dsa(l):
        with nc.reset_on_exit():
            kidx = p.sb("kidx", [64, NT], BF16)
            kdup = p.sb("kdup", [128, NT], BF16)
            vp = p.sb("vp", [128, NQB, 65], BF16)
            sc = p.sb("sc", [128, NT], F32)
            scr_ = [Res("sc%d" % i) for i in range((NT + 511) // 512)]
            msk = p.sb("msk", [128, NT], BF16)
            junk = msk
            zt = p.sb("zt", [128, NT], BF16)
            cum = p.sb("cum", [128, NT], mybir.dt.float16)
            SM = {k: p.sb("sm_" + k, [128, 1], F32) for k in ["cpos", "t1", "c1", "c2", "tie", "nd0", "need"]}
            tri = p.sb("tri", [128, 128], F32)
            sel = p.sb("sel", [65, 64], F32)
            nb = p.sb("nb", [128, 1], F32)
            TMQ = [p.sb("tmq%d" % i, [128, 512], F32) for i in range(2)]
            QF = [p.sb("qf%d" % i, [128, 4, 128], BF16) for i in range(2)]
            WAB = [p.sb("wab%d" % i, [128, 4], F32) for i in range(2)]
            QLN = [p.sb("qln%d" % i, [128, 64], F32) for i in range(2)]
            QS = [p.sb("qs%d" % i, [128, 5, 64], BF16) for i in range(2)]
            QST = [p.sb("qst%d" % i, [64, 5, 128], BF16) for i in range(2)]
            LO = [p.sb("lo%d" % i, [128, 1], F32) for i in range(2)]
            HI = [p.sb("hi%d" % i, [128, 1], F32) for i in range(2)]
            mid = p.sb("mid", [128, 1], F32)
            tq = p.sb("tq", [128, 1], F32)
            HH = p.sb("HH", [128, NIT], F32)
            pw2 = p.sb("pw2", [128, NIT], F32)
            for i_ in range(NIT):
                pool(lambda e, i_=i_: e.memset(pw2[:, i_:i_ + 1], 2.0 ** (-(i_ + 1))), writes=[pw2])
            cn = p.sb("cn", [128, 1], F32)
            pge = p.sb("pge", [128, 1], mybir.dt.int32)
            plt = p.sb("plt", [128, 1], mybir.dt.int32)
            EE = [p.sb("ee%d" % i, [128, 1024], BF16) for i in range(3)]
            EM = [p.sb("em%d" % i, [128, 1024], BF16) for i in range(3)]
            osb = p.sb("osb", [65, 1024], F32)
            rec = p.sb("rec", [64, 1024], F32)
            YA = [p.sb("yast%d" % i, [64, 1024], BF16) for i in range(2)]
            XH = [p.sb("xh%d" % i, [128, 512], F32) for i in range(3)]

            load(kidx[:], S_ik, kidx, reads=[R["ik"]])
            load(kdup[:], S_kdup, kdup, reads=[R["kdup"]])
            pool(lambda e: e.memset(vp[:, :, 64:65], 1.0), writes=[vp])
            vf = p.sb("vf", [128, NQB, 64], F32)
            load(vf[:], S_tm1[:, :, 0:64], vf, reads=[R["tm1"]])
            act(lambda e: e.activation(out=vp[:, :, 0:64], in_=vf[:], func=AF.Copy), reads=[vf], writes=[vp])
            pool(lambda e: e.memset(tri[:], 0.0), writes=[tri])
            pool(lambda e: e.affine_select(out=tri[:], in_=tri[:], pattern=[[-1, 128]], compare_op=ALU.is_ge,
                                           fill=-1e30, base=0, channel_multiplier=1), reads=[tri], writes=[tri])
            pool(lambda e: e.memset(sel[:], 0.0), writes=[sel])
            pool(lambda e: e.memset(sel[64:65, :], 1.0), writes=[sel])
            pool(lambda e: e.memset(nb[:], -8.0), writes=[nb])

            pl = PS[0]
            PH = [PS[1], PS[2]]
            ST = [PS[3], PS[4]]
            STB = [[PS[3], PS[4]], [PS[1], PS[2]]]
            PTR = [PS[5], PS[0]]
            ptr = PS[5]
            ptr_b = ptr.t[:].bitcast(BF16)
            OO = [PS[6], PS[7]]
            hcnt = [0]

            for qb in range(NQB):
                t0 = qb * 128
                N = t0 + 128
                b = qb % 2
                tmq, qf, wab, qln, qs, qst, lo, hi = TMQ[b], QF[b], WAB[b], QLN[b], QS[b], QST[b], LO[b], HI[b]
                load(tmq[:], S_tm1[:, qb, :], tmq, reads=[R["tm1"]])
                load(qf[:], S_aq[:, :, t0:t0 + 128], qf, reads=[R["aq"]])
                dve(lambda e, qs=qs, tmq=tmq: e.tensor_copy(out=qs[:, 0:4, :].rearrange("q h d -> q (h d)"), in_=tmq[:, 64:320]),
                    reads=[tmq], writes=[qs])
                for v in range(4):
                    pe(lambda e, v=v, qs=qs: e.transpose(out=ptr_b[0:64, v * 128:(v + 1) * 128], in_=qs[:, v, :], identity=identb[:]),
                       reads=[qs, identb], writes=[ptr])
                act(lambda e, qst=qst: e.activation(out=qst[:, 0:4, :].rearrange("q v t -> q (v t)"), in_=ptr_b[0:64, 0:512], func=AF.Copy),
                    reads=[ptr], writes=[qst])
                nk = (N + 511) // 512
                for kc in range(nk):
                    w = min(512, N - kc * 512)
                    ksl = kidx[:, kc * 512:kc * 512 + w]
                    scs = sc[:, kc * 512:kc * 512 + w]
                    for h in range(4):
                        ph = PH[hcnt[0] % 2]
                        xh = XH[hcnt[0] % 3]
                        hcnt[0] += 1
                        mm(ph[:, 0:w], qst[:, h, :], ksl, True, True, [qst, kidx], [ph])
                        if h == 0:
                            dve(lambda e, ph=ph, scs=scs, w=w, tmq=tmq: e.tensor_scalar(out=scs, in0=ph[:, 0:w], scalar1=0.0, scalar2=tmq[:, 384:385],
                                                                                        op0=ALU.max, op1=ALU.mult),
                                reads=[ph, tmq], writes=[scr_[kc]])
                        else:
                            dve(lambda e, ph=ph, xh=xh, w=w, tmq=tmq, h=h: e.tensor_scalar(out=xh[:, 0:w], in0=ph[:, 0:w], scalar1=0.0,
                                                                                           scalar2=tmq[:, 384 + h:385 + h], op0=ALU.max, op1=ALU.mult),
                                reads=[ph, tmq], writes=[xh])
                            pool(lambda e, xh=xh, scs=scs, w=w: e.tensor_tensor(out=scs, in0=xh[:, 0:w], in1=scs, op=ALU.add),
                                 reads=[xh, scr_[kc]], writes=[scr_[kc]])
                allsc = scr_[0:nk]
                if qb >= 2:
                    dve(lambda e, N=N, hi=hi: e.tensor_reduce(out=hi[:], in_=sc[:, 0:N], axis=AX.X, op=ALU.max), reads=allsc, writes=[hi])
                    dve(lambda e, N=N, lo=lo: e.tensor_reduce(out=lo[:], in_=sc[:, 0:N], axis=AX.X, op=ALU.min), reads=allsc, writes=[lo])
                else:
                    pool(lambda e, lo=lo: e.memset(lo[:], -1e29), writes=[lo])
                dve(lambda e, t0=t0: e.tensor_tensor(out=sc[:, t0:t0 + 128], in0=sc[:, t0:t0 + 128], in1=tri[:], op=ALU.add),
                    reads=[scr_[nk - 1], tri], writes=[scr_[nk - 1]])
                if qb >= 2:
                    pool(lambda e, N=N: e.tensor_single_scalar(out=zt[:, 0:N], in_=sc[:, 0:N], scalar=0.0, op=ALU.is_equal),
                         reads=allsc, writes=[zt])
                    dve(lambda e, N=N: e.tensor_tensor_scan(out=cum[:, 0:N], data0=zt[:, 0:N], data1=zt[:, 0:N], initial=0.0,
                                                            op0=ALU.add, op1=ALU.max), reads=[zt], writes=[cum])
                    dve(lambda e, N=N: e.tensor_scalar(out=junk[:, 0:N], in0=sc[:, 0:N], scalar1=0.0, scalar2=None,
                                                       op0=ALU.is_gt, op1=ALU.add, accum_out=SM["cpos"][:]),
                        reads=allsc, writes=[junk, SM["cpos"]])
                    dve(lambda e, N=N: e.tensor_tensor(out=SM["t1"][:], in0=SM["cpos"][:], in1=cum[:, N - 1:N], op=ALU.add),
                        reads=[SM["cpos"], cum], writes=[SM["t1"]])
                    dve(lambda e: e.tensor_single_scalar(out=SM["c1"][:], in_=SM["cpos"][:], scalar=255.5, op=ALU.is_lt),
                        reads=[SM["cpos"]], writes=[SM["c1"]])
                    dve(lambda e: e.tensor_single_scalar(out=SM["c2"][:], in_=SM["t1"][:], scalar=255.5, op=ALU.is_ge),
                        reads=[SM["t1"]], writes=[SM["c2"]])
                    dve(lambda e: e.tensor_tensor(out=SM["tie"][:], in0=SM["c1"][:], in1=SM["c2"][:], op=ALU.mult),
                        reads=[SM["c1"], SM["c2"]], writes=[SM["tie"]])
                    dve(lambda e: e.tensor_scalar(out=SM["nd0"][:], in0=SM["cpos"][:], scalar1=-1.0, scalar2=256.0 - 16384.0,
                                                  op0=ALU.mult, op1=ALU.add), reads=[SM["cpos"]], writes=[SM["nd0"]])
                    dve(lambda e: e.tensor_scalar(out=SM["need"][:], in0=SM["nd0"][:], scalar1=SM["tie"][:, 0:1], scalar2=16384.0,
                                                  op0=ALU.mult, op1=ALU.add), reads=[SM["nd0"], SM["tie"]], writes=[SM["need"]])
                    pool(lambda e, N=N: e.tensor_tensor(out=cum[:, 0:N], in0=cum[:, 0:N], in1=zt[:, 0:N], op=ALU.mult),
                         reads=[cum, zt], writes=[cum])
                    pool(lambda e, N=N: e.tensor_scalar(out=zt[:, 0:N], in0=cum[:, 0:N], scalar1=SM["need"][:, 0:1], scalar2=None,
                                                        op0=ALU.is_gt), reads=[cum, SM["need"]], writes=[zt])
                    dve(lambda e, lo=lo, hi=hi: e.tensor_tensor(out=hi[:], in0=hi[:], in1=lo[:], op=ALU.subtract), reads=[hi, lo], writes=[hi])
                    dve(lambda e, hi=hi: e.tensor_scalar(out=HH[:], in0=pw2[:], scalar1=hi[:, 0:1], scalar2=None, op0=ALU.mult),
                        reads=[pw2, hi], writes=[HH])
                    dve(lambda e, lo=lo: e.tensor_tensor(out=mid[:], in0=lo[:], in1=HH[:, 0:1], op=ALU.add), reads=[lo, HH], writes=[mid])
                    for it in range(NIT):
                        dve(lambda e, N=N: e.tensor_scalar(out=junk[:, 0:N], in0=sc[:, 0:N], scalar1=mid[:, 0:1], scalar2=None,
                                                           op0=ALU.is_ge, op1=ALU.add, accum_out=cn[:]),
                            reads=allsc + [mid], writes=[junk, cn])
                        dve(lambda e, it=it: e.tensor_scalar(out=tq[:], in0=cn[:], scalar1=255.5, scalar2=HH[:, it:it + 1],
                                                             op0=ALU.is_ge, op1=ALU.mult), reads=[cn, HH], writes=[tq])
                        if it < NIT - 1:
                            dve(lambda e, it=it: e.scalar_tensor_tensor(out=mid[:], in0=tq[:], scalar=HH[:, it + 1:it + 2], in1=mid[:],
                                                                        op0=ALU.subtract, op1=ALU.add), reads=[tq, HH, mid], writes=[mid])
                        else:
                            dve(lambda e, it=it, lo=lo: e.scalar_tensor_tensor(out=lo[:], in0=tq[:], scalar=HH[:, it:it + 1], in1=mid[:],
                                                                               op0=ALU.subtract, op1=ALU.add), reads=[tq, HH, mid], writes=[lo])
                if qb >= 2:
                    dve(lambda e, N=N, lo=lo: e.scalar_tensor_tensor(out=msk[:, 0:N], in0=sc[:, 0:N], scalar=lo[:, 0:1], in1=zt[:, 0:N],
                                                                     op0=ALU.is_ge, op1=ALU.subtract), reads=allsc + [lo, zt], writes=[msk])
                else:
                    dve(lambda e, N=N, lo=lo: e.tensor_scalar(out=msk[:, 0:N], in0=sc[:, 0:N], scalar1=lo[:, 0:1], scalar2=None, op0=ALU.is_ge),
                        reads=allsc + [lo], writes=[msk])
                if cfg.dbg:
                    p.op("sp", lambda e, qb=qb, N=N: e.dma_start(out=D_sc[qb, :, 0:N], in_=sc[:, 0:N]), reads=allsc, writes=[R["out"]], dma=sc, join=True)
                    p.op("sp", lambda e, qb=qb, lo=lo: e.dma_start(out=D_lo[qb, :, 0:1], in_=lo[:], allow_slow_non_contiguous=True), reads=[lo], writes=[R["out"]], dma=lo, join=True)
                    if qb >= 2:
                      p.op("sp", lambda e, qb=qb, hi=hi: e.dma_start(out=D_lo[qb, :, 1:2], in_=hi[:], allow_slow_non_contiguous=True), reads=[hi], writes=[R["out"]], dma=hi, join=True)
                pendA = []
                for c in range(qb + 1):
                    ee = EE[c % 3]
                    em = EM[c % 3]
                    STc = STB[c % 2]
                    pt = PTR[c % 2]
                    pt_b = pt.t[:].bitcast(BF16)
                    pe(lambda e, c=c, pt_b=pt_b: e.transpose(out=pt_b[:, 0:128], in_=msk[:, c * 128:(c + 1) * 128], identity=identb[:]),
                       reads=[msk, identb], writes=[pt])
                    for hl in range(2):
                        mm(STc[hl][:], kdup[hl * 64:(hl + 1) * 64, c * 128:(c + 1) * 128], qf[hl * 64:(hl + 1) * 64, :, :], True, True,
                           [kdup, qf], [STc[hl]])
                        act(lambda e, hl=hl, ee=ee, STc=STc: e.activation(out=ee[:, hl * 512:(hl + 1) * 512], in_=STc[hl][:], func=AF.Exp,
                                                                          bias=nb[:, 0:1], scale=0.125), reads=[STc[hl], nb], writes=[ee])
                    dve(lambda e, ee=ee, em=em, pt_b=pt_b: e.tensor_tensor(out=em[:].rearrange("q (h t) -> q h t", h=8),
                                                                           in0=ee[:].rearrange("q (h t) -> q h t", h=8),
                                                                           in1=pt_b[:, 0:128].unsqueeze(1).broadcast_to([128, 8, 128]), op=ALU.mult),
                        reads=[ee, pt], writes=[em])

                    def tailA(c=c, em=em, qb=qb):
                        for hl in range(2):
                            mm(OO[hl][0:65, :], vp[:, c, :], em[:, hl * 512:(hl + 1) * 512], c == 0, c == qb, [vp, em], [OO[hl]])
                    pendA.append(tailA)
                    if len(pendA) > 1:
                        pendA.pop(0)()
                while pendA:
                    pendA.pop(0)()
                for hl in range(2):
                    act(lambda e, hl=hl: e.activation(out=osb[:, hl * 512:(hl + 1) * 512], in_=OO[hl][0:65, :], func=AF.Copy),
                        reads=[OO[hl]], writes=[osb])
                for hl in range(2):
                    mm(PH[hl][0:64, :], sel[:, :], osb[:, hl * 512:(hl + 1) * 512], True, True, [sel, osb], [PH[hl]])
                    dve(lambda e, hl=hl: e.reciprocal(out=rec[:, hl * 512:(hl + 1) * 512], in_=PH[hl][0:64, :]), reads=[PH[hl]], writes=[rec])
                ya = YA[b]
                dve(lambda e, ya=ya: e.tensor_tensor(out=ya[:], in0=osb[0:64, :], in1=rec[:], op=ALU.mult), reads=[osb, rec], writes=[ya])
                for hl in range(2):
                    store(S_ya.rearrange("d (hp hl) t -> d hl hp t", hl=2)[:, hl, :, t0:t0 + 128],
                          ya[:, hl * 512:(hl + 1) * 512].rearrange("d (hp t) -> d hp t", hp=4), ya, R["ya"])
            p.flush()
        _after_reset(p)


    def phase_s5(l):
        TC = 512
        NTC = NT // TC
        with nc.reset_on_exit():
            def sm(name, shape=(128, 32), dt=F32):
                return p.sb("s5_" + name, list(shape), dt)
            lre, lim, dtt = sm("lre"), sm("lim"), sm("dtt")
            aa, th, rr = sm("aa"), sm("th"), sm("rr")
            ft, kk0, fr0, af0 = sm("ft"), sm("kk0"), sm("fr0"), sm("af0")
            sn, cs = sm("sn"), sm("cs")
            nre, nim, den = sm("nre"), sm("nim"), sm("den")
            cre, cim, ta, tb = sm("cre"), sm("cim"), sm("ta"), sm("tb")
            CB, CC = sm("CB"), sm("CC")
            hpi = sm("hpi", (128, 1))
            sgn1 = sm("sgn1", (128, 1))
            rowmask = sm("rowmask", (128, 8))
            colmask = sm("colmask", (128, 8, 128))
            dcol = sm("dcol", (128, 4))
            X1, X2 = sm("X1", (128, 32, 16)), sm("X2", (128, 32, 16))
            BB, BBs, tmpB = sm("BB", (128, 512)), sm("BBs", (128, 512)), sm("tmpB", (128, 512))
            Cin, Cin2 = sm("Cin", (128, 2, 64)), sm("Cin2", (128, 2, 64))
            LB = sm("LB", (128, 8, 2, 128), BF16)
            LC = sm("LC", (128, 8, 2, 128), BF16)
            uj = sm("uj", (128, NT), BF16)
            iota1, onesT = sm("iota1", (128, TC)), sm("onesT", (128, TC))
            carry = sm("carry", (128, 32))
            NB = 4
            TT = [sm("tt%d" % i, (128, TC)) for i in range(NB)]
            KK = [sm("kk%d" % i, (128, TC)) for i in range(NB)]
            FR = [sm("fr%d" % i, (128, TC)) for i in range(NB)]
            AFR = [sm("afr%d" % i, (128, TC)) for i in range(NB)]
            SN = [sm("sn%d" % i, (128, TC)) for i in range(NB)]
            CS = [sm("cs%d" % i, (128, TC)) for i in range(NB)]
            RT = [sm("rt%d" % i, (128, TC)) for i in range(NB)]
            T1 = [sm("t1%d" % i, (128, TC)) for i in range(NB)]
            T2 = [sm("t2%d" % i, (128, TC)) for i in range(NB)]
            STt = [sm("st%d" % i, (128, TC)) for i in range(NB)]
            Z1 = [sm("z1%d" % i, (128, TC), BF16) for i in range(NB)]
            Z2 = [sm("z2%d" % i, (128, TC), BF16) for i in range(NB)]
            YO = [sm("yo%d" % i, (128, TC)) for i in range(NB)]

            pool(lambda e: e.iota(iota1[:], pattern=[[1, TC]], base=1, channel_multiplier=0, allow_small_or_imprecise_dtypes=True), writes=[iota1])
            pool(lambda e: e.memset(onesT[:], 1.0), writes=[onesT])
            pool(lambda e: e.memset(hpi[:], TWO_PI / 4), writes=[hpi])
            pool(lambda e: e.memset(sgn1[:], 1.0), writes=[sgn1])
            pool(lambda e: e.memset(sgn1[64:128, :], -1.0), writes=[sgn1])
            pool(lambda e: e.memset(carry[:], 0.0), writes=[carry])
            pool(lambda e: e.memset(rowmask[:], 1.0), writes=[rowmask])
            pool(lambda e: e.affine_select(out=rowmask[:], in_=rowmask[:], pattern=[[-16, 8]], compare_op=ALU.is_ge, fill=0.0,
                                           base=0, channel_multiplier=1), reads=[rowmask], writes=[rowmask])
            pool(lambda e: e.affine_select(out=rowmask[:], in_=rowmask[:], pattern=[[16, 8]], compare_op=ALU.is_ge, fill=0.0,
                                           base=15, channel_multiplier=-1), reads=[rowmask], writes=[rowmask])
            pool(lambda e: e.memset(colmask[:], 0.0), writes=[colmask])
            for gl in range(8):
                pool(lambda e, gl=gl: e.memset(colmask[:, gl, 16 * gl:16 * gl + 16], 1.0), writes=[colmask])
            for hf in range(2):
                load(lre[hf * 64:(hf + 1) * 64, :], W["s5_lam_re"][l].rearrange("g q -> q g"), lre, slow=True, join=True)
                load(lim[hf * 64:(hf + 1) * 64, :], W["s5_lam_im"][l].rearrange("g q -> q g"), lim, slow=True, join=True)
            load(dtt[:], W["s5_log_dt"][l].partition_broadcast(128), dtt, slow=True)
            load(dcol[:], W["s5_d"][l].rearrange("g c -> (g c)").rearrange("(j q) -> q j", q=128), dcol, slow=True)
            bre = W["s5_b_re"][l].rearrange("g q c -> q g c")
            bim = W["s5_b_im"][l].rearrange("g q c -> q g c")
            load(X1[0:64], bre, X1, join=True)
            load(X1[64:128], bim, X1, join=True)
            load(X2[0:64], bim, X2, join=True)
            load(X2[64:128], bre, X2, join=True)

            def tt_(o, a, b, op):
                dve(lambda e: e.tensor_tensor(out=o[:], in0=a[:], in1=b[:], op=op), reads=[a, b], writes=[o])

            def ts_(o, a, s1, op0, s2=None, op1=None):
                if op1 is None:
                    dve(lambda e: e.tensor_scalar(out=o[:], in0=a[:], scalar1=s1, scalar2=None, op0=op0), reads=[a], writes=[o])
                else:
                    dve(lambda e: e.tensor_scalar(out=o[:], in0=a[:], scalar1=s1, scalar2=s2, op0=op0, op1=op1), reads=[a], writes=[o])

            act(lambda e: e.activation(out=dtt[:], in_=dtt[:], func=AF.Exp), reads=[dtt], writes=[dtt])
            ts_(lre, lre, -1e-4, ALU.min)
            tt_(aa, lre, dtt, ALU.mult)
            tt_(th, lim, dtt, ALU.mult)
            act(lambda e: e.activation(out=rr[:], in_=aa[:], func=AF.Exp), reads=[aa], writes=[rr])
            ts_(ft, th, 1.0 / TWO_PI, ALU.mult)
            ts_(kk0, ft, MAGIC, ALU.add)
            ts_(kk0, kk0, MAGIC, ALU.subtract)
            tt_(fr0, ft, kk0, ALU.subtract)
            act(lambda e: e.activation(out=sn[:], in_=fr0[:], func=AF.Sin, scale=TWO_PI), reads=[fr0], writes=[sn])
            act(lambda e: e.activation(out=af0[:], in_=fr0[:], func=AF.Abs), reads=[fr0], writes=[af0])
            act(lambda e: e.activation(out=cs[:], in_=af0[:], func=AF.Sin, scale=-TWO_PI, bias=hpi[:, 0:1]), reads=[af0, hpi], writes=[cs])
            tt_(nre, rr, cs, ALU.mult)
            ts_(nre, nre, -1.0, ALU.add)
            tt_(nim, rr, sn, ALU.mult)
            tt_(den, lre, lre, ALU.mult)
            tt_(ta, lim, lim, ALU.mult)
            tt_(den, den, ta, ALU.add)
            dve(lambda e: e.reciprocal(out=den[:], in_=den[:]), reads=[den], writes=[den])
            tt_(ta, nre, lre, ALU.mult)
            tt_(tb, nim, lim, ALU.mult)
            tt_(cre, ta, tb, ALU.add)
            tt_(cre, cre, den, ALU.mult)
            tt_(ta, nim, lre, ALU.mult)
            tt_(tb, nre, lim, ALU.mult)
            tt_(cim, ta, tb, ALU.subtract)
            tt_(cim, cim, den, ALU.mult)
            dve(lambda e: e.tensor_scalar(out=CB[:], in0=cim[:], scalar1=sgn1[:, 0:1], scalar2=-1.0, op0=ALU.mult, op1=ALU.mult),
                reads=[cim, sgn1], writes=[CB])
            dve(lambda e: e.tensor_scalar(out=CC[:], in0=cre[:], scalar1=sgn1[:, 0:1], scalar2=None, op0=ALU.mult),
                reads=[cre, sgn1], writes=[CC])

            def bc(t_):
                return t_[:].unsqueeze(2).broadcast_to([128, 32, 16])

            def v3(t_):
                return t_[:].rearrange("q (g c) -> q g c", c=16)
            dve(lambda e: e.tensor_tensor(out=v3(BB), in0=X1[:], in1=bc(cre), op=ALU.mult), reads=[X1, cre], writes=[BB])
            dve(lambda e: e.tensor_tensor(out=v3(tmpB), in0=X2[:], in1=bc(CB), op=ALU.mult), reads=[X2, CB], writes=[tmpB])
            tt_(BB, BB, tmpB, ALU.add)
            dve(lambda e: e.tensor_tensor(out=v3(BBs), in0=X2[:], in1=bc(CC), op=ALU.mult), reads=[X2, CC], writes=[BBs])
            dve(lambda e: e.tensor_tensor(out=v3(tmpB), in0=X1[:], in1=bc(cim), op=ALU.mult), reads=[X1, cim], writes=[tmpB])
            tt_(BBs, BBs, tmpB, ALU.add)

            cflat_re = W["s5_c_re"][l].rearrange("g c q -> (g c) q")
            cflat_im = W["s5_c_im"][l].rearrange("g c q -> (g c) q")
            bcnt = [0]
            ycnt = [0]
            pend = []
            for j in range(4):
                load(uj[:], S_su[:, j, :], uj, reads=[R["su"]])
                load(Cin[:, 0, :], cflat_re[j * 128:(j + 1) * 128, :], Cin, join=True)
                load(Cin[:, 1, :], cflat_im[j * 128:(j + 1) * 128, :], Cin, join=True)
                load(Cin2[:, 0, :], cflat_im[j * 128:(j + 1) * 128, :], Cin2, join=True)
                load(Cin2[:, 1, :], cflat_re[j * 128:(j + 1) * 128, :], Cin2, join=True)
                for var, src in ((0, BB), (1, BBs)):
                    ps = PS[var]
                    pe(lambda e, ps=ps, src=src, j=j: e.transpose(out=ps[:, 0:128], in_=src[:, j * 128:(j + 1) * 128], identity=ident32[:]),
                       reads=[src, ident32], writes=[ps])
                    for gl in range(8):
                        dve(lambda e, ps=ps, gl=gl, var=var: e.tensor_scalar(out=LB[:, gl, var, :], in0=ps[:, 0:128], scalar1=rowmask[:, gl:gl + 1],
                                                                             scalar2=None, op0=ALU.mult), reads=[ps, rowmask], writes=[LB])
                for var, src, sc_ in ((0, Cin, 1.0), (1, Cin2, -1.0)):
                    ps = PS[2 + var]
                    pe(lambda e, ps=ps, src=src: e.transpose(out=ps[:, 0:128], in_=src[:].rearrange("q a b -> q (a b)"), identity=ident32[:]),
                       reads=[src, ident32], writes=[ps])
                    for gl in range(8):
                        if var == 0:
                            dve(lambda e, ps=ps, gl=gl: e.scalar_tensor_tensor(out=LC[:, gl, 0, :], in0=ps[:, 0:128], scalar=sgn1[:, 0:1],
                                                                               in1=colmask[:, gl, :], op0=ALU.mult, op1=ALU.mult),
                                reads=[ps, sgn1, colmask], writes=[LC])
                        else:
                            dve(lambda e, ps=ps, gl=gl: e.scalar_tensor_tensor(out=LC[:, gl, 1, :], in0=ps[:, 0:128], scalar=-1.0,
                                                                               in1=colmask[:, gl, :], op0=ALU.mult, op1=ALU.mult),
                                reads=[ps, colmask], writes=[LC])
                its = [(tc, gl) for tc in range(NTC) for gl in range(8)]
                bufs = {}

                def head(i, j=j):
                    tc, gl = its[i]
                    g = 8 * j + gl
                    b = bcnt[0] % NB
                    bcnt[0] += 1
                    bufs[i] = b
                    tt, kk, fr, afr, snT, csT, rt = TT[b], KK[b], FR[b], AFR[b], SN[b], CS[b], RT[b]
                    pool(lambda e: e.tensor_scalar(out=tt[:], in0=iota1[:], scalar1=float(tc * TC), scalar2=ft[:, g:g + 1],
                                                   op0=ALU.add, op1=ALU.mult), reads=[iota1, ft], writes=[tt])
                    dve(lambda e: e.tensor_scalar(out=kk[:], in0=tt[:], scalar1=MAGIC, scalar2=MAGIC, op0=ALU.add, op1=ALU.subtract), reads=[tt], writes=[kk])
                    dve(lambda e: e.tensor_tensor(out=fr[:], in0=tt[:], in1=kk[:], op=ALU.subtract), reads=[tt, kk], writes=[fr])
                    act(lambda e: e.activation(out=snT[:], in_=fr[:], func=AF.Sin, scale=TWO_PI), reads=[fr], writes=[snT])
                    act(lambda e: e.activation(out=afr[:], in_=fr[:], func=AF.Abs), reads=[fr], writes=[afr])
                    act(lambda e: e.activation(out=csT[:], in_=afr[:], func=AF.Sin, scale=-TWO_PI, bias=hpi[:, 0:1]), reads=[afr, hpi], writes=[csT])
                    act(lambda e: e.activation(out=rt[:], in_=onesT[:], func=AF.Copy, scale=rr[:, g:g + 1]), reads=[onesT, rr], writes=[rt])

                def mid(i, j=j):
                    tc, gl = its[i]
                    g = 8 * j + gl
                    b = bufs.pop(i)
                    if gl == 0:
                        ycnt[0] += 1
                    yps = PS[6 + ycnt[0] % 2]
                    yo = YO[ycnt[0] % 2]
                    usl = uj[:, tc * TC:(tc + 1) * TC]
                    snT, csT, rt = SN[b], CS[b], RT[b]
                    t1, t2, st, z1, z2 = T1[b], T2[b], STt[b], Z1[b], Z2[b]
                    pa = PS[2 * (b % 2)]
                    pb = PS[2 * (b % 2) + 1]
                    mm(pa[:], LB[:, gl, 0, :], usl, True, True, [LB, uj], [pa])
                    mm(pb[:], LB[:, gl, 1, :], usl, True, True, [LB, uj], [pb])
                    dve(lambda e: e.tensor_tensor(out=t1[:], in0=pa[:], in1=csT[:], op=ALU.mult), reads=[pa, csT], writes=[t1])
                    dve(lambda e: e.tensor_tensor(out=t2[:], in0=pb[:], in1=snT[:], op=ALU.mult), reads=[pb, snT], writes=[t2])
                    dve(lambda e: e.tensor_tensor(out=t1[:], in0=t1[:], in1=t2[:], op=ALU.add), reads=[t1, t2], writes=[t1])
                    dve(lambda e: e.tensor_tensor_scan(out=st[:], data0=rt[:], data1=t1[:], initial=carry[:, g:g + 1],
                                                       op0=ALU.mult, op1=ALU.add), reads=[rt, t1, carry], writes=[st])
                    act(lambda e: e.activation(out=carry[:, g:g + 1], in_=st[:, TC - 1:TC], func=AF.Copy), reads=[st], writes=[carry])
                    pool(lambda e: e.tensor_tensor(out=z1[:], in0=st[:], in1=csT[:], op=ALU.mult), reads=[st, csT], writes=[z1])
                    pool(lambda e: e.tensor_tensor(out=z2[:], in0=st[:], in1=snT[:], op=ALU.mult), reads=[st, snT], writes=[z2])

                    def tail():
                        mm(yps[:], LC[:, gl, 0, :], z1[:], gl == 0, False, [LC, z1], [yps])
                        mm(yps[:], LC[:, gl, 1, :], z2[:], False, gl == 7, [LC, z2], [yps])
                        if gl == 7:
                            dve(lambda e: e.scalar_tensor_tensor(out=yo[:], in0=usl, scalar=dcol[:, j:j + 1], in1=yps[:],
                                                                 op0=ALU.mult, op1=ALU.add), reads=[uj, dcol, yps], writes=[yo])
                            store(S_ys5[:, j, tc * TC:(tc + 1) * TC], yo[:], yo, R["ys5"])
                    pend.append(tail)
                    if len(pend) > 1:
                        pend.pop(0)()

                LEAD = 2
                for i in range(len(its) + LEAD):
                    if i < len(its):
                        head(i)
                    if i >= LEAD:
                        mid(i - LEAD)
                while pend:
                    pend.pop(0)()
            p.flush()
        _after_reset(p)


    def phase_dil(l):
        NU = NT // 128
        with nc.reset_on_exit():
            QT = [p.sb("dq%d" % i, [128, NT], BF16) for i in range(2)]
            KT = [p.sb("dk%d" % i, [128, NT], BF16) for i in range(2)]
            VP = [p.sb("dv%d" % i, [128, NU, 65], BF16) for i in range(2)]
            VT = [p.sb("dvt%d" % i, [128, NT], BF16) for i in range(2)]
            ACC = p.sb("dacc", [65, NT], F32)
            band = p.sb("band", [128, 2, 128], BF16)
            bandf = p.sb("bandf", [128, 2, 128], F32)
            sel = p.sb("dsel", [65, 64], F32)
            nb = p.sb("dnb", [128, 1], F32)
            EE = [p.sb("dee%d" % i, [128, 256], BF16) for i in range(4)]
            EM = [p.sb("dem%d" % i, [128, 256], BF16) for i in range(4)]
            rec = p.sb("drec", [64, 512], F32)
            YC = [p.sb("dyc%d" % i, [64, 512], BF16) for i in range(2)]
            pool(lambda e: e.memset(bandf[:], 1.0), writes=[bandf])
            pool(lambda e: e.affine_select(out=bandf[:, 0, :], in_=bandf[:, 0, :], pattern=[[-1, 128]], compare_op=ALU.is_ge, fill=0.0,
                                           base=0, channel_multiplier=1), reads=[bandf], writes=[bandf])
            pool(lambda e: e.affine_select(out=bandf[:, 1, :], in_=bandf[:, 1, :], pattern=[[1, 128]], compare_op=ALU.is_ge, fill=0.0,
                                           base=0, channel_multiplier=-1), reads=[bandf], writes=[bandf])
            pool(lambda e: e.tensor_copy(out=band[:], in_=bandf[:]), reads=[bandf], writes=[band])
            pool(lambda e: e.memset(sel[:], 0.0), writes=[sel])
            pool(lambda e: e.memset(sel[64:65, :], 1.0), writes=[sel])
            pool(lambda e: e.memset(nb[:], -8.0), writes=[nb])
            for t_ in VP:
                pool(lambda e, t_=t_: e.memset(t_[:, :, 64:65], 1.0), writes=[t_])
            lc = [0]
            ec = [0]
            pend = []
            for hs in range(4):
                hp, hl = hs // 2, hs % 2
                psl = slice(hl * 64, (hl + 1) * 64)
                for g in range(3):
                    d = DIL[g]
                    nblk = NT // (d * 128)
                    qt, kt, vp = QT[lc[0] % 2], KT[lc[0] % 2], VP[lc[0] % 2]
                    lc[0] += 1
                    load(qt[psl, :], S_cq[g][psl, hp, :, :].rearrange("q r s -> q (r s)"), qt, reads=[R["cq"]])
                    load(kt[psl, :], S_ck[g][psl, hp, :, :].rearrange("q r s -> q (r s)"), kt, reads=[R["ck"]])
                    vt = VT[(lc[0] - 1) % 2]
                    load(vt[:, :], S_cv[g][:, hp, :, :].rearrange("q r s -> q (r s)"), vt, reads=[R["cv"]])
                    v3_ = vt[:].rearrange("q (r s) -> q r s", r=d)
                    for u in range(d * nblk):
                        r_, n_ = u // nblk, u % nblk
                        ptv = PS[6 + u % 2]
                        ptv_b = ptv.t[:].bitcast(BF16)
                        pe(lambda e, ptv_b=ptv_b, r_=r_, n_=n_, v3_=v3_: e.transpose(out=ptv_b[:, 0:128], in_=v3_[:, r_, n_ * 128:(n_ + 1) * 128],
                                                                                    identity=identb[:]), reads=[vt, identb], writes=[ptv])
                        act(lambda e, ptv_b=ptv_b, u=u, vp=vp, hl=hl: e.activation(out=vp[:, u, 0:64], in_=ptv_b[:, hl * 64:(hl + 1) * 64], func=AF.Copy),
                            reads=[ptv], writes=[vp])
                    if cfg.dbg and hs == 0 and g == 1:
                        p.op("sp", lambda e, vp=vp: e.dma_start(out=D_vp, in_=vp[:]), reads=[vp], writes=[R["out"]], dma=vp, join=True)
                    q3 = qt[:].rearrange("q (r s) -> q r s", r=d)
                    k3 = kt[:].rearrange("q (r s) -> q r s", r=d)
                    acc3 = ACC[:].rearrange("q (s r) -> q r s", r=d)
                    BU = min(4, nblk)
                    for r in range(d):
                        for n0 in range(0, nblk, BU):
                            ops_ = PS[4 + (ec[0] // BU) % 2]
                            for n in range(n0, n0 + BU):
                                u = r * nblk + n
                                stp = PS[ec[0] % 4]
                                ee = EE[ec[0] % 4]
                                em = EM[ec[0] % 4]
                                ec[0] += 1
                                qa = q3[psl, r, n * 128:(n + 1) * 128]
                                c0 = 0 if n > 0 else 128
                                if n > 0:
                                    mm(stp[:, 0:128], k3[psl, r, (n - 1) * 128:n * 128], qa, True, True, [kt, qt], [stp])
                                mm(stp[:, 128:256], k3[psl, r, n * 128:(n + 1) * 128], qa, True, True, [kt, qt], [stp])
                                act(lambda e, ee=ee, stp=stp, c0=c0: e.activation(out=ee[:, c0:256], in_=stp[:, c0:256], func=AF.Exp,
                                                                                 bias=nb[:, 0:1], scale=0.125), reads=[stp, nb], writes=[ee])
                                dve(lambda e, ee=ee, em=em, c0=c0: e.tensor_tensor(out=em[:, c0:256], in0=ee[:, c0:256],
                                                                                   in1=band[:].rearrange("q a b -> q (a b)")[:, c0:256], op=ALU.mult),
                                    reads=[ee, band], writes=[em])
                                oc = (n - n0) * 128
                                last = (n == n0 + BU - 1)
                                dst = acc3[:, r, n0 * 128:(n0 + BU) * 128]

                                def tail(n=n, u=u, oc=oc, ops_=ops_, em=em, vp=vp, last=last, dst=dst, g=g, BU=BU):
                                    if n > 0:
                                        mm(ops_[0:65, oc:oc + 128], vp[:, u - 1, :], em[:, 0:128], True, False, [vp, em], [ops_])
                                        mm(ops_[0:65, oc:oc + 128], vp[:, u, :], em[:, 128:256], False, True, [vp, em], [ops_])
                                    else:
                                        mm(ops_[0:65, oc:oc + 128], vp[:, u, :], em[:, 128:256], True, True, [vp, em], [ops_])
                                    if last:
                                        if g == 0:
                                            act(lambda e: e.activation(out=dst, in_=ops_[0:65, 0:BU * 128], func=AF.Copy), reads=[ops_], writes=[ACC])
                                        else:
                                            dve(lambda e: e.tensor_tensor(out=dst, in0=ops_[0:65, 0:BU * 128], in1=dst, op=ALU.add),
                                                reads=[ops_, ACC], writes=[ACC])
                                pend.append(tail)
                                if len(pend) > 2:
                                    pend.pop(0)()
                    while pend:
                        pend.pop(0)()
                for c in range(NT // 512):
                    bcp = PS[6 + c % 2]
                    mm(bcp[0:64, :], sel[:, :], ACC[:, c * 512:(c + 1) * 512], True, True, [sel, ACC], [bcp])
                    dve(lambda e, bcp=bcp: e.reciprocal(out=rec[:], in_=bcp[0:64, :]), reads=[bcp], writes=[rec])
                    yc = YC[c % 2]
                    dve(lambda e, yc=yc, c=c: e.tensor_tensor(out=yc[:], in0=ACC[0:64, c * 512:(c + 1) * 512], in1=rec[:], op=ALU.mult),
                        reads=[ACC, rec], writes=[yc])
                    store(S_yc[:, hs, c * 512:(c + 1) * 512], yc[:], yc, R["yc"])
            p.flush()
        _after_reset(p)

    def token_phase(lpost, lpre):
        nonlocal xt, hb, ab, SQ, SQB, rs, RSD, SG, WS, STG, xin, xtr, hbr, abr
        with nc.reset_on_exit():
            xt = p.sb("xt", [128, 8, T], F32)
            hb = p.sb("hb", [128, 8, T], BF16)
            ab = p.sb("ab", [128, 22, T], BF16)
            SQ = [p.sb("sq%d" % i, [128, T], F32) for i in range(2)]
            SQB = [p.sb("sqb%d" % i, [128, T], BF16) for i in range(2)]
            rs = p.sb("rs", [128, T], F32)
            RSD = [p.sb("rsd%d" % i, [128, T], F32) for i in range(2)]
            SG = [p.sb("sg%d" % i, [128, T], F32) for i in range(2)]
            WS = [p.sb("ws%d" % i, [128, 4096], BF16) for i in range(NWS)]
            STG = [p.sb("stg%d" % i, [128, 512], F32) for i in range(NST)]
            xin = p.sb("xin", [128, 4, D], F32)
            xtr = [Res("xt%d" % i) for i in range(8)]
            hbr = [Res("hb%d" % i) for i in range(8)]
            abr = [Res("ab%d" % i) for i in range(22)]
            if lpost is not None:
                ys5t = p.sb("ys5t", [128, 4, T], F32)
                tm_a = p.sb("tm_a", [128, 4, T], F32)
                tm_b = p.sb("tm_b", [128, 4, T], F32)
                yg = p.sb("yg", [128, 4, T], BF16)
                yb = p.sb("yb", [128, 4, T], BF16)
                yat = p.sb("yat", [64, 8, T], BF16)
                yct = p.sb("yct", [64, 4, T], BF16)
                gt = p.sb("gt", [128, 24, T], BF16)
                M1 = [p.sb("m1_%d" % i, [128, T], F32) for i in range(2)]
                M2 = [p.sb("m2_%d" % i, [128, T], F32) for i in range(2)]
                sgb = [p.sb("sgb%d" % i, [128, T], F32) for i in range(2)]
                ybr = [Res("yb%d" % i) for i in range(4)]

            def post_mixer(l, t0):
                tsl = slice(t0, t0 + T)
                p.op("sp", lambda e: e.dma_start(out=xt[:], in_=S_xT[:, :, tsl]), reads=[R["xT"]], writes=xtr, dma=xt)
                load(ys5t[:], S_ys5[:, :, tsl], ys5t, reads=[R["ys5"]])
                load(yat[:], S_ya[:, :, tsl], yat, reads=[R["ya"]])
                load(yct[:], S_yc[:, :, tsl], yct, reads=[R["yc"]])
                load(gt[:], S_g[:, :, tsl], gt, reads=[R["g"]])
                f2 = lambda t_: t_[:].rearrange("q a b -> q (a b)")
                act(lambda e: e.activation(out=f2(tm_a), in_=f2(ys5t), func=AF.Square), reads=[ys5t], writes=[tm_a])
                dve(lambda e: e.tensor_scalar(out=f2(tm_a), in0=f2(tm_a), scalar1=0.044715, scalar2=1.0, op0=ALU.mult, op1=ALU.add),
                    reads=[tm_a], writes=[tm_a])
                dve(lambda e: e.tensor_tensor(out=f2(tm_a), in0=f2(tm_a), in1=f2(ys5t), op=ALU.mult), reads=[tm_a, ys5t], writes=[tm_a])
                act(lambda e: e.activation(out=f2(tm_b), in_=f2(tm_a), func=AF.Sigmoid, scale=1.5957691216057308), reads=[tm_a], writes=[tm_b])
                dve(lambda e: e.tensor_tensor(out=f2(yg), in0=f2(tm_b), in1=f2(ys5t), op=ALU.mult), reads=[tm_b, ys5t], writes=[yg])
                sa, wa_ = wload(WB[l]["s5_glu_a"], WBres[l]["s5_glu_a"], 4, 512)
                sb_, wb_ = wload(WB[l]["s5_glu_b"], WBres[l]["s5_glu_b"], 4, 512)
                for oc in range(4):
                    pa, pb = psA(), psA()
                    for k in range(4):
                        mm(pa[:], wa_[:, k, oc * 128:(oc + 1) * 128], yg[:, k, :], k == 0, k == 3, [sa, yg], [pa])
                    for k in range(4):
                        mm(pb[:], wb_[:, k, oc * 128:(oc + 1) * 128], yg[:, k, :], k == 0, k == 3, [sb_, yg], [pb])
                    sg_ = sgb[oc % 2]
                    act(lambda e, sg_=sg_, pb=pb: e.activation(out=sg_[:], in_=pb[:], func=AF.Sigmoid), reads=[pb], writes=[sg_])
                    dve(lambda e, sg_=sg_, pa=pa, oc=oc: e.tensor_tensor(out=yb[:, oc, :], in0=pa[:], in1=sg_[:], op=ALU.mult),
                        reads=[pa, sg_], writes=[ybr[oc]])
                Wa, Wb, Wc = WB[l]["w_branch_a"], WB[l]["w_branch_b"], WB[l]["w_branch_c"]
                for half in range(2):
                    cs_ = slice(half * 512, (half + 1) * 512)
                    s1, wa = wload(Wa[:, cs_], WBres[l]["w_branch_a"], 8, 512, kp=64)
                    s2, wb = wload(Wb[:, cs_], WBres[l]["w_branch_b"], 4, 512)
                    s3, wc = wload(Wc[:, cs_], WBres[l]["w_branch_c"], 4, 512, kp=64)
                    for oc in range(4):
                        c = half * 4 + oc
                        osl = slice(oc * 128, (oc + 1) * 128)
                        pa, pb, pc = psA(), psA(), psA()
                        for h in range(8):
                            mm(pa[:], wa[:, h, osl], yat[:, h, :], h == 0, h == 7, [s1, yat], [pa])
                        for k in range(4):
                            mm(pb[:], wb[:, k, osl], yb[:, k, :], k == 0, k == 3, [s2, ybr[k]], [pb])
                        for k in range(4):
                            mm(pc[:], wc[:, k, osl], yct[:, k, :], k == 0, k == 3, [s3, yct], [pc])
                        m1, m2 = M1[c % 2], M2[c % 2]
                        dve(lambda e, m1=m1, pa=pa, c=c: e.tensor_tensor(out=m1[:], in0=pa[:], in1=gt[:, c, :], op=ALU.mult), reads=[pa, gt], writes=[m1])
                        dve(lambda e, m2=m2, pb=pb, c=c: e.tensor_tensor(out=m2[:], in0=pb[:], in1=gt[:, 8 + c, :], op=ALU.mult), reads=[pb, gt], writes=[m2])
                        pool(lambda e, m1=m1, m2=m2: e.tensor_tensor(out=m1[:], in0=m1[:], in1=m2[:], op=ALU.add), reads=[m1, m2], writes=[m1])
                        dve(lambda e, m2=m2, pc=pc, c=c: e.tensor_tensor(out=m2[:], in0=pc[:], in1=gt[:, 16 + c, :], op=ALU.mult), reads=[pc, gt], writes=[m2])
                        pool(lambda e, m1=m1, m2=m2, c=c: e.tensor_tensor(out=hb[:, c, :], in0=m1[:], in1=m2[:], op=ALU.add), reads=[m1, m2], writes=[hbr[c]])
                Wo = WB[l]["w_out"]
                for half in range(2):
                    so, wo = wload(Wo[:, half * 512:(half + 1) * 512], WBres[l]["w_out"], 8, 512)
                    for oc in range(4):
                        c = half * 4 + oc
                        ps = psA()
                        for k in range(8):
                            mm(ps[:], wo[:, k, oc * 128:(oc + 1) * 128], hb[:, k, :], k == 0, k == 7, [so, hbr[k]], [ps])
                        dve(lambda e, ps=ps, c=c: e.tensor_tensor(out=xt[:, c, :], in0=ps[:], in1=xt[:, c, :], op=ALU.add), reads=[ps, xtr[c]], writes=[xtr[c]])

            for tt in range(NTT):
                t0 = tt * T
                if lpost is None:
                    load_x_first(t0)
                else:
                    post_mixer(lpost, t0)
                    rmsnorm_fm(2, lpost)
                    ffn(lpost, "ffn2")
                if lpre is not None and cfg.sub < 9:
                    if cfg.sub >= 2:
                        rmsnorm_fm(0, lpre)
                    if cfg.sub >= 3:
                        ffn(lpre, "ffn1")
                    if cfg.sub >= 4:
                        rmsnorm_fm(1, lpre)
                        proj(lpre, t0)
                    store_x_last(t0)
                elif lpre is not None:
                    rmsnorm_fm(0, lpre)
                    ffn(lpre, "ffn1")
                    rmsnorm_fm(1, lpre)
                    proj(lpre, t0)
                    p.op("act", lambda e, t0=t0: e.dma_start(out=S_xT[:, :, t0:t0 + T], in_=xt[:]), reads=xtr, writes=[R["xT"]], dma=xt, join=True)
                else:
                    store_x_last(t0)
            p.flush()
        _after_reset(p)

    p.flush()
    if cfg.stage == 0:
        return nc
    token_phase(None, 0)
    for l in range(L):
        if cfg.stage in (2, 99):
            phase_dsa(l)
        if cfg.stage in (3, 99):
            phase_s5(l)
        if cfg.stage in (4, 99):
            phase_dil(l)
        if cfg.stage == 99:
            token_phase(l, l + 1 if l + 1 < L else None)
    print("ops", p.nops, "sems", p.nsem, "sbuf left", nc.sbuf_bytes_remaining)
    return nc


def _run(cfg, inputs):
    nc = build(cfg)
    x = np.ascontiguousarray(inputs["x"], dtype=np.float32)
    in_maps = []
    for c in range(cfg.NCORES):
        m = {"x": np.ascontiguousarray(x[c])}
        for k in WEIGHT_SHAPES:
            m[k] = np.ascontiguousarray(inputs[k], dtype=np.float32)
        in_maps.append(m)
    res = run_bass_kernel_spmd(nc, in_maps, core_ids=list(range(cfg.NCORES)))
    return res


def kernel(**inputs):
    cfg = Cfg()
    res = _run(cfg, inputs)
    return np.stack([r["out"] for r in res.results], axis=0).astype(np.float32)
```
